# Optimizing a Trainium2 kernel written in Bass

```python
import jax, jax.numpy as jnp
from jax import lax
import numpy as np

D_MODEL = 1024
BATCH = 8
SEQ = 4096
DEPTH = 2

CHUNK = 64
N_MIXERS = 2
N_RWKV = (DEPTH + N_MIXERS - 1) // N_MIXERS
N_GLA = DEPTH // N_MIXERS
D_FF = 4 * D_MODEL
NORM_EPS = 1e-5

RWKV_HEAD = 64
RWKV_HEADS = D_MODEL // RWKV_HEAD
N_SHIFT_MIX = 6
DECAY_LORA = max(32, int(round(1.8 * D_MODEL ** 0.5 / 32)) * 32)
A_LORA = max(32, int(round(1.8 * D_MODEL ** 0.5 / 32)) * 32)
GATE_LORA = max(32, int(round(0.6 * D_MODEL ** 0.8 / 32)) * 32)
GN_EPS = 1e-5 * RWKV_HEAD

GLA_HEADS = 4
GLA_QK = D_MODEL // 2
GLA_V = D_MODEL
GLA_DK = GLA_QK // GLA_HEADS
GLA_DV = GLA_V // GLA_HEADS
GLA_GATE_LORA = 16
GLA_GATE_NORMALIZER = 16.0
GLA_IN = 2 * GLA_QK + 2 * GLA_V + GLA_GATE_LORA

kernel_name = "rwkv7_gla_interleaved_hybrid"


def rms_norm(x, g, eps=NORM_EPS):
    xf = x.astype(jnp.float32)
    y = xf * lax.rsqrt(jnp.mean(xf * xf, axis=-1, keepdims=True) + eps)
    return (y * g.astype(jnp.float32)).astype(x.dtype)


def token_shift(x):
    return jnp.pad(x, ((0, 0), (1, 0), (0, 0)))[:, :-1]


def sq_relu_mlp(x, w_up, w_down):
    h = jax.nn.relu(x @ w_up)
    return (h * h) @ w_down


def rwkv7_step(state, inp):
    r_t, w_t, k_t, v_t, a_t, b_t = inp
    sa = jnp.einsum('bhvk,bhk->bhv', state, a_t)
    state = (state * w_t[:, :, None, :]
             + sa[..., None] * b_t[:, :, None, :]
             + v_t[..., None] * k_t[:, :, None, :])
    y = jnp.einsum('bhvk,bhk->bhv', state, r_t)
    return state, y


def rwkv7_time_mix(x, mu, w_rkv, w0, w1, w2, a0, a1, a2, g1, g2, k_k, k_a, r_k, lnx_w, lnx_b, w_o):
    b, s, d = x.shape
    f32 = jnp.float32
    xx = token_shift(x) - x
    xm = x[None] + xx[None] * mu[:, None, None, :]
    rkv = jnp.einsum('nbsd,nde->nbse', xm[:3], w_rkv)
    r, k, v = rkv[0], rkv[1], rkv[2]
    xw, xa, xg = xm[3], xm[4], xm[5]
    w_log = -jax.nn.softplus(-(w0 + jnp.tanh(xw @ w1) @ w2).astype(f32)) - 0.5
    decay = jnp.exp(-jnp.exp(w_log))
    a = jax.nn.sigmoid((a0 + (xa @ a1) @ a2).astype(f32))
    g = jax.nn.sigmoid(xg @ g1) @ g2

    def heads(t):
        return t.reshape(b, s, RWKV_HEADS, RWKV_HEAD)

    kk = heads((k * k_k).astype(f32))
    kk = kk / jnp.maximum(jnp.linalg.norm(kk, axis=-1, keepdims=True), 1e-12)
    k_mod = k.astype(f32) * (1.0 + (a - 1.0) * k_a)
    r_h, k_h, v_h = heads(r.astype(f32)), heads(k_mod), heads(v.astype(f32))
    w_h, a_h = heads(decay), heads(a)

    def tm(t):
        return jnp.moveaxis(t, 1, 0)

    xs = (tm(r_h), tm(w_h), tm(k_h), tm(v_h), tm(-kk), tm(kk * a_h))
    s0 = jnp.zeros((b, RWKV_HEADS, RWKV_HEAD, RWKV_HEAD), f32)
    _, y = lax.scan(rwkv7_step, s0, xs)
    y = jnp.moveaxis(y, 0, 1)
    mean = jnp.mean(y, axis=-1, keepdims=True)
    var = jnp.mean(jnp.square(y - mean), axis=-1, keepdims=True)
    y = ((y - mean) * lax.rsqrt(var + GN_EPS)).reshape(b, s, d) * lnx_w + lnx_b
    bonus = jnp.sum(r_h * k_h * r_k, axis=-1, keepdims=True) * v_h
    y = (y + bonus.reshape(b, s, d)) * g
    return y.astype(x.dtype) @ w_o


def gla_time_mix(x, w_in, w_gk2, b_gk2, gnorm_g, w_o):
    b, s, d = x.shape
    f32 = jnp.float32
    nc = s // CHUNK
    proj = x @ w_in
    q, k, v, og, gk_low = jnp.split(
        proj, [GLA_QK, 2 * GLA_QK, 2 * GLA_QK + GLA_V, 2 * GLA_QK + 2 * GLA_V], axis=-1)
    gk = jax.nn.log_sigmoid((gk_low @ w_gk2 + b_gk2).astype(f32)) / GLA_GATE_NORMALIZER
    q = q * (GLA_DK ** -0.5)

    def chunks(t, dh):
        return t.astype(f32).reshape(b, nc, CHUNK, GLA_HEADS, dh).transpose(1, 0, 3, 2, 4)

    causal = jnp.tril(jnp.ones((CHUNK, CHUNK), dtype=bool))

    def step(state, inp):
        qc, kc, vc, gc = inp
        cum = jnp.cumsum(gc, axis=2)
        inter = jnp.einsum('bhtk,bhkv->bhtv', qc * jnp.exp(cum), state)
        diff = cum[:, :, :, None, :] - cum[:, :, None, :, :]
        decay = jnp.exp(jnp.where(causal[:, :, None], diff, -jnp.inf))
        scores = jnp.einsum('bhtk,bhsk,bhtsk->bhts', qc, kc, decay)
        intra = jnp.einsum('bhts,bhsv->bhtv', scores, vc)
        last = cum[:, :, -1:, :]
        state = (jnp.exp(last[:, :, 0, :])[..., None] * state
                 + jnp.einsum('bhsk,bhsv->bhkv', kc * jnp.exp(last - cum), vc))
        return state, inter + intra

    s0 = jnp.zeros((b, GLA_HEADS, GLA_DK, GLA_DV), f32)
    _, o = lax.scan(step, s0, (chunks(q, GLA_DK), chunks(k, GLA_DK),
                               chunks(v, GLA_DV), chunks(gk, GLA_DK)))
    o = o.transpose(1, 0, 3, 2, 4).reshape(b, s, GLA_HEADS, GLA_DV)
    o = o * lax.rsqrt(jnp.mean(o * o, axis=-1, keepdims=True) + NORM_EPS) * gnorm_g.astype(f32)
    o = o.reshape(b, s, d) * jax.nn.silu(og.astype(f32))
    return o.astype(x.dtype) @ w_o


def setup_inputs(seed: int = 0) -> dict:
    key = jax.random.key(seed)
    ks = jax.random.split(key, 32)
    f32 = jnp.float32

    def nrm(k, shape, scale):
        return jax.random.normal(k, shape, f32) * scale

    D = D_MODEL
    return {
        "x": nrm(ks[0], (BATCH, SEQ, D), 1.0),
        "norm_mix_g": 1.0 + nrm(ks[1], (DEPTH, D), 0.02),
        "norm_ffn_g": 1.0 + nrm(ks[2], (DEPTH, D), 0.02),
        "mlp_up": nrm(ks[3], (DEPTH, D, D_FF), D ** -0.5),
        "mlp_down": nrm(ks[4], (DEPTH, D_FF, D), D_FF ** -0.5),
        "rwkv_mu": jax.random.uniform(ks[5], (N_RWKV, N_SHIFT_MIX, D), f32),
        "rwkv_w_rkv": nrm(ks[6], (N_RWKV, 3, D, D), D ** -0.5),
        "rwkv_w0": nrm(ks[7], (N_RWKV, D), 1.0),
        "rwkv_w1": nrm(ks[8], (N_RWKV, D, DECAY_LORA), D ** -0.5),
        "rwkv_w2": nrm(ks[9], (N_RWKV, DECAY_LORA, D), 0.1 * DECAY_LORA ** -0.5),
        "rwkv_a0": nrm(ks[10], (N_RWKV, D), 0.1),
        "rwkv_a1": nrm(ks[11], (N_RWKV, D, A_LORA), D ** -0.5),
        "rwkv_a2": nrm(ks[12], (N_RWKV, A_LORA, D), 0.1 * A_LORA ** -0.5),
        "rwkv_g1": nrm(ks[13], (N_RWKV, D, GATE_LORA), D ** -0.5),
        "rwkv_g2": nrm(ks[14], (N_RWKV, GATE_LORA, D), GATE_LORA ** -0.5),
        "rwkv_k_k": 0.85 + nrm(ks[15], (N_RWKV, D), 0.02),
        "rwkv_k_a": 1.0 + nrm(ks[16], (N_RWKV, D), 0.02),
        "rwkv_r_k": nrm(ks[17], (N_RWKV, RWKV_HEADS, RWKV_HEAD), 0.1),
        "rwkv_lnx_w": 1.0 + nrm(ks[18], (N_RWKV, D), 0.02),
        "rwkv_lnx_b": nrm(ks[19], (N_RWKV, D), 0.02),
        "rwkv_w_o": nrm(ks[20], (N_RWKV, D, D), D ** -0.5),
        "gla_w_in": nrm(ks[21], (N_GLA, D, GLA_IN), D ** -0.5),
        "gla_w_gk2": nrm(ks[22], (N_GLA, GLA_GATE_LORA, GLA_QK), GLA_GATE_LORA ** -0.5),
        "gla_b_gk2": nrm(ks[23], (N_GLA, GLA_QK), 0.1),
        "gla_gnorm_g": 1.0 + nrm(ks[24], (N_GLA, GLA_DV), 0.02),
        "gla_w_o": nrm(ks[25], (N_GLA, GLA_V, D), GLA_V ** -0.5),
        "final_g": 1.0 + nrm(ks[26], (D,), 0.02),
    }


def reference(x, norm_mix_g, norm_ffn_g, mlp_up, mlp_down,
              rwkv_mu, rwkv_w_rkv, rwkv_w0, rwkv_w1, rwkv_w2, rwkv_a0, rwkv_a1, rwkv_a2,
              rwkv_g1, rwkv_g2, rwkv_k_k, rwkv_k_a, rwkv_r_k, rwkv_lnx_w, rwkv_lnx_b, rwkv_w_o,
              gla_w_in, gla_w_gk2, gla_b_gk2, gla_gnorm_g, gla_w_o, final_g):
    h = x
    for i in range(DEPTH):
        hn = rms_norm(h, norm_mix_g[i])
        j = i // N_MIXERS
        if i % N_MIXERS == 0:
            mix = rwkv7_time_mix(hn, rwkv_mu[j], rwkv_w_rkv[j], rwkv_w0[j], rwkv_w1[j], rwkv_w2[j],
                                 rwkv_a0[j], rwkv_a1[j], rwkv_a2[j], rwkv_g1[j], rwkv_g2[j],
                                 rwkv_k_k[j], rwkv_k_a[j], rwkv_r_k[j], rwkv_lnx_w[j],
                                 rwkv_lnx_b[j], rwkv_w_o[j])
        else:
            mix = gla_time_mix(hn, gla_w_in[j], gla_w_gk2[j], gla_b_gk2[j], gla_gnorm_g[j], gla_w_o[j])
        h = h + mix
        h = h + sq_relu_mlp(rms_norm(h, norm_ffn_g[i]), mlp_up[i], mlp_down[i])
    return rms_norm(h, final_g)
```

```python
import contextlib
import numpy as np
import concourse.bass as bass
import concourse.mybir as mybir
from concourse.bass_utils import run_bass_kernel_spmd

F32 = mybir.dt.float32
BF16 = mybir.dt.bfloat16
AF = mybir.ActivationFunctionType
ALU = mybir.AluOpType
AX = mybir.AxisListType

D = 1024
KC = 8
DFF = 4096
NEPS = 1e-5
import os
SAME_ENGINE_SYNC = os.environ.get('SES', '1') == '1'
NOCAST = os.environ.get('NOCAST', '0') == '1'
RW_STOP = os.environ.get('RW_STOP', '')


class _Stop(Exception):
    pass


def chk(tag):
    if RW_STOP == tag:
        raise _Stop()


class Prog:
    ENGS = ("pe", "dve", "act", "pool", "sp")

    def __init__(self, nc):
        self.nc = nc
        self.ops = {e: [] for e in self.ENGS}
        self.lastw = {}
        self.readers = {}
        self.dma_cnt = {}
        self.waited = {e: {} for e in self.ENGS}
        self.marked = {e: set() for e in self.ENGS}
        self.pe_cls = None
        self.pe_tok = None

    def _need(self, eng, tok, waits):
        if tok is None:
            return
        if tok[0] == "eng":
            _, e2, idx2 = tok
            if e2 == eng and (eng == "pe" or not SAME_ENGINE_SYNC):
                return
            if e2 == eng and eng == "sp":
                return
            k = ("eng", e2)
            if self.waited[eng].get(k, -1) >= idx2:
                return
            self.waited[eng][k] = idx2
            self.marked[e2].add(idx2)
            waits.append(tok)
        else:
            _, sem, cnt = tok
            k = ("dma", sem)
            if self.waited[eng].get(k, -1) >= cnt:
                return
            self.waited[eng][k] = cnt
            waits.append(tok)

    def add(self, eng, fn, reads=(), writes=(), dma_sem=None, extra=(), force=(), rt=None):
        waits = []
        if eng == "pe" and fn is not None:
            cls = "full" if rt is None else rt
            if self.pe_cls is not None and cls != self.pe_cls:
                force = list(force) + [self.pe_tok]
        for t in force:
            if t is not None and t[0] == "eng":
                self.marked[t[1]].add(t[2])
                waits.append(t)
        for r in reads:
            self._need(eng, self.lastw.get(r), waits)
        for w in writes:
            self._need(eng, self.lastw.get(w), waits)
            for t in self.readers.get(w, ()):
                self._need(eng, t, waits)
        for t in extra:
            self._need(eng, t, waits)
        idx = len(self.ops[eng])
        if dma_sem is not None:
            self.dma_cnt[dma_sem] = self.dma_cnt.get(dma_sem, 0) + 16
            tok = ("dma", dma_sem, self.dma_cnt[dma_sem])
        else:
            tok = ("eng", eng, idx)
        self.ops[eng].append(dict(fn=fn, waits=waits, dma_sem=dma_sem))
        if eng == "pe" and fn is not None:
            self.pe_cls = cls
            self.pe_tok = tok
        for r in reads:
            self.readers.setdefault(r, []).append(tok)
        for w in writes:
            self.lastw[w] = tok
            self.readers[w] = []
        return tok

    def last_tokens(self):
        toks = []
        for e in self.ENGS:
            for i in range(len(self.ops[e]) - 1, -1, -1):
                if self.ops[e][i]["dma_sem"] is None:
                    toks.append(("eng", e, i))
                    break
        for s, c in self.dma_cnt.items():
            toks.append(("dma", s, c))
        return toks

    def barrier(self):
        toks = self.last_tokens()
        for e in self.ENGS:
            self.add(e, None, extra=toks)
        self.lastw = {}
        self.readers = {}

    def emit(self, stack):
        nc = self.nc
        esem = {e: stack.enter_context(nc.semaphore("es_" + e)) for e in self.ENGS}
        dsem = {s: stack.enter_context(nc.semaphore("ds_%d" % i))
                for i, s in enumerate(sorted(self.dma_cnt))}
        tick = {}
        for e in self.ENGS:
            c = 0
            tick[e] = {}
            for i in range(len(self.ops[e])):
                if i in self.marked[e]:
                    c += 1
                    tick[e][i] = c
        block = stack.enter_context(nc.Block())
        sect = dict(pe=block.tensor, dve=block.vector, act=block.scalar,
                    pool=block.gpsimd, sp=block.sync)

        def make(e):
            def body(eng):
                for i, op in enumerate(self.ops[e]):
                    for t in op["waits"]:
                        if t[0] == "eng":
                            eng.wait_ge(esem[t[1]], tick[t[1]][t[2]])
                        else:
                            eng.wait_ge(dsem[t[1]], t[2])
                    if op["fn"] is None:
                        if i in self.marked[e]:
                            eng.drain().then_inc(esem[e], 1)
                        continue
                    ins = op["fn"](eng)
                    if op["dma_sem"] is not None:
                        ins.then_inc(dsem[op["dma_sem"]], 16)
                    elif i in self.marked[e]:
                        ins.then_inc(esem[e], 1)
            return body

        for e in self.ENGS:
            sect[e](make(e))


class Arena:
    def __init__(self, ap, words):
        self.ap = ap
        self.words = words
        self.off = 0

    def reset(self, off=0):
        self.off = off

    def alloc(self, shape, dtype):
        assert shape[0] <= 128
        n = 1
        for s in shape[1:]:
            n *= s
        w = n if dtype == F32 else (n + 1) // 2
        w = (w + 1) // 2 * 2
        assert self.off + w <= self.words, ("SBUF arena overflow", self.off, w, self.words)
        v = self.ap[0:shape[0], self.off:self.off + w]
        self.off += w
        if dtype != F32:
            v = v.bitcast(dtype)
        v = v[:, 0:n]
        if len(shape) == 3:
            v = v.rearrange("p (a b) -> p a b", a=shape[1])
        elif len(shape) == 4:
            v = v.rearrange("p (a b c) -> p a b c", a=shape[1], b=shape[2])
        return v


def build_nc(S, mode="full", dbg=False):
    nc = bass.Bass("TRN2", target_bir_lowering=False)
    NB = S // 512
    stack = contextlib.ExitStack()
    with stack:
        P = Prog(nc)
        dt = {}

        def din(name, shape, dtype=F32):
            dt[name] = nc.dram_tensor(name, list(shape), dtype, kind="ExternalInput").ap()
            return dt[name]

        def dscr(name, shape, dtype):
            return nc.dram_tensor(name, list(shape), dtype, kind="Internal").ap()

        x_d = din("x", [S, D])
        out_d = nc.dram_tensor("out", [S, D], F32, kind="ExternalOutput").ap()
        cst_d = din("cst", [128, CST_W])
        vec_d = din("vec", [128, VEC_W])
        mlp_up_d = din("mlp_up", [2, D, DFF])
        mlp_down_d = din("mlp_down", [2, DFF, D])
        gla_w_in_d = din("gla_w_in", [D, 3088])
        gla_w_o_d = din("gla_w_o", [D, D])
        din("gla_w_gk2", [16, 512])
        rwkv_w_rkv_d = din("rwkv_w_rkv", [3, D, D])
        rwkv_w_o_d = din("rwkv_w_o", [D, D])
        din("rwkv_w1", [D, 64])
        din("rwkv_w2", [64, D])
        din("rwkv_a1", [D, 64])
        din("rwkv_a2", [64, D])
        din("rwkv_g1", [D, 160])
        din("rwkv_g2", [160, D])
        din("rep", [128, 2 * D])
        wrkv_s = dscr("wrkv_s", [3, D, D], BF16)
        rwo_s = dscr("rwo_s", [D, D], BF16)
        win_s = dscr("win_s", [D, 3088], BF16)
        gwo_s = dscr("gwo_s", [D, D], BF16)

        up_s = dscr("up_s", [2, D, DFF], BF16)
        down_s = dscr("down_s", [2, DFF, D], BF16)
        hT_s = dscr("hT_s", [KC, 128, S], F32)

        ARENA_WORDS = 51 * 1024
        arena_t = stack.enter_context(nc.sbuf_tensor("arena", [128, ARENA_WORDS], F32))
        A = Arena(arena_t[:], ARENA_WORDS)
        psum2 = [stack.enter_context(nc.psum_tensor("pp%d" % i, [128, 1024], F32))[:]
                 for i in range(4)]
        psum = [psum2[i // 2][:, (i % 2) * 512:(i % 2 + 1) * 512] for i in range(8)]

        cst = A.alloc([128, CST_W], F32)
        vec = A.alloc([128, VEC_W], F32)
        ident_f = cst[:, C_IDENT:C_IDENT + 128]
        ones_bf = A.alloc([128, 128], BF16)
        P.add("sp", lambda e: e.dma_start(out=cst, in_=cst_d), writes=["cst"], dma_sem="cst")
        P.add("sp", lambda e: e.dma_start(out=vec, in_=vec_d), writes=["vec"], dma_sem="vec")
        P.add("dve", lambda e: e.tensor_copy(out=ones_bf, in_=cst[:, C_ONES:C_ONES + 128]),
              reads=["cst"], writes=["ones_bf"])
        ident_b = A.alloc([128, 128], BF16)
        P.add("dve", lambda e: e.tensor_copy(out=ident_b, in_=ident_f), reads=["cst"], writes=["ident_b"])
        base_off = A.off

        small_w = {}
        if mode in ("rwkv", "full", "gla"):
            specs = []
            if mode in ("rwkv", "full"):
                specs += [("w1", [128, KC, 64], dt["rwkv_w1"].rearrange("(k p) f -> p k f", p=128)),
                          ("a1", [128, KC, 64], dt["rwkv_a1"].rearrange("(k p) f -> p k f", p=128)),
                          ("g1", [128, KC, 160], dt["rwkv_g1"].rearrange("(k p) f -> p k f", p=128)),
                          ("w2", [64, D], dt["rwkv_w2"]), ("a2", [64, D], dt["rwkv_a2"]),
                          ("g2a", [128, D], dt["rwkv_g2"][0:128, :]), ("g2b", [32, D], dt["rwkv_g2"][128:160, :])]
            if mode in ("gla", "full"):
                specs += [("wgk2", [16, 512], dt["gla_w_gk2"])]
            for nm, shp, src in specs:
                small_w[nm] = A.alloc(shp, BF16)
                P.add("pool", lambda e, buf=small_w[nm], src=src: e.dma_start(out=buf, in_=src),
                      writes=[nm], dma_sem=nm)
        base_off = A.off

        cast_state = dict(n=0, toks=[])
        wkeys = {}

        def cast_w(src, dst, rows, cols, name):
            step = max(1, (1 << 20) // (cols * 4))
            wkeys[name] = []
            for r0 in range(0, rows, step):
                r1 = min(rows, r0 + step)
                n = cast_state["n"]
                extra = [cast_state["toks"][n - 2]] if n >= 2 else []
                key = (name, r0)
                wkeys[name].append(key)
                tok = P.add("pool", lambda e, r0=r0, r1=r1: e.dma_start(out=dst[r0:r1, :], in_=src[r0:r1, :]),
                            writes=[key], dma_sem="cast%d" % (n % 2), extra=extra)
                cast_state["toks"].append(tok)
                cast_state["n"] = n + 1

        def cast_mlp(l):
            cast_w(mlp_up_d[l], up_s[l], D, DFF, "up%d" % l)
            cast_w(mlp_down_d[l], down_s[l], DFF, D, "down%d" % l)

        if mode in ("rwkv", "full"):
            for i in range(3):
                cast_w(rwkv_w_rkv_d[i], wrkv_s[i], D, D, "wrkv%d" % i)
            cast_w(rwkv_w_o_d, rwo_s, D, D, "rwo")
        if mode in ("mlp", "full"):
            cast_mlp(0)
        if mode in ("gla", "full"):
            cast_w(gla_w_in_d, win_s, D, 3088, "win")
            cast_w(gla_w_o_d, gwo_s, D, D, "gwo")
        if mode in ("full",):
            cast_mlp(1)

        def phase_load_x():
            A.reset(base_off)
            xin = [A.alloc([128, 4, D], F32) for _ in range(2)]
            hb = [A.alloc([128, KC, 512], F32) for _ in range(2)]
            for b in range(NB):
                xi = xin[b % 2]
                h = hb[b % 2]
                P.add("sp", lambda e, xi=xi, b=b: e.dma_start(
                    out=xi, in_=x_d[b * 512:(b + 1) * 512, :].rearrange("(j p) d -> p j d", p=128)),
                    writes=[("xin", b % 2)], dma_sem="xin%d" % (b % 2))
                for kc in range(KC):
                    ps = psum[kc % 4]
                    for j in range(4):
                        P.add("pe", lambda e, ps=ps, xi=xi, j=j, kc=kc: e.transpose(
                            out=ps[:, j * 128:(j + 1) * 128], in_=xi[:, j, kc * 128:(kc + 1) * 128],
                            identity=ident_f),
                            reads=[("xin", b % 2), "cst"], writes=[("ps", kc % 4)])
                    eng = "dve" if kc % 2 == 0 else "act"
                    if eng == "dve":
                        P.add("dve", lambda e, ps=ps, h=h, kc=kc: e.tensor_copy(out=h[:, kc, :], in_=ps),
                              reads=[("ps", kc % 4)], writes=[("hb", b % 2, kc)])
                    else:
                        P.add("act", lambda e, ps=ps, h=h, kc=kc: e.copy(out=h[:, kc, :], in_=ps),
                              reads=[("ps", kc % 4)], writes=[("hb", b % 2, kc)])
                P.add("sp", lambda e, h=h, b=b: e.dma_start(
                    out=hT_s[:, :, b * 512:(b + 1) * 512].rearrange("k p s -> p k s"), in_=h),
                    reads=[("hb", b % 2, kc) for kc in range(KC)], writes=[("hT", b)],
                    dma_sem="hTw%d" % (b % 2))

        def rms_rstd(h, hkey, sq, sqkey, rstd, rkey, psb):
            for kc in range(KC):
                P.add("act", lambda e, kc=kc: e.activation(out=sq[:, kc, :], in_=h[:, kc, :], func=AF.Square),
                      reads=[hkey + (kc,)], writes=[sqkey + (kc,)])
            for kc in range(KC):
                P.add("pe", lambda e, kc=kc: e.matmul(psum[psb], lhsT=ones_bf, rhs=sq[:, kc, :],
                                                      start=(kc == 0), stop=(kc == KC - 1)),
                      reads=[sqkey + (kc,), "ones_bf"], writes=[("ps", psb)])
            P.add("act", lambda e: e.activation(out=rstd, in_=psum[psb], func=AF.Sqrt,
                                                scale=1.0 / D, bias=eps_col),
                  reads=[("ps", psb), "epsc"], writes=[rkey])
            P.add("dve", lambda e: e.reciprocal(out=rstd, in_=rstd), reads=[rkey], writes=[rkey])

        eps_col = A.alloc([128, 1], F32)
        P.add("dve", lambda e: e.memset(eps_col, NEPS), writes=["epsc"])
        base_off = A.off

        def phase_mlp(l, final):
            A.reset(base_off)
            hb = [A.alloc([128, KC, 512], F32) for _ in range(2)]
            xn = A.alloc([128, KC, 512], BF16)
            h1 = A.alloc([128, 32, 512], BF16)
            rstd = A.alloc([128, 512], F32)
            rl = [A.alloc([128, 512], F32) for _ in range(2)]
            NWU = 4
            wu = [A.alloc([128, KC, 512], BF16) for _ in range(NWU)]
            NWD = 4
            wd = [A.alloc([128, 8, 512], BF16) for _ in range(NWD)]
            if final:
                yo = [A.alloc([128, 4, D], F32) for _ in range(1)]
                yT = A.alloc([128, KC, 512], F32)
            g_ffn = vec[:, V_GFFN + l * KC: V_GFFN + (l + 1) * KC]
            g_fin = vec[:, V_GFIN: V_GFIN + KC]
            nu = 0
            nd = 0
            for b in range(NB):
                h = hb[b % 2]
                hkey = ("mh", b % 2)
                P.add("sp", lambda e, h=h, b=b: e.dma_start(
                    out=h, in_=hT_s[:, :, b * 512:(b + 1) * 512].rearrange("k p s -> p k s")),
                    reads=[("hT", b)], writes=[hkey + (kc,) for kc in range(KC)],
                    dma_sem="mh%d" % (b % 2))
                sq = h1[:, 0:KC, :]
                rms_rstd(h, hkey, sq, ("h1",), rstd, "rstd", 7)
                for kc in range(KC):
                    P.add("dve", lambda e, h=h, kc=kc: e.scalar_tensor_tensor(
                        out=xn[:, kc, :], in0=h[:, kc, :], scalar=g_ffn[:, kc:kc + 1], in1=rstd,
                        op0=ALU.mult, op1=ALU.mult),
                        reads=[hkey + (kc,), "rstd", "vec"], writes=[("xn", kc)])
                for eg in range(8):
                    w = wu[nu % NWU]
                    wkey = ("wu", nu % NWU)
                    P.add("sp", lambda e, w=w, eg=eg: e.dma_start(
                        out=w, in_=up_s[l][:, eg * 512:(eg + 1) * 512].rearrange("(k p) e -> p k e", p=128)),
                        reads=wkeys["up%d" % l], writes=[wkey], dma_sem="wu%d" % (nu % NWU))
                    nu += 1
                    for j in range(4):
                        et = eg * 4 + j
                        pb = et % 4
                        for kc in range(KC):
                            P.add("pe", lambda e, w=w, j=j, kc=kc, pb=pb: e.matmul(
                                psum[pb], lhsT=w[:, kc, j * 128:(j + 1) * 128], rhs=xn[:, kc, :],
                                start=(kc == 0), stop=(kc == KC - 1)),
                                reads=[wkey, ("xn", kc)], writes=[("ps", pb)])
                        r = rl[et % 2]
                        P.add("act", lambda e, r=r, pb=pb: e.activation(out=r, in_=psum[pb], func=AF.Relu),
                              reads=[("ps", pb)], writes=[("rl", et % 2)])
                        P.add("dve", lambda e, r=r, pb=pb, et=et: e.tensor_tensor(
                            out=h1[:, et, :], in0=r, in1=psum[pb], op=ALU.mult),
                            reads=[("ps", pb), ("rl", et % 2)], writes=[("h1", et)])
                for fg in range(2):
                    for e4 in range(4):
                        w = wd[nd % NWD]
                        wkey = ("wd", nd % NWD)
                        P.add("sp", lambda e, w=w, fg=fg, e4=e4: e.dma_start(
                            out=w, in_=down_s[l][e4 * 1024:(e4 + 1) * 1024, fg * 512:(fg + 1) * 512]
                            .rearrange("(k p) f -> p k f", p=128)),
                            reads=wkeys["down%d" % l], writes=[wkey], dma_sem="wd%d" % (nd % NWD))
                        nd += 1
                        for fj in range(4):
                            pb = 4 + fj
                            for ek in range(8):
                                et = e4 * 8 + ek
                                P.add("pe", lambda e, w=w, fj=fj, ek=ek, et=et, pb=pb, e4=e4: e.matmul(
                                    psum[pb], lhsT=w[:, ek, fj * 128:(fj + 1) * 128], rhs=h1[:, et, :],
                                    start=(e4 == 0 and ek == 0), stop=(e4 == 3 and ek == 7)),
                                    reads=[wkey, ("h1", et)], writes=[("ps", pb)])
                    for fj in range(4):
                        f = fg * 4 + fj
                        pb = 4 + fj
                        P.add("dve", lambda e, h=h, f=f, pb=pb: e.tensor_tensor(
                            out=h[:, f, :], in0=h[:, f, :], in1=psum[pb], op=ALU.add),
                            reads=[("ps", pb), hkey + (f,)], writes=[hkey + (f,)])
                if not final:
                    P.add("sp", lambda e, h=h, b=b: e.dma_start(
                        out=hT_s[:, :, b * 512:(b + 1) * 512].rearrange("k p s -> p k s"), in_=h),
                        reads=[hkey + (kc,) for kc in range(KC)], writes=[("hT", b)],
                        dma_sem="mhw%d" % (b % 2))
                else:
                    final_out(h, hkey, b, h1[:, 0:KC, :], ("h1",), rstd, yT, yo[0])

        def final_out(h, hkey, b, sq2, sqkey, rstd, yT, y):
            g_fin = vec[:, V_GFIN: V_GFIN + KC]
            rms_rstd(h, hkey, sq2, sqkey, rstd, "rstd", 7)
            for kc in range(KC):
                P.add("dve", lambda e, h=h, kc=kc: e.scalar_tensor_tensor(
                    out=yT[:, kc, :], in0=h[:, kc, :], scalar=g_fin[:, kc:kc + 1], in1=rstd,
                    op0=ALU.mult, op1=ALU.mult),
                    reads=[hkey + (kc,), "rstd", "vec"], writes=[("yT", kc)])
            for j in range(4):
                for half in range(2):
                    pb = (j * 2 + half) % 4
                    for q in range(4):
                        kc = half * 4 + q
                        P.add("pe", lambda e, pb=pb, q=q, kc=kc, j=j: e.transpose(
                            out=psum[pb][:, q * 128:(q + 1) * 128],
                            in_=yT[:, kc, j * 128:(j + 1) * 128], identity=ident_f),
                            reads=[("yT", kc), "cst"], writes=[("ps", pb)])
                    if half == 0:
                        P.add("act", lambda e, y=y, j=j, pb=pb: e.copy(
                            out=y[:, j, 0:512], in_=psum[pb]),
                            reads=[("ps", pb)], writes=[("yo", j, 0)])
                    else:
                        P.add("dve", lambda e, y=y, j=j, pb=pb: e.tensor_copy(
                            out=y[:, j, 512:1024], in_=psum[pb]),
                            reads=[("ps", pb)], writes=[("yo", j, 1)])
            P.add("sp", lambda e, y=y, b=b: e.dma_start(
                out=out_d[b * 512:(b + 1) * 512, :].rearrange("(j p) d -> p j d", p=128), in_=y),
                reads=[("yo", j, hf) for j in range(4) for hf in range(2)],
                writes=[("out", b)], dma_sem="out")

        def phase_final():
            A.reset(base_off)
            hb = [A.alloc([128, KC, 512], F32) for _ in range(2)]
            sq = A.alloc([128, KC, 512], BF16)
            rstd = A.alloc([128, 512], F32)
            yT = A.alloc([128, KC, 512], F32)
            y = A.alloc([128, 4, D], F32)
            for b in range(NB):
                h = hb[b % 2]
                hkey = ("fh", b % 2)
                P.add("sp", lambda e, h=h, b=b: e.dma_start(
                    out=h, in_=hT_s[:, :, b * 512:(b + 1) * 512].rearrange("k p s -> p k s")),
                    reads=[("hT", b)], writes=[hkey + (kc,) for kc in range(KC)],
                    dma_sem="fh%d" % (b % 2))
                final_out(h, hkey, b, sq, ("fsq",), rstd, yT, y)


        class WStream:
            def __init__(self, name, nslots, shape):
                self.name = name
                self.slots = [A.alloc(shape, BF16) for _ in range(nslots)]
                self.n = 0

            def load(self, src, srckeys, view=None):
                i = self.n % len(self.slots)
                self.n += 1
                w = self.slots[i]
                dst = w if view is None else view(w)
                key = (self.name, i)
                P.add("sp", lambda e: e.dma_start(out=dst, in_=src), reads=srckeys, writes=[key],
                      dma_sem="%s%d" % (self.name, i))
                return w, key

        def load_norm(b, h, hkey, sq, sqkey, rstd, hn, gcols, sem):
            P.add("sp", lambda e: e.dma_start(
                out=h, in_=hT_s[:, :, b * 512:(b + 1) * 512].rearrange("k p s -> p k s")),
                reads=[("hT", b)], writes=[hkey + (kc,) for kc in range(KC)], dma_sem=sem)
            rms_rstd(h, hkey, sq, sqkey, rstd, "rstd", 7)
            for kc in range(KC):
                P.add("dve", lambda e, kc=kc: e.scalar_tensor_tensor(
                    out=hn[:, kc, :], in0=h[:, kc, :], scalar=gcols[:, kc:kc + 1], in1=rstd,
                    op0=ALU.mult, op1=ALU.mult),
                    reads=[hkey + (kc,), "rstd", "vec"], writes=[("hn", kc)])

        def store_h(b, h, hkey, sem):
            P.add("sp", lambda e: e.dma_start(
                out=hT_s[:, :, b * 512:(b + 1) * 512].rearrange("k p s -> p k s"), in_=h),
                reads=[hkey + (kc,) for kc in range(KC)], writes=[("hT", b)], dma_sem=sem)

        def out_proj(wo, wokey, zT, zkey, h, hkey):
            for f in range(KC):
                pb = 4 + (f % 2)
                for kc in range(KC):
                    P.add("pe", lambda e, f=f, kc=kc, pb=pb: e.matmul(
                        psum[pb], lhsT=wo[:, kc, f * 128:(f + 1) * 128], rhs=zT[:, kc, :],
                        start=(kc == 0), stop=(kc == KC - 1)),
                        reads=[wokey, (zkey, kc)], writes=[("ps", pb)])
                P.add("dve", lambda e, f=f, pb=pb: e.tensor_tensor(
                    out=h[:, f, :], in0=h[:, f, :], in1=psum[pb], op=ALU.add),
                    reads=[("ps", pb), hkey + (f,)], writes=[hkey + (f,)])

        def phase_gla():
            A.reset(base_off)
            l = 1
            g_mix = vec[:, V_GMIX + l * KC: V_GMIX + (l + 1) * KC]
            bgk = vec[:, V_BGK:V_BGK + 4]
            mask_u = cst[:, C_MU:C_MU + 64]
            rmask = cst[:, C_RM:C_RM + 512]
            gn_rep = cst[:, C_GN:C_GN + 256]
            hb = [A.alloc([128, KC, 512], F32) for _ in range(2)]
            hn = A.alloc([128, KC, 512], BF16)
            sq = A.alloc([128, KC, 512], BF16)
            zT = sq
            rstd = A.alloc([128, 512], F32)
            wt = WStream("gw", 3, [128, KC, 512])
            wo = A.alloc([128, KC, D], BF16)
            wgl = A.alloc([128, KC, 16], BF16)
            wgk2 = small_w["wgk2"]
            gl = A.alloc([16, 512], BF16)
            gkp = A.alloc([128, 4, 512], F32)
            cum = A.alloc([128, 4, 512], F32)
            et = [A.alloc([128, 512], F32) for _ in range(2)]
            qtT = A.alloc([128, 4, 512], BF16)
            ktT = A.alloc([128, 4, 512], BF16)
            khT = A.alloc([128, 4, 512], BF16)
            kh = A.alloc([128, 4, 512], BF16)
            V = A.alloc([128, 4, D], BF16)
            sog = A.alloc([128, 4, D], F32)
            scT = A.alloc([128, 256], BF16)
            S32 = A.alloc([128, 4, 256], F32)
            Sbf = A.alloc([128, 4, 256], BF16)
            elast = A.alloc([128, 4, 8], F32)
            t1 = [A.alloc([128, 256], F32) for _ in range(2)]
            zb = [A.alloc([128, D], BF16) for _ in range(2)]
            ssq = A.alloc([128, 4], F32)
            rinv = A.alloc([128, 4], F32)
            junk = A.alloc([128, 256], BF16)
            ps_bf6 = psum[6].bitcast(BF16)

            P.add("sp", lambda e: e.dma_start(out=wo, in_=gwo_s.rearrange("(k p) f -> p k f", p=128)),
                  reads=wkeys["gwo"], writes=["gwo_sb"], dma_sem="gwo_sb")
            P.add("sp", lambda e: e.dma_start(
                out=wgl, in_=win_s[:, 3072:3088].rearrange("(k p) f -> p k f", p=128)),
                reads=wkeys["win"], writes=["wgl"], dma_sem="wgl")
            P.add("dve", lambda e: e.memset(S32, 0.0), writes=[("S32", hh) for hh in range(4)])
            P.add("dve", lambda e: e.memset(Sbf, 0.0), writes=[("Sbf", hh) for hh in range(4)])

            def wcols(c0):
                return win_s[:, c0:c0 + 512].rearrange("(k p) f -> p k f", p=128)

            for b in range(NB):
                h = hb[b % 2]
                hkey = ("gh", b % 2)
                load_norm(b, h, hkey, sq, ("sq",), rstd, hn, g_mix, "gh%d" % (b % 2))
                for kc in range(KC):
                    P.add("pe", lambda e, kc=kc: e.matmul(psum[4][0:16, :], lhsT=wgl[:, kc, :], rhs=hn[:, kc, :],
                                                          start=(kc == 0), stop=(kc == KC - 1)),
                          reads=["wgl", ("hn", kc)], writes=[("ps", 4)])
                P.add("act", lambda e: e.copy(out=gl, in_=psum[4][0:16, :]), reads=[("ps", 4)], writes=["gl"])
                for hh in range(4):
                    pb = 4 + (hh + 1) % 2
                    P.add("pe", lambda e, hh=hh, pb=pb: e.matmul(
                        psum[pb], lhsT=wgk2[:, hh * 128:(hh + 1) * 128], rhs=gl, start=True, stop=True),
                        reads=["wgk2", "gl"], writes=[("ps", pb)], rt=0)
                    P.add("act", lambda e, hh=hh, pb=pb: e.activation(
                        out=gkp[:, hh, :], in_=psum[pb], func=AF.Sigmoid, bias=bgk[:, hh:hh + 1]),
                        reads=[("ps", pb), "vec"], writes=[("gkp", hh)])
                for hh in range(4):
                    P.add("act", lambda e, hh=hh: e.activation(out=gkp[:, hh, :], in_=gkp[:, hh, :], func=AF.Ln),
                          reads=[("gkp", hh)], writes=[("gkp", hh)])
                    P.add("dve", lambda e, hh=hh: e.tensor_tensor_scan(
                        out=cum[:, hh, :], data0=rmask, data1=gkp[:, hh, :], initial=0.0,
                        op0=ALU.mult, op1=ALU.add),
                        reads=[("gkp", hh), "cst"], writes=[("cum", hh)])
                P.add("act", lambda e: e.activation(
                    out=elast, in_=cum.rearrange("p h (c t) -> p h c t", t=64)[:, :, :, 63],
                    func=AF.Exp, scale=1.0 / 16.0),
                    reads=[("cum", hh) for hh in range(4)], writes=["elast"])
                wq, wqk = wt.load(wcols(0), wkeys["win"])
                for hh in range(4):
                    pb = 4 + hh % 2
                    e_ = et[hh % 2]
                    for kc in range(KC):
                        P.add("pe", lambda e, hh=hh, kc=kc, pb=pb: e.matmul(
                            psum[pb], lhsT=wq[:, kc, hh * 128:(hh + 1) * 128], rhs=hn[:, kc, :],
                            start=(kc == 0), stop=(kc == KC - 1)),
                            reads=[wqk, ("hn", kc)], writes=[("ps", pb)])
                    P.add("act", lambda e, hh=hh, e_=e_: e.activation(
                        out=e_, in_=cum[:, hh, :], func=AF.Exp, scale=1.0 / 16.0),
                        reads=[("cum", hh)], writes=[("et", hh % 2)])
                    P.add("dve", lambda e, hh=hh, e_=e_, pb=pb: e.scalar_tensor_tensor(
                        out=qtT[:, hh, :], in0=psum[pb], scalar=128.0 ** -0.5, in1=e_,
                        op0=ALU.mult, op1=ALU.mult),
                        reads=[("ps", pb), ("et", hh % 2)], writes=[("qtT", hh)])
                wk, wkk = wt.load(wcols(512), wkeys["win"])
                for hh in range(4):
                    pb = 4 + hh % 2
                    for kc in range(KC):
                        P.add("pe", lambda e, hh=hh, kc=kc, pb=pb: e.matmul(
                            psum[pb], lhsT=wk[:, kc, hh * 128:(hh + 1) * 128], rhs=hn[:, kc, :],
                            start=(kc == 0), stop=(kc == KC - 1)),
                            reads=[wkk, ("hn", kc)], writes=[("ps", pb)])
                    P.add("act", lambda e, hh=hh: e.activation(
                        out=et[0], in_=cum[:, hh, :], func=AF.Exp, scale=-1.0 / 16.0),
                        reads=[("cum", hh)], writes=[("et", 0)])
                    P.add("dve", lambda e, hh=hh, pb=pb: e.tensor_tensor(
                        out=ktT[:, hh, :], in0=psum[pb], in1=et[0], op=ALU.mult),
                        reads=[("ps", pb), ("et", 0)], writes=[("ktT", hh)])
                    cv = cum[:, hh, :].rearrange("p (c t) -> p c t", t=64)
                    P.add("dve", lambda e, hh=hh, cv=cv: e.tensor_tensor(
                        out=et[1].rearrange("p (c t) -> p c t", t=64),
                        in0=cv[:, :, 63:64].to_broadcast([128, 8, 64]), in1=cv, op=ALU.subtract),
                        reads=[("cum", hh)], writes=[("et", 1)])
                    P.add("act", lambda e: e.activation(out=et[1], in_=et[1], func=AF.Exp, scale=1.0 / 16.0),
                          reads=[("et", 1)], writes=[("et", 1)])
                    P.add("dve", lambda e, hh=hh, pb=pb: e.tensor_tensor(
                        out=khT[:, hh, :], in0=psum[pb], in1=et[1], op=ALU.mult),
                        reads=[("ps", pb), ("et", 1)], writes=[("khT", hh)])
                for j in range(4):
                    for hh in range(4):
                        P.add("pe", lambda e, j=j, hh=hh: e.transpose(
                            out=ps_bf6[:, hh * 128:(hh + 1) * 128], in_=khT[:, hh, j * 128:(j + 1) * 128],
                            identity=ident_b),
                            reads=[("khT", hh), "ident_b"], writes=[("ps", 6)])
                    P.add("act", lambda e, j=j: e.copy(out=kh[:, j, :], in_=ps_bf6[:, 0:512]),
                          reads=[("ps", 6)], writes=[("kh", j)])
                for half in range(2):
                    wv, wvk = wt.load(wcols(1024 + half * 512), wkeys["win"])
                    for j in range(4):
                        pb = 4 + j % 2
                        for kc in range(KC):
                            P.add("pe", lambda e, j=j, kc=kc, pb=pb, wv=wv: e.matmul(
                                psum[pb], lhsT=hn[:, kc, j * 128:(j + 1) * 128], rhs=wv[:, kc, :],
                                start=(kc == 0), stop=(kc == KC - 1)),
                                reads=[wvk, ("hn", kc)], writes=[("ps", pb)])
                        P.add("act", lambda e, j=j, pb=pb, half=half: e.copy(
                            out=V[:, j, half * 512:(half + 1) * 512], in_=psum[pb]),
                            reads=[("ps", pb)], writes=[("V", j, half)])
                for half in range(2):
                    wg, wgk = wt.load(wcols(2048 + half * 512), wkeys["win"])
                    for j in range(4):
                        pb = 4 + j % 2
                        for kc in range(KC):
                            P.add("pe", lambda e, j=j, kc=kc, pb=pb, wg=wg: e.matmul(
                                psum[pb], lhsT=hn[:, kc, j * 128:(j + 1) * 128], rhs=wg[:, kc, :],
                                start=(kc == 0), stop=(kc == KC - 1)),
                                reads=[wgk, ("hn", kc)], writes=[("ps", pb)])
                        P.add("act", lambda e, j=j, pb=pb, half=half: e.activation(
                            out=sog[:, j, half * 512:(half + 1) * 512], in_=psum[pb], func=AF.Silu),
                            reads=[("ps", pb)], writes=[("sog", j, half)])
                for j in range(4):
                    for cc in range(2):
                        c = j * 2 + cc
                        p0 = cc * 64
                        t0 = j * 128 + cc * 64
                        for hh in range(4):
                            P.add("pe", lambda e, hh=hh, p0=p0, t0=t0: e.matmul(
                                psum[2][p0:p0 + 64, hh * 64:(hh + 1) * 64],
                                lhsT=ktT[:, hh, t0:t0 + 64], rhs=qtT[:, hh, t0:t0 + 64], start=True, stop=True),
                                reads=[("ktT", hh), ("qtT", hh)], writes=[("ps", 2)])
                        P.add("dve", lambda e, p0=p0: e.tensor_tensor(
                            out=scT[p0:p0 + 64, :].rearrange("p (h t) -> p h t", h=4),
                            in0=psum[2][p0:p0 + 64, 0:256].rearrange("p (h t) -> p h t", h=4),
                            in1=mask_u[p0:p0 + 64, :].unsqueeze(1).to_broadcast([64, 4, 64]), op=ALU.mult),
                            reads=[("ps", 2), "cst"], writes=[("scT", cc)])
                        for hh in range(4):
                            ob = hh // 2
                            oc = (hh % 2) * 256
                            P.add("pe", lambda e, hh=hh, p0=p0, ob=ob, oc=oc, j=j: e.matmul(
                                psum[ob][p0:p0 + 64, oc:oc + 256], lhsT=scT[p0:p0 + 64, hh * 64:(hh + 1) * 64],
                                rhs=V[p0:p0 + 64, j, hh * 256:(hh + 1) * 256], start=True, stop=False),
                                reads=[("scT", cc), ("V", j, hh // 2)], writes=[("ps", ob)], rt=p0)
                            P.add("pe", lambda e, hh=hh, p0=p0, ob=ob, oc=oc, t0=t0: e.matmul(
                                psum[ob][p0:p0 + 64, oc:oc + 256], lhsT=qtT[:, hh, t0:t0 + 64],
                                rhs=Sbf[:, hh, :], start=False, stop=True),
                                reads=[("qtT", hh), ("Sbf", hh)], writes=[("ps", ob)])
                        for hh in range(4):
                            gb = 3 if hh % 2 == 0 else 7
                            P.add("pe", lambda e, hh=hh, p0=p0, gb=gb, j=j: e.matmul(
                                psum[gb][:, 0:256], lhsT=kh[p0:p0 + 64, j, hh * 128:(hh + 1) * 128],
                                rhs=V[p0:p0 + 64, j, hh * 256:(hh + 1) * 256], start=True, stop=True),
                                reads=[("kh", j), ("V", j, hh // 2)], writes=[("ps", gb)], rt=p0)
                            P.add("dve", lambda e, hh=hh, gb=gb, c=c: e.scalar_tensor_tensor(
                                out=S32[:, hh, :], in0=S32[:, hh, :], scalar=elast[:, hh, c:c + 1],
                                in1=psum[gb][:, 0:256], op0=ALU.mult, op1=ALU.add),
                                reads=[("ps", gb), ("S32", hh), "elast"], writes=[("S32", hh)])
                            P.add("act", lambda e, hh=hh: e.copy(out=Sbf[:, hh, :], in_=S32[:, hh, :]),
                                  reads=[("S32", hh)], writes=[("Sbf", hh)])
                    z = zb[j % 2]
                    for hh in range(4):
                        ob = hh // 2
                        oc = (hh % 2) * 256
                        P.add("act", lambda e, hh=hh, ob=ob, oc=oc: e.activation(
                            out=junk, in_=psum[ob][:, oc:oc + 256], func=AF.Square, accum_out=ssq[:, hh:hh + 1]),
                            reads=[("ps", ob)], writes=["junk", ("ssq", hh)])
                    P.add("dve", lambda e: e.tensor_scalar(out=rinv, in0=ssq, scalar1=1.0 / 256.0, scalar2=NEPS,
                                                           op0=ALU.mult, op1=ALU.add),
                          reads=[("ssq", hh) for hh in range(4)], writes=["rinv"])
                    P.add("act", lambda e: e.activation(out=rinv, in_=rinv, func=AF.Sqrt),
                          reads=["rinv"], writes=["rinv"])
                    P.add("dve", lambda e: e.reciprocal(out=rinv, in_=rinv), reads=["rinv"], writes=["rinv"])
                    for hh in range(4):
                        ob = hh // 2
                        oc = (hh % 2) * 256
                        tt = t1[hh % 2]
                        P.add("dve", lambda e, hh=hh, ob=ob, oc=oc, tt=tt: e.scalar_tensor_tensor(
                            out=tt, in0=psum[ob][:, oc:oc + 256], scalar=rinv[:, hh:hh + 1], in1=gn_rep,
                            op0=ALU.mult, op1=ALU.mult),
                            reads=[("ps", ob), "rinv", "cst"], writes=[("t1", hh % 2)])
                        P.add("dve", lambda e, hh=hh, tt=tt, z=z, j=j: e.tensor_tensor(
                            out=z[:, hh * 256:(hh + 1) * 256], in0=tt, in1=sog[:, j, hh * 256:(hh + 1) * 256],
                            op=ALU.mult),
                            reads=[("t1", hh % 2), ("sog", j, hh // 2)], writes=[("zb", j % 2, hh)])
                    for kc in range(KC):
                        P.add("pe", lambda e, kc=kc, z=z: e.transpose(
                            out=ps_bf6[:, kc * 128:(kc + 1) * 128], in_=z[:, kc * 128:(kc + 1) * 128],
                            identity=ident_b),
                            reads=[("zb", j % 2, kc // 2), "ident_b"], writes=[("ps", 6)])
                    P.add("act", lambda e, j=j: e.copy(
                        out=zT[:, :, j * 128:(j + 1) * 128],
                        in_=ps_bf6.rearrange("p (k t) -> p k t", k=KC)),
                        reads=[("ps", 6)], writes=[("sq", kc) for kc in range(KC)])
                out_proj(wo, "gwo_sb", zT, "sq", h, hkey)
                store_h(b, h, hkey, "ghw%d" % (b % 2))


        def MM(out, lhsT, rhs, reads, writes, start=True, stop=True, rt=None):
            cls = None if lhsT.partition_size == 128 else lhsT.start_partition
            return P.add("pe", lambda e: e.matmul(out, lhsT=lhsT, rhs=rhs, start=start, stop=stop),
                         reads=reads, writes=writes, rt=cls)

        def TT(out, in0, in1, op, reads, writes):
            P.add("dve", lambda e: e.tensor_tensor(out=out, in0=in0, in1=in1, op=op), reads=reads, writes=writes)

        def STT(out, in0, scalar, in1, op0, op1, reads, writes):
            P.add("dve", lambda e: e.scalar_tensor_tensor(out=out, in0=in0, scalar=scalar, in1=in1,
                                                          op0=op0, op1=op1), reads=reads, writes=writes)

        def TS(out, in0, s1, s2, op0, op1, reads, writes):
            P.add("dve", lambda e: e.tensor_scalar(out=out, in0=in0, scalar1=s1, scalar2=s2, op0=op0, op1=op1),
                  reads=reads, writes=writes)

        def ACT(out, in_, func, reads, writes, **kw):
            P.add("act", lambda e: e.activation(out=out, in_=in_, func=func, **kw), reads=reads, writes=writes)

        def ACOPY(out, in_, reads, writes):
            P.add("act", lambda e: e.copy(out=out, in_=in_), reads=reads, writes=writes)

        def phase_rwkv():
            A.reset(base_off)
            HORDER = [h for i in range(8) for h in (i, i + 8)]
            T = 256
            NBR = S // T
            C0 = float(np.exp(-0.5))
            g_mix = vec[:, V_GMIX: V_GMIX + KC]
            mu = [vec[:, V_MU + i * KC: V_MU + (i + 1) * KC] for i in range(6)]
            w0c = vec[:, V_W0:V_W0 + KC]
            a0c = vec[:, V_A0:V_A0 + KC]
            kkc = vec[:, V_KK:V_KK + KC]
            kac = vec[:, V_KA:V_KA + KC]
            rkc = vec[:, V_RK:V_RK + KC]
            m_sl = cst[:, C_MSL:C_MSL + 64]
            m_su = cst[:, C_MSU:C_MSU + 64]
            m_u = cst[:, C_MU:C_MU + 64]
            ist = cst[:, C_IST:C_IST + 64]
            rmask = cst[:, C_RM:C_RM + T]

            def bc16(m):
                return m.unsqueeze(1).to_broadcast([128, 16, 64])

            rep = A.alloc([128, 2 * D], F32)
            lnw_rep = rep[:, 0:D]
            lnb_rep = rep[:, D:2 * D]
            omka = A.alloc([128, KC], F32)
            gneps = A.alloc([128, 1], F32)
            bones = A.alloc([128, 128], BF16)
            hsel = A.alloc([128, 2], BF16)
            w1, a1, g1, w2, a2, g2a, g2b = (small_w[n_] for n_ in ("w1", "a1", "g1", "w2", "a2", "g2a", "g2b"))
            P.add("sp", lambda e: e.dma_start(out=rep, in_=dt["rep"]), writes=["rep"], dma_sem="rep")
            TS(omka, kac, -1.0, 1.0, ALU.mult, ALU.add, ["vec"], ["omka"])
            P.add("dve", lambda e: e.memset(gneps, 64e-5), writes=["gneps"])
            P.add("dve", lambda e: e.tensor_copy(out=bones, in_=cst[:, C_BONES:C_BONES + 128]),
                  reads=["cst"], writes=["bones"])
            P.add("dve", lambda e: e.tensor_copy(out=hsel, in_=cst[:, C_HSEL:C_HSEL + 2]),
                  reads=["cst"], writes=["hsel"])

            hx = A.alloc([128, KC, T + 1], F32)
            sq = A.alloc([128, KC, T], BF16)
            zT = sq
            rstd = A.alloc([128, T], F32)
            xx = A.alloc([128, KC, T], F32)
            xm = [A.alloc([128, KC, T], BF16) for _ in range(2)]
            wt = WStream("rw", 3, [128, KC, 512])
            twT = A.alloc([64, T], BF16)
            taT = A.alloc([64, T], BF16)
            sgA = A.alloc([128, T], BF16)
            sgB = A.alloc([32, T], BF16)
            tn = ["s", "al", "cum", "kk", "rn", "kkn", "tmp", "kmod", "bs", "cp", "Em", "Ep", "Epv", "El"]
            tb = {n: A.alloc([128, T], F32) for n in tn}
            kk2 = A.alloc([128, T], BF16)
            ATf = A.alloc([128, KC, T], BF16)
            BTf = A.alloc([128, KC, T], BF16)
            KTf = A.alloc([128, KC, T], BF16)
            RTf = A.alloc([128, KC, T], BF16)
            BhTf = A.alloc([128, KC, T], BF16)
            KhTf = A.alloc([128, KC, T], BF16)
            rkT = [A.alloc([128, T], BF16) for _ in range(2)]
            Atm = A.alloc([128, 2, D], BF16)
            Bh = A.alloc([128, 2, D], BF16)
            Kh = A.alloc([128, 2, D], BF16)
            V = A.alloc([128, 2, D], BF16)
            g32 = A.alloc([128, 2, D], F32)
            rks = A.alloc([128, 2, 16], F32)
            wc = A.alloc([128, KC, 4], F32)
            cl = {n: A.alloc([128, D], BF16) for n in
                  ["Lm", "LTm", "AkT", "RbT", "RkT", "P0", "PT0", "P1", "PT1", "TT0", "TT1", "Xv", "Uv", "Ah",
                   "MT", "RhT"]}
            G32 = A.alloc([128, D], F32)
            H32 = A.alloc([128, KC, 64], F32)
            Hbf = A.alloc([128, KC, 64], BF16)
            y32 = A.alloc([128, D], F32)
            ysq = A.alloc([128, D], F32)
            yt = A.alloc([128, D], F32)
            zb = A.alloc([128, D], BF16)
            st = {n: A.alloc([128, 16], F32) for n in ["s1", "s2", "mean", "msq", "var"]}
            ps_bf6 = psum[6].bitcast(BF16)
            pp = [psum2[0], psum2[1], psum2[2]]

            P.add("dve", lambda e: e.memset(H32, 0.0), writes=["H32"])
            P.add("dve", lambda e: e.memset(Hbf, 0.0), writes=["Hbf"])
            P.add("dve", lambda e: e.memset(hx[:, :, 0:1], 0.0), writes=["hxp"])
            hxk = [("hx", kc) for kc in range(KC)]
            xxk = [("xx", kc) for kc in range(KC)]

            def wtile(wi, q):
                return wt.load(wrkv_s[wi][:, q * 512:(q + 1) * 512].rearrange("(k p) f -> p k f", p=128),
                               wkeys["wrkv%d" % wi])

            for b in range(NBR):
              try:
                if b > 0:
                    P.add("dve", lambda e: e.tensor_copy(out=hx[:, :, 0:1], in_=hx[:, :, T:T + 1]),
                          reads=hxk, writes=["hxp"])
                P.add("sp", lambda e, b=b: e.dma_start(
                    out=hx[:, :, 1:T + 1], in_=hT_s[:, :, b * T:(b + 1) * T].rearrange("k p s -> p k s")),
                    reads=[("hT", b)] + (["hxp"] if b > 0 else []), writes=hxk, dma_sem="rhx")
                hb_ = hx[:, :, 1:T + 1]
                for kc in range(KC):
                    ACT(sq[:, kc, :], hb_[:, kc, :], AF.Square, [("hx", kc)], [("sq", kc)])
                for kc in range(KC):
                    MM(psum[7][:, 0:T], ones_bf, sq[:, kc, :], [("sq", kc), "ones_bf"], [("ps", 7)],
                       start=(kc == 0), stop=(kc == KC - 1))
                ACT(rstd, psum[7][:, 0:T], AF.Sqrt, [("ps", 7), "epsc"], ["rstd"], scale=1.0 / D, bias=eps_col)
                P.add("dve", lambda e: e.reciprocal(out=rstd, in_=rstd), reads=["rstd"], writes=["rstd"])
                for kc in range(KC):
                    STT(hb_[:, kc, :], hb_[:, kc, :], g_mix[:, kc:kc + 1], rstd, ALU.mult, ALU.mult,
                        [("hx", kc), "rstd", "vec"], [("hx", kc)])
                for kc in range(KC):
                    TT(xx[:, kc, :], hx[:, kc, 0:T], hb_[:, kc, :], ALU.subtract,
                       [("hx", kc), "hxp"], [("xx", kc)])

                def mix(i, slot):
                    for kc in range(KC):
                        STT(xm[slot][:, kc, :], xx[:, kc, :], mu[i][:, kc:kc + 1], hb_[:, kc, :], ALU.mult, ALU.add,
                            [("xx", kc), ("hx", kc), "vec"], [("xm", slot, kc)])

                def proj_fm(out_ps, M, wfn, slot, wkey):
                    for kc in range(KC):
                        MM(out_ps, wfn(kc), xm[slot][:, kc, :], [wkey, ("xm", slot, kc)], [("ps", out_ps_id[0])],
                           start=(kc == 0), stop=(kc == KC - 1))

                out_ps_id = [0]
                chk('A0')
                mix(3, 0)
                out_ps_id[0] = 0
                proj_fm(psum[0][0:64, 0:T], 64, lambda kc: w1[:, kc, :], 0, "w1")
                ACT(twT, psum[0][0:64, 0:T], AF.Tanh, [("ps", 0)], ["twT"])
                mix(4, 1)
                out_ps_id[0] = 1
                proj_fm(psum[1][0:64, 0:T], 64, lambda kc: a1[:, kc, :], 1, "a1")
                ACOPY(taT, psum[1][0:64, 0:T], [("ps", 1)], ["taT"])
                mix(5, 0)
                out_ps_id[0] = 2
                proj_fm(psum[2][:, 0:T], 128, lambda kc: g1[:, kc, 0:128], 0, "g1")
                ACT(sgA, psum[2][:, 0:T], AF.Sigmoid, [("ps", 2)], ["sgA"])
                out_ps_id[0] = 3
                proj_fm(psum[3][0:32, 0:T], 32, lambda kc: g1[:, kc, 128:160], 0, "g1")
                ACT(sgB, psum[3][0:32, 0:T], AF.Sigmoid, [("ps", 3)], ["sgB"])
                for j in range(2):
                    for half in range(2):
                        pb = (j * 2 + half) % 4
                        MM(psum[pb], sgA[:, j * 128:(j + 1) * 128], g2a[:, half * 512:(half + 1) * 512],
                           ["sgA", "g2a"], [("ps", pb)], start=True, stop=False)
                        MM(psum[pb], sgB[:, j * 128:(j + 1) * 128], g2b[:, half * 512:(half + 1) * 512],
                           ["sgB", "g2b"], [("ps", pb)], start=False, stop=True)
                        ACOPY(g32[:, j, half * 512:(half + 1) * 512], psum[pb], [("ps", pb)], [("g32", j)])
                mix(2, 1)
                for half in range(2):
                    wv, wvk = wtile(2, half)
                    for j in range(2):
                        pb = (j * 2 + half) % 4
                        for kc in range(KC):
                            MM(psum[pb], xm[1][:, kc, j * 128:(j + 1) * 128], wv[:, kc, :],
                               [wvk, ("xm", 1, kc)], [("ps", pb)], start=(kc == 0), stop=(kc == KC - 1))
                        ACOPY(V[:, j, half * 512:(half + 1) * 512], psum[pb], [("ps", pb)], [("V", j)])
                chk('A1')
                mix(0, 0)
                mix(1, 1)
                for p in range(KC):
                    if p % 4 == 0:
                        wr, wrk = wtile(0, p // 4)
                        wk_, wkk = wtile(1, p // 4)
                    c4 = (p % 4) * 128
                    for kc in range(KC):
                        MM(psum[0][:, 0:T], wr[:, kc, c4:c4 + 128], xm[0][:, kc, :], [wrk, ("xm", 0, kc)],
                           [("ps", 0)], start=(kc == 0), stop=(kc == KC - 1))
                    for kc in range(KC):
                        MM(psum[1][:, 0:T], wk_[:, kc, c4:c4 + 128], xm[1][:, kc, :], [wkk, ("xm", 1, kc)],
                           [("ps", 1)], start=(kc == 0), stop=(kc == KC - 1))
                    MM(psum[2][:, 0:T], w2[:, p * 128:(p + 1) * 128], twT, ["w2", "twT"], [("ps", 2)])
                    MM(psum[3][:, 0:T], a2[:, p * 128:(p + 1) * 128], taT, ["a2", "taT"], [("ps", 3)])
                    r_ps = psum[0][:, 0:T]
                    k_ps = psum[1][:, 0:T]
                    pc = slice(p, p + 1)
                    ACT(tb["s"], psum[2][:, 0:T], AF.Sigmoid, [("ps", 2), "vec"], ["t_s"], bias=w0c[:, pc])
                    ACT(tb["al"], psum[3][:, 0:T], AF.Sigmoid, [("ps", 3), "vec"], ["t_al"], bias=a0c[:, pc])
                    P.add("dve", lambda e: e.tensor_tensor_scan(
                        out=tb["cum"], data0=rmask, data1=tb["s"], initial=0.0, op0=ALU.mult, op1=ALU.add),
                        reads=["t_s", "cst"], writes=["t_cum"])
                    TS(tb["kk"], k_ps, kkc[:, pc], None, ALU.mult, ALU.bypass, [("ps", 1), "vec"], ["t_kk"])
                    ACT(kk2, tb["kk"], AF.Square, ["t_kk"], ["kk2"])
                    MM(psum[6][:, 0:T], bones, kk2, ["bones", "kk2"], [("ps", 6)])
                    ACT(tb["rn"], psum[6][:, 0:T], AF.Sqrt, [("ps", 6)], ["t_rn"])
                    TS(tb["rn"], tb["rn"], 1e-12, None, ALU.max, ALU.bypass, ["t_rn"], ["t_rn"])
                    P.add("dve", lambda e: e.reciprocal(out=tb["rn"], in_=tb["rn"]), reads=["t_rn"], writes=["t_rn"])
                    TT(tb["kkn"], tb["kk"], tb["rn"], ALU.mult, ["t_kk", "t_rn"], ["t_kkn"])
                    TS(tb["tmp"], tb["al"], kac[:, pc], omka[:, pc], ALU.mult, ALU.add,
                       ["t_al", "vec", "omka"], ["t_tmp"])
                    TT(tb["kmod"], k_ps, tb["tmp"], ALU.mult, [("ps", 1), "t_tmp"], ["t_kmod"])
                    TT(tb["bs"], tb["kkn"], tb["al"], ALU.mult, ["t_kkn", "t_al"], ["t_bs"])
                    ACT(tb["Em"], tb["cum"], AF.Exp, ["t_cum"], ["t_Em"], scale=C0)
                    ACT(tb["Ep"], tb["cum"], AF.Exp, ["t_cum"], ["t_Ep"], scale=-C0)
                    TT(tb["cp"], tb["cum"], tb["s"], ALU.subtract, ["t_cum", "t_s"], ["t_cp"])
                    ACT(tb["Epv"], tb["cp"], AF.Exp, ["t_cp"], ["t_Epv"], scale=-C0)
                    cv = tb["cum"].rearrange("p (c t) -> p c t", t=64)
                    TT(tb["cp"].rearrange("p (c t) -> p c t", t=64), cv,
                       cv[:, :, 63:64].to_broadcast([128, T // 64, 64]), ALU.subtract, ["t_cum", "t_Epv"], ["t_cp"])
                    ACT(tb["El"], tb["cp"], AF.Exp, ["t_cp"], ["t_El"], scale=C0)
                    STT(ATf[:, p, :], tb["kkn"], -1.0, tb["Epv"], ALU.mult, ALU.mult, ["t_kkn", "t_Epv"], [("ATf", p)])
                    TT(BTf[:, p, :], tb["bs"], tb["Em"], ALU.mult, ["t_bs", "t_Em"], [("BTf", p)])
                    TT(KTf[:, p, :], tb["kmod"], tb["Em"], ALU.mult, ["t_kmod", "t_Em"], [("KTf", p)])
                    TT(RTf[:, p, :], r_ps, tb["Ep"], ALU.mult, [("ps", 0), "t_Ep"], [("RTf", p)])
                    TT(BhTf[:, p, :], tb["bs"], tb["El"], ALU.mult, ["t_bs", "t_El"], [("BhTf", p)])
                    TT(KhTf[:, p, :], tb["kmod"], tb["El"], ALU.mult, ["t_kmod", "t_El"], [("KhTf", p)])
                    rk_ = rkT[p % 2]
                    STT(rk_, r_ps, rkc[:, pc], tb["kmod"], ALU.mult, ALU.mult, [("ps", 0), "t_kmod", "vec"],
                        [("rkT", p % 2)])
                    ACOPY(wc[:, p, :], tb["Ep"].rearrange("p (c t) -> p c t", t=64)[:, :, 63], ["t_Ep"], [("wc", p)])
                    for j in range(2):
                        MM(psum[7][:, j * 16 + p * 2: j * 16 + p * 2 + 2], rk_[:, j * 128:(j + 1) * 128], hsel,
                           [("rkT", p % 2), "hsel"], [("ps", 7)])
                ACOPY(rks, psum[7][:, 0:32].rearrange("p (j h) -> p j h", j=2), [("ps", 7)], ["rks"])
                chk('A2')
                for (srcf, skey, dstm, dkey) in ((ATf, "ATf", Atm, "Atm"), (BhTf, "BhTf", Bh, "Bh"), (KhTf, "KhTf", Kh, "Kh")):
                    for j in range(2):
                        for p in range(KC):
                            P.add("pe", lambda e, srcf=srcf, j=j, p=p: e.transpose(
                                out=ps_bf6[:, p * 128:(p + 1) * 128], in_=srcf[:, p, j * 128:(j + 1) * 128],
                                identity=ident_b), reads=[(skey, p), "ident_b"], writes=[("ps", 6)])
                        ACOPY(dstm[:, j, :], ps_bf6, [("ps", 6)], [(dkey, j)])

                chk('A3')
                for j in range(2):
                    rot = [0]

                    def nextpp():
                        k = rot[0] % 2
                        rot[0] += 1
                        return k

                    def layer(k, opfn, rd, feat_out=False, rtk="p0", ccs=(0, 1), kp=None):
                        jobs = [(cc, hh) for cc in ccs for hh in range(16)]
                        rtof = (lambda cc, hh: (hh % 2) * 64) if rtk == "hb" else (lambda cc, hh: cc * 64)
                        first = P.pe_cls if P.pe_cls in (0, 64) else 0
                        jobs.sort(key=lambda ch: (rtof(*ch) != first, ch[0], ch[1]))
                        wkeys_ = kp if kp is not None else [("ps", 2 * k), ("ps", 2 * k + 1)]
                        for cc, hh in jobs:
                            p_, hb2, p0 = hh // 2, (hh % 2) * 64, cc * 64
                            if feat_out:
                                o = pp[k][hb2:hb2 + 64, (cc * 8 + p_) * 64:(cc * 8 + p_ + 1) * 64]
                            else:
                                o = pp[k][p0:p0 + 64, hh * 64:(hh + 1) * 64]
                            ops = opfn(cc, hh)
                            for i, (l_, r_) in enumerate(ops):
                                MM(o, l_, r_, rd, wkeys_, start=(i == 0), stop=(i == len(ops) - 1),
                                   rt=rtof(cc, hh))

                    def tsl(buf, cc, hh):
                        return buf[cc * 64:cc * 64 + 64, hh * 64:(hh + 1) * 64]

                    def fsl(buf, cc, hh):
                        p_, hb2 = hh // 2, (hh % 2) * 64
                        return buf[hb2:hb2 + 64, (cc * 8 + p_) * 64:(cc * 8 + p_ + 1) * 64]

                    def fT(bufF, cc, hh):
                        p_, hb2 = hh // 2, (hh % 2) * 64
                        t0 = j * 128 + cc * 64
                        return bufF[hb2:hb2 + 64, p_, t0:t0 + 64]

                    def tM(bufM, cc, hh):
                        return bufM[cc * 64:cc * 64 + 64, j, hh * 64:(hh + 1) * 64]

                    def pk(k):
                        return [("ps", 2 * k), ("ps", 2 * k + 1)]

                    def ACOPY2(dst, k, wkey):
                        for hf in range(2):
                            ACOPY(dst[:, hf * 512:(hf + 1) * 512], pp[k][:, hf * 512:(hf + 1) * 512],
                                  [("ps", 2 * k + hf)], [wkey])

                    fkeys = lambda nm: [(nm, p_) for p_ in range(KC)]

                    def evac_mask(k, dst, mask):
                        for hf in range(2):
                            sl = slice(hf * 512, (hf + 1) * 512)
                            TT(cl[dst][:, sl].rearrange("p (h t) -> p h t", h=8),
                               pp[k][:, sl].rearrange("p (h t) -> p h t", h=8),
                               mask.unsqueeze(1).to_broadcast([128, 8, 64]), ALU.mult,
                               [("ps", 2 * k + hf), "cst"], [dst])

                    k = nextpp()
                    layer(k, lambda cc, hh: [(fT(ATf, cc, hh), fT(BTf, cc, hh))], fkeys("ATf") + fkeys("BTf"), rtk="hb")
                    evac_mask(k, "Lm", m_sl)
                    k = nextpp()
                    layer(k, lambda cc, hh: [(fT(BTf, cc, hh), fT(ATf, cc, hh))], fkeys("ATf") + fkeys("BTf"), rtk="hb")
                    evac_mask(k, "LTm", m_su)
                    TT(cl["TT0"].rearrange("p (h t) -> p h t", h=16), cl["LTm"].rearrange("p (h t) -> p h t", h=16),
                       bc16(ist), ALU.add, ["LTm", "cst"], ["TT0"])
                    k = nextpp()
                    layer(k, lambda cc, hh: [(fT(KTf, cc, hh), fT(ATf, cc, hh))], fkeys("ATf") + fkeys("KTf"), rtk="hb")
                    evac_mask(k, "AkT", m_su)
                    k = nextpp()
                    layer(k, lambda cc, hh: [(fT(BTf, cc, hh), fT(RTf, cc, hh))], fkeys("RTf") + fkeys("BTf"), rtk="hb")
                    evac_mask(k, "RbT", m_u)
                    k = nextpp()
                    layer(k, lambda cc, hh: [(fT(KTf, cc, hh), fT(RTf, cc, hh))], fkeys("RTf") + fkeys("KTf"), rtk="hb")
                    evac_mask(k, "RkT", m_u)
                    chk('B0')
                    k = nextpp()
                    layer(k, lambda cc, hh: [(tsl(cl["AkT"], cc, hh), tM(V, cc, hh))], ["AkT", ("V", j)])
                    ACOPY2(cl["Xv"], k, "Xv")
                    chk('B1')
                    Pn, PTn, TTn = "Lm", "LTm", "TT0"
                    for lev in range(5):
                        Pnew = "P%d" % (lev % 2)
                        PTnew = "PT%d" % (lev % 2)
                        TTnew = "TT%d" % ((lev + 1) % 2)
                        k = nextpp()
                        layer(k, lambda cc, hh, PTn=PTn, Pn=Pn: [(tsl(cl[PTn], cc, hh), tsl(cl[Pn], cc, hh))], [Pn, PTn])
                        ACOPY2(cl[Pnew], k, Pnew)
                        if lev < 4:
                            k = nextpp()
                            layer(k, lambda cc, hh, PTn=PTn, Pn=Pn: [(tsl(cl[Pn], cc, hh), tsl(cl[PTn], cc, hh))], [Pn, PTn])
                            ACOPY2(cl[PTnew], k, PTnew)
                        k = nextpp()
                        layer(k, lambda cc, hh, Pnew=Pnew, TTn=TTn: [(tsl(cl[Pnew], cc, hh), tsl(cl[TTn], cc, hh))],
                              [Pnew, TTn])
                        for hf in range(2):
                            sl = slice(hf * 512, (hf + 1) * 512)
                            TT(cl[TTnew][:, sl], pp[k][:, sl], cl[TTn][:, sl], ALU.add,
                               [("ps", 2 * k + hf), TTn], [TTnew])
                        Pn, PTn, TTn = Pnew, PTnew, TTnew
                    k = nextpp()
                    layer(k, lambda cc, hh: [(tsl(cl[TTn], cc, hh), tsl(cl["Xv"], cc, hh))], [TTn, "Xv"])
                    ACOPY2(cl["Uv"], k, "Uv")
                    k = nextpp()
                    layer(k, lambda cc, hh: [(tsl(cl[TTn], cc, hh), tM(Atm, cc, hh))], [TTn, ("Atm", j)])
                    ACOPY2(cl["Ah"], k, "Ah")
                    chk('B2')
                    k = nextpp()
                    layer(k, lambda cc, hh: [(tsl(cl["Ah"], cc, hh), tM(Bh, cc, hh))], ["Ah", ("Bh", j)], feat_out=True)
                    for cc in range(2):
                        for p_ in range(KC):
                            blk = slice((cc * 8 + p_) * 64, (cc * 8 + p_ + 1) * 64)
                            STT(cl["MT"][:, blk], ist, wc[:, p_, j * 2 + cc: j * 2 + cc + 1], pp[k][:, blk],
                                ALU.mult, ALU.add, pk(k) + ["cst", ("wc", p_)], ["MT"])
                    k = nextpp()
                    layer(k, lambda cc, hh: [(tM(Bh, cc, hh), tsl(cl["Uv"], cc, hh)), (tM(Kh, cc, hh), tM(V, cc, hh))],
                          ["Uv", ("Bh", j), ("Kh", j), ("V", j)], feat_out=True)
                    ACOPY2(G32, k, "G32")
                    k = nextpp()
                    layer(k, lambda cc, hh: [(tsl(cl["Ah"], cc, hh), tsl(cl["RbT"], cc, hh))], ["Ah", "RbT"], feat_out=True)
                    for cc in range(2):
                        t0 = j * 128 + cc * 64
                        TT(cl["RhT"][:, cc * 512:(cc + 1) * 512].rearrange("p (k t) -> p k t", k=KC),
                           pp[k][:, cc * 512:(cc + 1) * 512].rearrange("p (k t) -> p k t", k=KC),
                           RTf[:, :, t0:t0 + 64], ALU.add, pk(k) + fkeys("RTf"), ["RhT"])
                    chk('B3')
                    layer(2, lambda cc, hh: [(tsl(cl["RbT"], cc, hh), tsl(cl["Uv"], cc, hh)),
                                             (tsl(cl["RkT"], cc, hh), tM(V, cc, hh))],
                          ["RbT", "Uv", "RkT", ("V", j)])
                    for cc in range(2):
                        layer(0, lambda cc_, hh: [(fsl(cl["RhT"], cc_, hh), Hbf[(hh % 2) * 64:(hh % 2) * 64 + 64, hh // 2, :])],
                              ["RhT", "Hbf"], rtk="hb", ccs=(cc,))
                        hjobs = list(range(16))
                        first = P.pe_cls if P.pe_cls in (0, 64) else 0
                        hjobs.sort(key=lambda hh: ((hh % 2) * 64 != first, hh))
                        for hh in hjobs:
                            p_, hb2 = hh // 2, (hh % 2) * 64
                            hbank = 7 if hb2 == 0 else 6
                            MM(psum[hbank][hb2:hb2 + 64, p_ * 64:(p_ + 1) * 64], fsl(cl["MT"], cc, hh),
                               Hbf[hb2:hb2 + 64, p_, :], ["MT", "Hbf"], [("ps", hbank)], rt=hb2)
                        H32f = H32.rearrange("p k v -> p (k v)")
                        TT(H32f[0:64, :], psum[7][0:64, :], G32[0:64, cc * 512:(cc + 1) * 512], ALU.add,
                           [("ps", 7), "G32"], ["H32"])
                        TT(H32f[64:128, :], psum[6][64:128, :], G32[64:128, cc * 512:(cc + 1) * 512], ALU.add,
                           [("ps", 6), "G32"], ["H32"])
                        ACOPY(Hbf, H32, ["H32"], ["Hbf"])
                    chk('B4')
                    ACOPY2(y32, 2, "y32")
                    for hf in range(2):
                        sl = slice(hf * 512, (hf + 1) * 512)
                        TT(y32[:, sl], y32[:, sl], pp[0][:, sl], ALU.add, [("ps", hf), "y32"], ["y32"])
                    ACT(ysq, y32, AF.Square, ["y32"], ["ysq"])
                    v3 = lambda a: a.rearrange("p (h t) -> p h t", h=16)
                    P.add("dve", lambda e: e.tensor_reduce(out=st["s1"], in_=v3(y32), axis=AX.X, op=ALU.add),
                          reads=["y32"], writes=["s1"])
                    P.add("dve", lambda e: e.tensor_reduce(out=st["s2"], in_=v3(ysq), axis=AX.X, op=ALU.add),
                          reads=["ysq"], writes=["s2"])
                    TS(st["mean"], st["s1"], 1.0 / 64.0, None, ALU.mult, ALU.bypass, ["s1"], ["mean"])
                    TT(st["msq"], st["mean"], st["mean"], ALU.mult, ["mean"], ["msq"])
                    STT(st["var"], st["s2"], 1.0 / 64.0, st["msq"], ALU.mult, ALU.subtract, ["s2", "msq"], ["var"])
                    ACT(st["var"], st["var"], AF.Sqrt, ["var", "gneps"], ["var"], bias=gneps)
                    P.add("dve", lambda e: e.reciprocal(out=st["var"], in_=st["var"]), reads=["var"], writes=["var"])
                    bcs = lambda a: a.unsqueeze(2).to_broadcast([128, 16, 64])
                    TT(v3(y32), v3(y32), bcs(st["mean"]), ALU.subtract, ["y32", "mean"], ["y32"])
                    TT(v3(y32), v3(y32), bcs(st["var"]), ALU.mult, ["y32", "var"], ["y32"])
                    TT(y32, y32, lnw_rep, ALU.mult, ["y32", "rep"], ["y32"])
                    TT(y32, y32, lnb_rep, ALU.add, ["y32", "rep"], ["y32"])
                    TT(v3(yt), v3(V[:, j, :]), bcs(rks[:, j, :]), ALU.mult, [("V", j), "rks"], ["yt"])
                    TT(y32, y32, yt, ALU.add, ["y32", "yt"], ["y32"])
                    TT(zb, y32, g32[:, j, :], ALU.mult, ["y32", ("g32", j)], ["zb"])
                    for kc in range(KC):
                        P.add("pe", lambda e, kc=kc: e.transpose(
                            out=ps_bf6[:, kc * 128:(kc + 1) * 128], in_=zb[:, kc * 128:(kc + 1) * 128],
                            identity=ident_b), reads=["zb", "ident_b"], writes=[("ps", 6)])
                    ACOPY(zT[:, :, j * 128:(j + 1) * 128], ps_bf6.rearrange("p (k t) -> p k t", k=KC),
                          [("ps", 6)], [("sq", kc) for kc in range(KC)])
                chk('B5')
                P.add("sp", lambda e, b=b: e.dma_start(
                    out=xx, in_=hT_s[:, :, b * T:(b + 1) * T].rearrange("k p s -> p k s")),
                    reads=[("hT", b)], writes=xxk, dma_sem="rxx")
                for q in range(2):
                    wo_, wok = wt.load(rwo_s[:, q * 512:(q + 1) * 512].rearrange("(k p) f -> p k f", p=128),
                                       wkeys["rwo"])
                    for f4 in range(4):
                        f = q * 4 + f4
                        pb = f % 2
                        for kc in range(KC):
                            MM(psum[pb][:, 0:T], wo_[:, kc, f4 * 128:(f4 + 1) * 128], zT[:, kc, :],
                               [wok, ("sq", kc)], [("ps", pb)], start=(kc == 0), stop=(kc == KC - 1))
                        TT(xx[:, f, :], xx[:, f, :], psum[pb][:, 0:T], ALU.add, [("ps", pb), ("xx", f)], [("xx", f)])
                P.add("sp", lambda e, b=b: e.dma_start(
                    out=hT_s[:, :, b * T:(b + 1) * T].rearrange("k p s -> p k s"), in_=xx),
                    reads=xxk, writes=[("hT", b)], dma_sem="rxw")
              except _Stop:
                break

        phase_load_x()
        P.barrier()
        if mode == "mlp":
            phase_mlp(0, True)
        if mode == "gla":
            phase_gla()
            P.barrier()
            phase_final()
        if mode == "rwkv":
            phase_rwkv()
            P.barrier()
            phase_final()
        if mode == "full":
            phase_rwkv()
            P.barrier()
            phase_mlp(0, False)
            P.barrier()
            phase_gla()
            P.barrier()
            phase_mlp(1, True)
        P.add("sp", None, extra=P.last_tokens())
        P.emit(stack)
    return nc


C_IDENT = 0
C_ONES = 128
C_MU = 256
C_RM = 320
C_GN = 832
C_MSL = 1088
C_MSU = 1152
C_IST = 1216
C_BONES = 1280
C_HSEL = 1408
CST_W = 1410
V_GMIX = 0
V_GFFN = 16
V_GFIN = 32
V_BGK = 40
V_MU = 44
V_W0 = 92
V_A0 = 100
V_KK = 108
V_KA = 116
V_RK = 124
VEC_W = 132


def fm(v):
    return np.ascontiguousarray(np.asarray(v, np.float32).reshape(KC, 128).T)


def make_tables(inp):
    cst = np.zeros((128, CST_W), np.float32)
    cst[:, C_IDENT:C_IDENT + 128] = np.eye(128, dtype=np.float32)
    cst[:, C_ONES:C_ONES + 128] = 1.0
    pp = np.arange(128)[:, None] % 64
    tt = np.arange(64)[None, :]
    cst[:, C_MU:C_MU + 64] = (tt >= pp)
    cst[:, C_RM:C_RM + 512] = (np.arange(512)[None, :] % 64 != 0)
    cst[:, C_GN:C_GN + 256] = np.asarray(inp["gla_gnorm_g"], np.float32).reshape(1, 256)
    cst[:, C_MSL:C_MSL + 64] = (tt < pp)
    cst[:, C_MSU:C_MSU + 64] = (tt > pp)
    cst[:, C_IST:C_IST + 64] = (tt == pp)
    blk = np.arange(128) // 64
    cst[:, C_BONES:C_BONES + 128] = (blk[:, None] == blk[None, :])
    cst[:, C_HSEL:C_HSEL + 2] = (blk[:, None] == np.arange(2)[None, :])
    vec = np.zeros((128, VEC_W), np.float32)
    for l in range(2):
        vec[:, V_GMIX + l * KC:V_GMIX + (l + 1) * KC] = fm(inp["norm_mix_g"][l])
        vec[:, V_GFFN + l * KC:V_GFFN + (l + 1) * KC] = fm(inp["norm_ffn_g"][l])
    vec[:, V_GFIN:V_GFIN + KC] = fm(inp["final_g"])
    vec[:, V_BGK:V_BGK + 4] = np.asarray(inp["gla_b_gk2"], np.float32).reshape(4, 128).T
    for i in range(6):
        vec[:, V_MU + i * KC:V_MU + (i + 1) * KC] = fm(inp["rwkv_mu"][0][i])
    vec[:, V_W0:V_W0 + KC] = fm(inp["rwkv_w0"][0])
    vec[:, V_A0:V_A0 + KC] = fm(inp["rwkv_a0"][0])
    vec[:, V_KK:V_KK + KC] = fm(inp["rwkv_k_k"][0])
    vec[:, V_KA:V_KA + KC] = fm(inp["rwkv_k_a"][0])
    vec[:, V_RK:V_RK + KC] = fm(np.asarray(inp["rwkv_r_k"][0]).reshape(-1))
    return cst, vec


def make_in_map(inp, c, S=None):
    cst, vec = make_tables(inp)
    x = inp["x"][c]
    if S is not None:
        x = x[:S]
    m = dict(x=np.ascontiguousarray(x), cst=cst, vec=vec,
             mlp_up=np.ascontiguousarray(inp["mlp_up"]),
             mlp_down=np.ascontiguousarray(inp["mlp_down"]),
             gla_w_in=np.ascontiguousarray(inp["gla_w_in"][0]),
             gla_w_o=np.ascontiguousarray(inp["gla_w_o"][0]),
             gla_w_gk2=np.ascontiguousarray(inp["gla_w_gk2"][0]),
             rwkv_w_rkv=np.ascontiguousarray(inp["rwkv_w_rkv"][0]),
             rwkv_w_o=np.ascontiguousarray(inp["rwkv_w_o"][0]),
             rwkv_w1=np.ascontiguousarray(inp["rwkv_w1"][0]), rwkv_w2=np.ascontiguousarray(inp["rwkv_w2"][0]),
             rwkv_a1=np.ascontiguousarray(inp["rwkv_a1"][0]), rwkv_a2=np.ascontiguousarray(inp["rwkv_a2"][0]),
             rwkv_g1=np.ascontiguousarray(inp["rwkv_g1"][0]), rwkv_g2=np.ascontiguousarray(inp["rwkv_g2"][0]),
             rep=np.ascontiguousarray(np.concatenate(
                 [np.broadcast_to(np.asarray(inp["rwkv_lnx_w"][0], np.float32)[None, :], (128, D)),
                  np.broadcast_to(np.asarray(inp["rwkv_lnx_b"][0], np.float32)[None, :], (128, D))], axis=1)))
    return m


def kernel(**inp):
    S = inp["x"].shape[1]
    B = inp["x"].shape[0]
    nc = build_nc(S)
    in_maps = [make_in_map(inp, c) for c in range(B)]
    res = run_bass_kernel_spmd(nc, in_maps, core_ids=list(range(B)))
    return np.stack([r["out"] for r in res.results], axis=0)
```

```python
import contextlib
import numpy as np
import concourse.bass as bass
import concourse.mybir as mybir
from concourse.bass_utils import run_bass_kernel_spmd

F32 = mybir.dt.float32
BF16 = mybir.dt.bfloat16
AF = mybir.ActivationFunctionType
ALU = mybir.AluOpType
AX = mybir.AxisListType

D = 1024
KC = 8
DFF = 4096
NEPS = 1e-5
import os
SAME_ENGINE_SYNC = os.environ.get('SES', '1') == '1'
NOCAST = os.environ.get('NOCAST', '0') == '1'
RW_STOP = os.environ.get('RW_STOP', '')


class _Stop(Exception):
    pass


def chk(tag):
    if RW_STOP == tag:
        raise _Stop()


class Prog:
    ENGS = ("pe", "dve", "act", "pool", "sp")

    def __init__(self, nc):
        self.nc = nc
        self.ops = {e: [] for e in self.ENGS}
        self.lastw = {}
        self.readers = {}
        self.dma_cnt = {}
        self.waited = {e: {} for e in self.ENGS}
        self.marked = {e: set() for e in self.ENGS}
        self.pe_cls = None
        self.pe_tok = None

    def _need(self, eng, tok, waits):
        if tok is None:
            return
        if tok[0] == "eng":
            _, e2, idx2 = tok
            if e2 == eng and (eng == "pe" or not SAME_ENGINE_SYNC):
                return
            if e2 == eng and eng == "sp":
                return
            k = ("eng", e2)
            if self.waited[eng].get(k, -1) >= idx2:
                return
            self.waited[eng][k] = idx2
            self.marked[e2].add(idx2)
            waits.append(tok)
        else:
            _, sem, cnt = tok
            k = ("dma", sem)
            if self.waited[eng].get(k, -1) >= cnt:
                return
            self.waited[eng][k] = cnt
            waits.append(tok)

    def add(self, eng, fn, reads=(), writes=(), dma_sem=None, extra=(), force=(), rt=None):
        waits = []
        if eng == "pe" and fn is not None:
            cls = "full" if rt is None else rt
            if self.pe_cls is not None and cls != self.pe_cls:
                force = list(force) + [self.pe_tok]
        for t in force:
            if t is not None and t[0] == "eng":
                self.marked[t[1]].add(t[2])
                waits.append(t)
        for r in reads:
            self._need(eng, self.lastw.get(r), waits)
        for w in writes:
            self._need(eng, self.lastw.get(w), waits)
            for t in self.readers.get(w, ()):
                self._need(eng, t, waits)
        for t in extra:
            self._need(eng, t, waits)
        idx = len(self.ops[eng])
        if dma_sem is not None:
            self.dma_cnt[dma_sem] = self.dma_cnt.get(dma_sem, 0) + 16
            tok = ("dma", dma_sem, self.dma_cnt[dma_sem])
        else:
            tok = ("eng", eng, idx)
        self.ops[eng].append(dict(fn=fn, waits=waits, dma_sem=dma_sem))
        if eng == "pe" and fn is not None:
            self.pe_cls = cls
            self.pe_tok = tok
        for r in reads:
            self.readers.setdefault(r, []).append(tok)
        for w in writes:
            self.lastw[w] = tok
            self.readers[w] = []
        return tok

    def last_tokens(self):
        toks = []
        for e in self.ENGS:
            for i in range(len(self.ops[e]) - 1, -1, -1):
                if self.ops[e][i]["dma_sem"] is None:
                    toks.append(("eng", e, i))
                    break
        for s, c in self.dma_cnt.items():
            toks.append(("dma", s, c))
        return toks

    def barrier(self):
        toks = self.last_tokens()
        for e in self.ENGS:
            self.add(e, None, extra=toks)
        self.lastw = {}
        self.readers = {}

    def emit(self, stack):
        nc = self.nc
        esem = {e: stack.enter_context(nc.semaphore("es_" + e)) for e in self.ENGS}
        dsem = {s: stack.enter_context(nc.semaphore("ds_%d" % i))
                for i, s in enumerate(sorted(self.dma_cnt))}
        tick = {}
        for e in self.ENGS:
            c = 0
            tick[e] = {}
            for i in range(len(self.ops[e])):
                if i in self.marked[e]:
                    c += 1
                    tick[e][i] = c
        block = stack.enter_context(nc.Block())
        sect = dict(pe=block.tensor, dve=block.vector, act=block.scalar,
                    pool=block.gpsimd, sp=block.sync)

        def make(e):
            def body(eng):
                for i, op in enumerate(self.ops[e]):
                    for t in op["waits"]:
                        if t[0] == "eng":
                            eng.wait_ge(esem[t[1]], tick[t[1]][t[2]])
                        else:
                            eng.wait_ge(dsem[t[1]], t[2])
                    if op["fn"] is None:
                        if i in self.marked[e]:
                            eng.drain().then_inc(esem[e], 1)
                        continue
                    ins = op["fn"](eng)
                    if op["dma_sem"] is not None:
                        ins.then_inc(dsem[op["dma_sem"]], 16)
                    elif i in self.marked[e]:
                        ins.then_inc(esem[e], 1)
            return body

        for e in self.ENGS:
            sect[e](make(e))


class Arena:
    def __init__(self, ap, words):
        self.ap = ap
        self.words = words
        self.off = 0

    def reset(self, off=0):
        self.off = off

    def alloc(self, shape, dtype):
        assert shape[0] <= 128
        n = 1
        for s in shape[1:]:
            n *= s
        w = n if dtype == F32 else (n + 1) // 2
        w = (w + 1) // 2 * 2
        assert self.off + w <= self.words, ("SBUF arena overflow", self.off, w, self.words)
        v = self.ap[0:shape[0], self.off:self.off + w]
        self.off += w
        if dtype != F32:
            v = v.bitcast(dtype)
        v = v[:, 0:n]
        if len(shape) == 3:
            v = v.rearrange("p (a b) -> p a b", a=shape[1])
        elif len(shape) == 4:
            v = v.rearrange("p (a b c) -> p a b c", a=shape[1], b=shape[2])
        return v


def build_nc(S, mode="full", dbg=False):
    nc = bass.Bass("TRN2", target_bir_lowering=False)
    NB = S // 512
    stack = contextlib.ExitStack()
    with stack:
        P = Prog(nc)
        dt = {}

        def din(name, shape, dtype=F32):
            dt[name] = nc.dram_tensor(name, list(shape), dtype, kind="ExternalInput").ap()
            return dt[name]

        def dscr(name, shape, dtype):
            return nc.dram_tensor(name, list(shape), dtype, kind="Internal").ap()

        x_d = din("x", [S, D])
        out_d = nc.dram_tensor("out", [S, D], F32, kind="ExternalOutput").ap()
        cst_d = din("cst", [128, CST_W])
        vec_d = din("vec", [128, VEC_W])
        mlp_up_d = din("mlp_up", [2, D, DFF])
        mlp_down_d = din("mlp_down", [2, DFF, D])
        gla_w_in_d = din("gla_w_in", [D, 3088])
        gla_w_o_d = din("gla_w_o", [D, D])
        din("gla_w_gk2", [16, 512])
        rwkv_w_rkv_d = din("rwkv_w_rkv", [3, D, D])
        rwkv_w_o_d = din("rwkv_w_o", [D, D])
        din("rwkv_w1", [D, 64])
        din("rwkv_w2", [64, D])
        din("rwkv_a1", [D, 64])
        din("rwkv_a2", [64, D])
        din("rwkv_g1", [D, 160])
        din("rwkv_g2", [160, D])
        din("rep", [128, 2 * D])
        wrkv_s = dscr("wrkv_s", [3, D, D], BF16)
        rwo_s = dscr("rwo_s", [D, D], BF16)
        win_s = dscr("win_s", [D, 3088], BF16)
        gwo_s = dscr("gwo_s", [D, D], BF16)

        up_s = dscr("up_s", [2, D, DFF], BF16)
        down_s = dscr("down_s", [2, DFF, D], BF16)
        hT_s = dscr("hT_s", [KC, 128, S], F32)

        ARENA_WORDS = 51 * 1024
        arena_t = stack.enter_context(nc.sbuf_tensor("arena", [128, ARENA_WORDS], F32))
        A = Arena(arena_t[:], ARENA_WORDS)
        psum2 = [stack.enter_context(nc.psum_tensor("pp%d" % i, [128, 1024], F32))[:]
                 for i in range(4)]
        psum = [psum2[i // 2][:, (i % 2) * 512:(i % 2 + 1) * 512] for i in range(8)]

        cst = A.alloc([128, CST_W], F32)
        vec = A.alloc([128, VEC_W], F32)
        ident_f = cst[:, C_IDENT:C_IDENT + 128]
        ones_bf = A.alloc([128, 128], BF16)
        P.add("sp", lambda e: e.dma_start(out=cst, in_=cst_d), writes=["cst"], dma_sem="cst")
        P.add("sp", lambda e: e.dma_start(out=vec, in_=vec_d), writes=["vec"], dma_sem="vec")
        P.add("dve", lambda e: e.tensor_copy(out=ones_bf, in_=cst[:, C_ONES:C_ONES + 128]),
              reads=["cst"], writes=["ones_bf"])
        ident_b = A.alloc([128, 128], BF16)
        P.add("dve", lambda e: e.tensor_copy(out=ident_b, in_=ident_f), reads=["cst"], writes=["ident_b"])
        base_off = A.off

        small_w = {}
        if mode in ("rwkv", "full", "gla"):
            specs = []
            if mode in ("rwkv", "full"):
                specs += [("w1", [128, KC, 64], dt["rwkv_w1"].rearrange("(k p) f -> p k f", p=128)),
                          ("a1", [128, KC, 64], dt["rwkv_a1"].rearrange("(k p) f -> p k f", p=128)),
                          ("g1", [128, KC, 160], dt["rwkv_g1"].rearrange("(k p) f -> p k f", p=128)),
                          ("w2", [64, D], dt["rwkv_w2"]), ("a2", [64, D], dt["rwkv_a2"]),
                          ("g2a", [128, D], dt["rwkv_g2"][0:128, :]), ("g2b", [32, D], dt["rwkv_g2"][128:160, :])]
            if mode in ("gla", "full"):
                specs += [("wgk2", [16, 512], dt["gla_w_gk2"])]
            for nm, shp, src in specs:
                small_w[nm] = A.alloc(shp, BF16)
                P.add("pool", lambda e, buf=small_w[nm], src=src: e.dma_start(out=buf, in_=src),
                      writes=[nm], dma_sem=nm)
        base_off = A.off

        cast_state = dict(n=0, toks=[])
        wkeys = {}

        def cast_w(src, dst, rows, cols, name):
            step = max(1, (1 << 20) // (cols * 4))
            wkeys[name] = []
            for r0 in range(0, rows, step):
                r1 = min(rows, r0 + step)
                n = cast_state["n"]
                extra = [cast_state["toks"][n - 2]] if n >= 2 else []
                key = (name, r0)
                wkeys[name].append(key)
                tok = P.add("pool", lambda e, r0=r0, r1=r1: e.dma_start(out=dst[r0:r1, :], in_=src[r0:r1, :]),
                            writes=[key], dma_sem="cast%d" % (n % 2), extra=extra)
                cast_state["toks"].append(tok)
                cast_state["n"] = n + 1

        def cast_mlp(l):
            cast_w(mlp_up_d[l], up_s[l], D, DFF, "up%d" % l)
            cast_w(mlp_down_d[l], down_s[l], DFF, D, "down%d" % l)

        if mode in ("rwkv", "full"):
            for i in range(3):
                cast_w(rwkv_w_rkv_d[i], wrkv_s[i], D, D, "wrkv%d" % i)
            cast_w(rwkv_w_o_d, rwo_s, D, D, "rwo")
        if mode in ("mlp", "full"):
            cast_mlp(0)
        if mode in ("gla", "full"):
            cast_w(gla_w_in_d, win_s, D, 3088, "win")
            cast_w(gla_w_o_d, gwo_s, D, D, "gwo")
        if mode in ("full",):
            cast_mlp(1)

        def phase_load_x():
            A.reset(base_off)
            xin = [A.alloc([128, 4, D], F32) for _ in range(2)]
            hb = [A.alloc([128, KC, 512], F32) for _ in range(2)]
            for b in range(NB):
                xi = xin[b % 2]
                h = hb[b % 2]
                P.add("sp", lambda e, xi=xi, b=b: e.dma_start(
                    out=xi, in_=x_d[b * 512:(b + 1) * 512, :].rearrange("(j p) d -> p j d", p=128)),
                    writes=[("xin", b % 2)], dma_sem="xin%d" % (b % 2))
                for kc in range(KC):
                    ps = psum[kc % 4]
                    for j in range(4):
                        P.add("pe", lambda e, ps=ps, xi=xi, j=j, kc=kc: e.transpose(
                            out=ps[:, j * 128:(j + 1) * 128], in_=xi[:, j, kc * 128:(kc + 1) * 128],
                            identity=ident_f),
                            reads=[("xin", b % 2), "cst"], writes=[("ps", kc % 4)])
                    eng = "dve" if kc % 2 == 0 else "act"
                    if eng == "dve":
                        P.add("dve", lambda e, ps=ps, h=h, kc=kc: e.tensor_copy(out=h[:, kc, :], in_=ps),
                              reads=[("ps", kc % 4)], writes=[("hb", b % 2, kc)])
                    else:
                        P.add("act", lambda e, ps=ps, h=h, kc=kc: e.copy(out=h[:, kc, :], in_=ps),
                              reads=[("ps", kc % 4)], writes=[("hb", b % 2, kc)])
                P.add("sp", lambda e, h=h, b=b: e.dma_start(
                    out=hT_s[:, :, b * 512:(b + 1) * 512].rearrange("k p s -> p k s"), in_=h),
                    reads=[("hb", b % 2, kc) for kc in range(KC)], writes=[("hT", b)],
                    dma_sem="hTw%d" % (b % 2))

        def rms_rstd(h, hkey, sq, sqkey, rstd, rkey, psb):
            for kc in range(KC):
                P.add("act", lambda e, kc=kc: e.activation(out=sq[:, kc, :], in_=h[:, kc, :], func=AF.Square),
                      reads=[hkey + (kc,)], writes=[sqkey + (kc,)])
            for kc in range(KC):
                P.add("pe", lambda e, kc=kc: e.matmul(psum[psb], lhsT=ones_bf, rhs=sq[:, kc, :],
                                                      start=(kc == 0), stop=(kc == KC - 1)),
                      reads=[sqkey + (kc,), "ones_bf"], writes=[("ps", psb)])
            P.add("act", lambda e: e.activation(out=rstd, in_=psum[psb], func=AF.Sqrt,
                                                scale=1.0 / D, bias=eps_col),
                  reads=[("ps", psb), "epsc"], writes=[rkey])
            P.add("dve", lambda e: e.reciprocal(out=rstd, in_=rstd), reads=[rkey], writes=[rkey])

        eps_col = A.alloc([128, 1], F32)
        P.add("dve", lambda e: e.memset(eps_col, NEPS), writes=["epsc"])
        base_off = A.off

        def phase_mlp(l, final):
            A.reset(base_off)
            hb = [A.alloc([128, KC, 512], F32) for _ in range(2)]
            xn = A.alloc([128, KC, 512], BF16)
            h1 = A.alloc([128, 32, 512], BF16)
            rstd = A.alloc([128, 512], F32)
            rl = [A.alloc([128, 512], F32) for _ in range(2)]
            NWU = 4
            wu = [A.alloc([128, KC, 512], BF16) for _ in range(NWU)]
            NWD = 4
            wd = [A.alloc([128, 8, 512], BF16) for _ in range(NWD)]
            if final:
                yo = [A.alloc([128, 4, D], F32) for _ in range(1)]
                yT = A.alloc([128, KC, 512], F32)
            g_ffn = vec[:, V_GFFN + l * KC: V_GFFN + (l + 1) * KC]
            g_fin = vec[:, V_GFIN: V_GFIN + KC]
            nu = 0
            nd = 0
            for b in range(NB):
                h = hb[b % 2]
                hkey = ("mh", b % 2)
                P.add("sp", lambda e, h=h, b=b: e.dma_start(
                    out=h, in_=hT_s[:, :, b * 512:(b + 1) * 512].rearrange("k p s -> p k s")),
                    reads=[("hT", b)], writes=[hkey + (kc,) for kc in range(KC)],
                    dma_sem="mh%d" % (b % 2))
                sq = h1[:, 0:KC, :]
                rms_rstd(h, hkey, sq, ("h1",), rstd, "rstd", 7)
                for kc in range(KC):
                    P.add("dve", lambda e, h=h, kc=kc: e.scalar_tensor_tensor(
                        out=xn[:, kc, :], in0=h[:, kc, :], scalar=g_ffn[:, kc:kc + 1], in1=rstd,
                        op0=ALU.mult, op1=ALU.mult),
                        reads=[hkey + (kc,), "rstd", "vec"], writes=[("xn", kc)])
                for eg in range(8):
                    w = wu[nu % NWU]
                    wkey = ("wu", nu % NWU)
                    P.add("sp", lambda e, w=w, eg=eg: e.dma_start(
                        out=w, in_=up_s[l][:, eg * 512:(eg + 1) * 512].rearrange("(k p) e -> p k e", p=128)),
                        reads=wkeys["up%d" % l], writes=[wkey], dma_sem="wu%d" % (nu % NWU))
                    nu += 1
                    for j in range(4):
                        et = eg * 4 + j
                        pb = et % 4
                        for kc in range(KC):
                            P.add("pe", lambda e, w=w, j=j, kc=kc, pb=pb: e.matmul(
                                psum[pb], lhsT=w[:, kc, j * 128:(j + 1) * 128], rhs=xn[:, kc, :],
                                start=(kc == 0), stop=(kc == KC - 1)),
                                reads=[wkey, ("xn", kc)], writes=[("ps", pb)])
                        r = rl[et % 2]
                        P.add("act", lambda e, r=r, pb=pb: e.activation(out=r, in_=psum[pb], func=AF.Relu),
                              reads=[("ps", pb)], writes=[("rl", et % 2)])
                        P.add("dve", lambda e, r=r, pb=pb, et=et: e.tensor_tensor(
                            out=h1[:, et, :], in0=r, in1=psum[pb], op=ALU.mult),
                            reads=[("ps", pb), ("rl", et % 2)], writes=[("h1", et)])
                for fg in range(2):
                    for e4 in range(4):
                        w = wd[nd % NWD]
                        wkey = ("wd", nd % NWD)
                        P.add("sp", lambda e, w=w, fg=fg, e4=e4: e.dma_start(
                            out=w, in_=down_s[l][e4 * 1024:(e4 + 1) * 1024, fg * 512:(fg + 1) * 512]
                            .rearrange("(k p) f -> p k f", p=128)),
                            reads=wkeys["down%d" % l], writes=[wkey], dma_sem="wd%d" % (nd % NWD))
                        nd += 1
                        for fj in range(4):
                            pb = 4 + fj
                            for ek in range(8):
                                et = e4 * 8 + ek
                                P.add("pe", lambda e, w=w, fj=fj, ek=ek, et=et, pb=pb, e4=e4: e.matmul(
                                    psum[pb], lhsT=w[:, ek, fj * 128:(fj + 1) * 128], rhs=h1[:, et, :],
                                    start=(e4 == 0 and ek == 0), stop=(e4 == 3 and ek == 7)),
                                    reads=[wkey, ("h1", et)], writes=[("ps", pb)])
                    for fj in range(4):
                        f = fg * 4 + fj
                        pb = 4 + fj
                        P.add("dve", lambda e, h=h, f=f, pb=pb: e.tensor_tensor(
                            out=h[:, f, :], in0=h[:, f, :], in1=psum[pb], op=ALU.add),
                            reads=[("ps", pb), hkey + (f,)], writes=[hkey + (f,)])
                if not final:
                    P.add("sp", lambda e, h=h, b=b: e.dma_start(
                        out=hT_s[:, :, b * 512:(b + 1) * 512].rearrange("k p s -> p k s"), in_=h),
                        reads=[hkey + (kc,) for kc in range(KC)], writes=[("hT", b)],
                        dma_sem="mhw%d" % (b % 2))
                else:
                    final_out(h, hkey, b, h1[:, 0:KC, :], ("h1",), rstd, yT, yo[0])

        def final_out(h, hkey, b, sq2, sqkey, rstd, yT, y):
            g_fin = vec[:, V_GFIN: V_GFIN + KC]
            rms_rstd(h, hkey, sq2, sqkey, rstd, "rstd", 7)
            for kc in range(KC):
                P.add("dve", lambda e, h=h, kc=kc: e.scalar_tensor_tensor(
                    out=yT[:, kc, :], in0=h[:, kc, :], scalar=g_fin[:, kc:kc + 1], in1=rstd,
                    op0=ALU.mult, op1=ALU.mult),
                    reads=[hkey + (kc,), "rstd", "vec"], writes=[("yT", kc)])
            for j in range(4):
                for half in range(2):
                    pb = (j * 2 + half) % 4
                    for q in range(4):
                        kc = half * 4 + q
                        P.add("pe", lambda e, pb=pb, q=q, kc=kc, j=j: e.transpose(
                            out=psum[pb][:, q * 128:(q + 1) * 128],
                            in_=yT[:, kc, j * 128:(j + 1) * 128], identity=ident_f),
                            reads=[("yT", kc), "cst"], writes=[("ps", pb)])
                    if half == 0:
                        P.add("act", lambda e, y=y, j=j, pb=pb: e.copy(
                            out=y[:, j, 0:512], in_=psum[pb]),
                            reads=[("ps", pb)], writes=[("yo", j, 0)])
                    else:
                        P.add("dve", lambda e, y=y, j=j, pb=pb: e.tensor_copy(
                            out=y[:, j, 512:1024], in_=psum[pb]),
                            reads=[("ps", pb)], writes=[("yo", j, 1)])
            P.add("sp", lambda e, y=y, b=b: e.dma_start(
                out=out_d[b * 512:(b + 1) * 512, :].rearrange("(j p) d -> p j d", p=128), in_=y),
                reads=[("yo", j, hf) for j in range(4) for hf in range(2)],
                writes=[("out", b)], dma_sem="out")

        def phase_final():
            A.reset(base_off)
            hb = [A.alloc([128, KC, 512], F32) for _ in range(2)]
            sq = A.alloc([128, KC, 512], BF16)
            rstd = A.alloc([128, 512], F32)
            yT = A.alloc([128, KC, 512], F32)
            y = A.alloc([128, 4, D], F32)
            for b in range(NB):
                h = hb[b % 2]
                hkey = ("fh", b % 2)
                P.add("sp", lambda e, h=h, b=b: e.dma_start(
                    out=h, in_=hT_s[:, :, b * 512:(b + 1) * 512].rearrange("k p s -> p k s")),
                    reads=[("hT", b)], writes=[hkey + (kc,) for kc in range(KC)],
                    dma_sem="fh%d" % (b % 2))
                final_out(h, hkey, b, sq, ("fsq",), rstd, yT, y)


        class WStream:
            def __init__(self, name, nslots, shape):
                self.name = name
                self.slots = [A.alloc(shape, BF16) for _ in range(nslots)]
                self.n = 0

            def load(self, src, srckeys, view=None):
                i = self.n % len(self.slots)
                self.n += 1
                w = self.slots[i]
                dst = w if view is None else view(w)
                key = (self.name, i)
                P.add("sp", lambda e: e.dma_start(out=dst, in_=src), reads=srckeys, writes=[key],
                      dma_sem="%s%d" % (self.name, i))
                return w, key

        def load_norm(b, h, hkey, sq, sqkey, rstd, hn, gcols, sem):
            P.add("sp", lambda e: e.dma_start(
                out=h, in_=hT_s[:, :, b * 512:(b + 1) * 512].rearrange("k p s -> p k s")),
                reads=[("hT", b)], writes=[hkey + (kc,) for kc in range(KC)], dma_sem=sem)
            rms_rstd(h, hkey, sq, sqkey, rstd, "rstd", 7)
            for kc in range(KC):
                P.add("dve", lambda e, kc=kc: e.scalar_tensor_tensor(
                    out=hn[:, kc, :], in0=h[:, kc, :], scalar=gcols[:, kc:kc + 1], in1=rstd,
                    op0=ALU.mult, op1=ALU.mult),
                    reads=[hkey + (kc,), "rstd", "vec"], writes=[("hn", kc)])

        def store_h(b, h, hkey, sem):
            P.add("sp", lambda e: e.dma_start(
                out=hT_s[:, :, b * 512:(b + 1) * 512].rearrange("k p s -> p k s"), in_=h),
                reads=[hkey + (kc,) for kc in range(KC)], writes=[("hT", b)], dma_sem=sem)

        def out_proj(wo, wokey, zT, zkey, h, hkey):
            for f in range(KC):
                pb = 4 + (f % 2)
                for kc in range(KC):
                    P.add("pe", lambda e, f=f, kc=kc, pb=pb: e.matmul(
                        psum[pb], lhsT=wo[:, kc, f * 128:(f + 1) * 128], rhs=zT[:, kc, :],
                        start=(kc == 0), stop=(kc == KC - 1)),
                        reads=[wokey, (zkey, kc)], writes=[("ps", pb)])
                P.add("dve", lambda e, f=f, pb=pb: e.tensor_tensor(
                    out=h[:, f, :], in0=h[:, f, :], in1=psum[pb], op=ALU.add),
                    reads=[("ps", pb), hkey + (f,)], writes=[hkey + (f,)])

        def phase_gla():
            A.reset(base_off)
            l = 1
            g_mix = vec[:, V_GMIX + l * KC: V_GMIX + (l + 1) * KC]
            bgk = vec[:, V_BGK:V_BGK + 4]
            mask_u = cst[:, C_MU:C_MU + 64]
            rmask = cst[:, C_RM:C_RM + 512]
            gn_rep = cst[:, C_GN:C_GN + 256]
            hb = [A.alloc([128, KC, 512], F32) for _ in range(2)]
            hn = A.alloc([128, KC, 512], BF16)
            sq = A.alloc([128, KC, 512], BF16)
            zT = sq
            rstd = A.alloc([128, 512], F32)
            wt = WStream("gw", 3, [128, KC, 512])
            wo = A.alloc([128, KC, D], BF16)
            wgl = A.alloc([128, KC, 16], BF16)
            wgk2 = small_w["wgk2"]
            gl = A.alloc([16, 512], BF16)
            gkp = A.alloc([128, 4, 512], F32)
            cum = A.alloc([128, 4, 512], F32)
            et = [A.alloc([128, 512], F32) for _ in range(2)]
            qtT = A.alloc([128, 4, 512], BF16)
            ktT = A.alloc([128, 4, 512], BF16)
            khT = A.alloc([128, 4, 512], BF16)
            kh = A.alloc([128, 4, 512], BF16)
            V = A.alloc([128, 4, D], BF16)
            sog = A.alloc([128, 4, D], F32)
            scT = A.alloc([128, 256], BF16)
            S32 = A.alloc([128, 4, 256], F32)
            Sbf = A.alloc([128, 4, 256], BF16)
            elast = A.alloc([128, 4, 8], F32)
            t1 = [A.alloc([128, 256], F32) for _ in range(2)]
            zb = [A.alloc([128, D], BF16) for _ in range(2)]
            ssq = A.alloc([128, 4], F32)
            rinv = A.alloc([128, 4], F32)
            junk = A.alloc([128, 256], BF16)
            ps_bf6 = psum[6].bitcast(BF16)

            P.add("sp", lambda e: e.dma_start(out=wo, in_=gwo_s.rearrange("(k p) f -> p k f", p=128)),
                  reads=wkeys["gwo"], writes=["gwo_sb"], dma_sem="gwo_sb")
            P.add("sp", lambda e: e.dma_start(
                out=wgl, in_=win_s[:, 3072:3088].rearrange("(k p) f -> p k f", p=128)),
                reads=wkeys["win"], writes=["wgl"], dma_sem="wgl")
            P.add("dve", lambda e: e.memset(S32, 0.0), writes=[("S32", hh) for hh in range(4)])
            P.add("dve", lambda e: e.memset(Sbf, 0.0), writes=[("Sbf", hh) for hh in range(4)])

            def wcols(c0):
                return win_s[:, c0:c0 + 512].rearrange("(k p) f -> p k f", p=128)

            for b in range(NB):
                h = hb[b % 2]
                hkey = ("gh", b % 2)
                load_norm(b, h, hkey, sq, ("sq",), rstd, hn, g_mix, "gh%d" % (b % 2))
                for kc in range(KC):
                    P.add("pe", lambda e, kc=kc: e.matmul(psum[4][0:16, :], lhsT=wgl[:, kc, :], rhs=hn[:, kc, :],
                                                          start=(kc == 0), stop=(kc == KC - 1)),
                          reads=["wgl", ("hn", kc)], writes=[("ps", 4)])
                P.add("act", lambda e: e.copy(out=gl, in_=psum[4][0:16, :]), reads=[("ps", 4)], writes=["gl"])
                for hh in range(4):
                    pb = 4 + (hh + 1) % 2
                    P.add("pe", lambda e, hh=hh, pb=pb: e.matmul(
                        psum[pb], lhsT=wgk2[:, hh * 128:(hh + 1) * 128], rhs=gl, start=True, stop=True),
                        reads=["wgk2", "gl"], writes=[("ps", pb)], rt=0)
                    P.add("act", lambda e, hh=hh, pb=pb: e.activation(
                        out=gkp[:, hh, :], in_=psum[pb], func=AF.Sigmoid, bias=bgk[:, hh:hh + 1]),
                        reads=[("ps", pb), "vec"], writes=[("gkp", hh)])
                for hh in range(4):
                    P.add("act", lambda e, hh=hh: e.activation(out=gkp[:, hh, :], in_=gkp[:, hh, :], func=AF.Ln),
                          reads=[("gkp", hh)], writes=[("gkp", hh)])
                    P.add("dve", lambda e, hh=hh: e.tensor_tensor_scan(
                        out=cum[:, hh, :], data0=rmask, data1=gkp[:, hh, :], initial=0.0,
                        op0=ALU.mult, op1=ALU.add),
                        reads=[("gkp", hh), "cst"], writes=[("cum", hh)])
                P.add("act", lambda e: e.activation(
                    out=elast, in_=cum.rearrange("p h (c t) -> p h c t", t=64)[:, :, :, 63],
                    func=AF.Exp, scale=1.0 / 16.0),
                    reads=[("cum", hh) for hh in range(4)], writes=["elast"])
                wq, wqk = wt.load(wcols(0), wkeys["win"])
                for hh in range(4):
                    pb = 4 + hh % 2
                    e_ = et[hh % 2]
                    for kc in range(KC):
                        P.add("pe", lambda e, hh=hh, kc=kc, pb=pb: e.matmul(
                            psum[pb], lhsT=wq[:, kc, hh * 128:(hh + 1) * 128], rhs=hn[:, kc, :],
                            start=(kc == 0), stop=(kc == KC - 1)),
                            reads=[wqk, ("hn", kc)], writes=[("ps", pb)])
                    P.add("act", lambda e, hh=hh, e_=e_: e.activation(
                        out=e_, in_=cum[:, hh, :], func=AF.Exp, scale=1.0 / 16.0),
                        reads=[("cum", hh)], writes=[("et", hh % 2)])
                    P.add("dve", lambda e, hh=hh, e_=e_, pb=pb: e.scalar_tensor_tensor(
                        out=qtT[:, hh, :], in0=psum[pb], scalar=128.0 ** -0.5, in1=e_,
                        op0=ALU.mult, op1=ALU.mult),
                        reads=[("ps", pb), ("et", hh % 2)], writes=[("qtT", hh)])
                wk, wkk = wt.load(wcols(512), wkeys["win"])
                for hh in range(4):
                    pb = 4 + hh % 2
                    for kc in range(KC):
                        P.add("pe", lambda e, hh=hh, kc=kc, pb=pb: e.matmul(
                            psum[pb], lhsT=wk[:, kc, hh * 128:(hh + 1) * 128], rhs=hn[:, kc, :],
                            start=(kc == 0), stop=(kc == KC - 1)),
                            reads=[wkk, ("hn", kc)], writes=[("ps", pb)])
                    P.add("act", lambda e, hh=hh: e.activation(
                        out=et[0], in_=cum[:, hh, :], func=AF.Exp, scale=-1.0 / 16.0),
                        reads=[("cum", hh)], writes=[("et", 0)])
                    P.add("dve", lambda e, hh=hh, pb=pb: e.tensor_tensor(
                        out=ktT[:, hh, :], in0=psum[pb], in1=et[0], op=ALU.mult),
                        reads=[("ps", pb), ("et", 0)], writes=[("ktT", hh)])
                    cv = cum[:, hh, :].rearrange("p (c t) -> p c t", t=64)
                    P.add("dve", lambda e, hh=hh, cv=cv: e.tensor_tensor(
                        out=et[1].rearrange("p (c t) -> p c t", t=64),
                        in0=cv[:, :, 63:64].to_broadcast([128, 8, 64]), in1=cv, op=ALU.subtract),
                        reads=[("cum", hh)], writes=[("et", 1)])
                    P.add("act", lambda e: e.activation(out=et[1], in_=et[1], func=AF.Exp, scale=1.0 / 16.0),
                          reads=[("et", 1)], writes=[("et", 1)])
                    P.add("dve", lambda e, hh=hh, pb=pb: e.tensor_tensor(
                        out=khT[:, hh, :], in0=psum[pb], in1=et[1], op=ALU.mult),
                        reads=[("ps", pb), ("et", 1)], writes=[("khT", hh)])
                for j in range(4):
                    for hh in range(4):
                        P.add("pe", lambda e, j=j, hh=hh: e.transpose(
                            out=ps_bf6[:, hh * 128:(hh + 1) * 128], in_=khT[:, hh, j * 128:(j + 1) * 128],
                            identity=ident_b),
                            reads=[("khT", hh), "ident_b"], writes=[("ps", 6)])
                    P.add("act", lambda e, j=j: e.copy(out=kh[:, j, :], in_=ps_bf6[:, 0:512]),
                          reads=[("ps", 6)], writes=[("kh", j)])
                for half in range(2):
                    wv, wvk = wt.load(wcols(1024 + half * 512), wkeys["win"])
                    for j in range(4):
                        pb = 4 + j % 2
                        for kc in range(KC):
                            P.add("pe", lambda e, j=j, kc=kc, pb=pb, wv=wv: e.matmul(
                                psum[pb], lhsT=hn[:, kc, j * 128:(j + 1) * 128], rhs=wv[:, kc, :],
                                start=(kc == 0), stop=(kc == KC - 1)),
                                reads=[wvk, ("hn", kc)], writes=[("ps", pb)])
                        P.add("act", lambda e, j=j, pb=pb, half=half: e.copy(
                            out=V[:, j, half * 512:(half + 1) * 512], in_=psum[pb]),
                            reads=[("ps", pb)], writes=[("V", j, half)])
                for half in range(2):
                    wg, wgk = wt.load(wcols(2048 + half * 512), wkeys["win"])
                    for j in range(4):
                        pb = 4 + j % 2
                        for kc in range(KC):
                            P.add("pe", lambda e, j=j, kc=kc, pb=pb, wg=wg: e.matmul(
                                psum[pb], lhsT=hn[:, kc, j * 128:(j + 1) * 128], rhs=wg[:, kc, :],
                                start=(kc == 0), stop=(kc == KC - 1)),
                                reads=[wgk, ("hn", kc)], writes=[("ps", pb)])
                        P.add("act", lambda e, j=j, pb=pb, half=half: e.activation(
                            out=sog[:, j, half * 512:(half + 1) * 512], in_=psum[pb], func=AF.Silu),
                            reads=[("ps", pb)], writes=[("sog", j, half)])
                for j in range(4):
                    for cc in range(2):
                        c = j * 2 + cc
                        p0 = cc * 64
                        t0 = j * 128 + cc * 64
                        for hh in range(4):
                            P.add("pe", lambda e, hh=hh, p0=p0, t0=t0: e.matmul(
                                psum[2][p0:p0 + 64, hh * 64:(hh + 1) * 64],
                                lhsT=ktT[:, hh, t0:t0 + 64], rhs=qtT[:, hh, t0:t0 + 64], start=True, stop=True),
                                reads=[("ktT", hh), ("qtT", hh)], writes=[("ps", 2)])
                        P.add("dve", lambda e, p0=p0: e.tensor_tensor(
                            out=scT[p0:p0 + 64, :].rearrange("p (h t) -> p h t", h=4),
                            in0=psum[2][p0:p0 + 64, 0:256].rearrange("p (h t) -> p h t", h=4),
                            in1=mask_u[p0:p0 + 64, :].unsqueeze(1).to_broadcast([64, 4, 64]), op=ALU.mult),
                            reads=[("ps", 2), "cst"], writes=[("scT", cc)])
                        for hh in range(4):
                            ob = hh // 2
                            oc = (hh % 2) * 256
                            P.add("pe", lambda e, hh=hh, p0=p0, ob=ob, oc=oc, j=j: e.matmul(
                                psum[ob][p0:p0 + 64, oc:oc + 256], lhsT=scT[p0:p0 + 64, hh * 64:(hh + 1) * 64],
                                rhs=V[p0:p0 + 64, j, hh * 256:(hh + 1) * 256], start=True, stop=False),
                                reads=[("scT", cc), ("V", j, hh // 2)], writes=[("ps", ob)], rt=p0)
                            P.add("pe", lambda e, hh=hh, p0=p0, ob=ob, oc=oc, t0=t0: e.matmul(
                                psum[ob][p0:p0 + 64, oc:oc + 256], lhsT=qtT[:, hh, t0:t0 + 64],
                                rhs=Sbf[:, hh, :], start=False, stop=True),
                                reads=[("qtT", hh), ("Sbf", hh)], writes=[("ps", ob)])
                        for hh in range(4):
                            gb = 3 if hh % 2 == 0 else 7
                            P.add("pe", lambda e, hh=hh, p0=p0, gb=gb, j=j: e.matmul(
                                psum[gb][:, 0:256], lhsT=kh[p0:p0 + 64, j, hh * 128:(hh + 1) * 128],
                                rhs=V[p0:p0 + 64, j, hh * 256:(hh + 1) * 256], start=True, stop=True),
                                reads=[("kh", j), ("V", j, hh // 2)], writes=[("ps", gb)], rt=p0)
                            P.add("dve", lambda e, hh=hh, gb=gb, c=c: e.scalar_tensor_tensor(
                                out=S32[:, hh, :], in0=S32[:, hh, :], scalar=elast[:, hh, c:c + 1],
                                in1=psum[gb][:, 0:256], op0=ALU.mult, op1=ALU.add),
                                reads=[("ps", gb), ("S32", hh), "elast"], writes=[("S32", hh)])
                            P.add("act", lambda e, hh=hh: e.copy(out=Sbf[:, hh, :], in_=S32[:, hh, :]),
                                  reads=[("S32", hh)], writes=[("Sbf", hh)])
                    z = zb[j % 2]
                    for hh in range(4):
                        ob = hh // 2
                        oc = (hh % 2) * 256
                        P.add("act", lambda e, hh=hh, ob=ob, oc=oc: e.activation(
                            out=junk, in_=psum[ob][:, oc:oc + 256], func=AF.Square, accum_out=ssq[:, hh:hh + 1]),
                            reads=[("ps", ob)], writes=["junk", ("ssq", hh)])
                    P.add("dve", lambda e: e.tensor_scalar(out=rinv, in0=ssq, scalar1=1.0 / 256.0, scalar2=NEPS,
                                                           op0=ALU.mult, op1=ALU.add),
                          reads=[("ssq", hh) for hh in range(4)], writes=["rinv"])
                    P.add("act", lambda e: e.activation(out=rinv, in_=rinv, func=AF.Sqrt),
                          reads=["rinv"], writes=["rinv"])
                    P.add("dve", lambda e: e.reciprocal(out=rinv, in_=rinv), reads=["rinv"], writes=["rinv"])
                    for hh in range(4):
                        ob = hh // 2
                        oc = (hh % 2) * 256
                        tt = t1[hh % 2]
                        P.add("dve", lambda e, hh=hh, ob=ob, oc=oc, tt=tt: e.scalar_tensor_tensor(
                            out=tt, in0=psum[ob][:, oc:oc + 256], scalar=rinv[:, hh:hh + 1], in1=gn_rep,
                            op0=ALU.mult, op1=ALU.mult),
                            reads=[("ps", ob), "rinv", "cst"], writes=[("t1", hh % 2)])
                        P.add("dve", lambda e, hh=hh, tt=tt, z=z, j=j: e.tensor_tensor(
                            out=z[:, hh * 256:(hh + 1) * 256], in0=tt, in1=sog[:, j, hh * 256:(hh + 1) * 256],
                            op=ALU.mult),
                            reads=[("t1", hh % 2), ("sog", j, hh // 2)], writes=[("zb", j % 2, hh)])
                    for kc in range(KC):
                        P.add("pe", lambda e, kc=kc, z=z: e.transpose(
                            out=ps_bf6[:, kc * 128:(kc + 1) * 128], in_=z[:, kc * 128:(kc + 1) * 128],
                            identity=ident_b),
                            reads=[("zb", j % 2, kc // 2), "ident_b"], writes=[("ps", 6)])
                    P.add("act", lambda e, j=j: e.copy(
                        out=zT[:, :, j * 128:(j + 1) * 128],
                        in_=ps_bf6.rearrange("p (k t) -> p k t", k=KC)),
                        reads=[("ps", 6)], writes=[("sq", kc) for kc in range(KC)])
                out_proj(wo, "gwo_sb", zT, "sq", h, hkey)
                store_h(b, h, hkey, "ghw%d" % (b % 2))


        def MM(out, lhsT, rhs, reads, writes, start=True, stop=True, rt=None):
            cls = None if lhsT.partition_size == 128 else lhsT.start_partition
            return P.add("pe", lambda e: e.matmul(out, lhsT=lhsT, rhs=rhs, start=start, stop=stop),
                         reads=reads, writes=writes, rt=cls)

        def TT(out, in0, in1, op, reads, writes):
            P.add("dve", lambda e: e.tensor_tensor(out=out, in0=in0, in1=in1, op=op), reads=reads, writes=writes)

        def STT(out, in0, scalar, in1, op0, op1, reads, writes):
            P.add("dve", lambda e: e.scalar_tensor_tensor(out=out, in0=in0, scalar=scalar, in1=in1,
                                                          op0=op0, op1=op1), reads=reads, writes=writes)

        def TS(out, in0, s1, s2, op0, op1, reads, writes):
            P.add("dve", lambda e: e.tensor_scalar(out=out, in0=in0, scalar1=s1, scalar2=s2, op0=op0, op1=op1),
                  reads=reads, writes=writes)

        def ACT(out, in_, func, reads, writes, **kw):
            P.add("act", lambda e: e.activation(out=out, in_=in_, func=func, **kw), reads=reads, writes=writes)

        def ACOPY(out, in_, reads, writes):
            P.add("act", lambda e: e.copy(out=out, in_=in_), reads=reads, writes=writes)

        def phase_rwkv():
            A.reset(base_off)
            HORDER = [h for i in range(8) for h in (i, i + 8)]
            T = 256
            NBR = S // T
            C0 = float(np.exp(-0.5))
            g_mix = vec[:, V_GMIX: V_GMIX + KC]
            mu = [vec[:, V_MU + i * KC: V_MU + (i + 1) * KC] for i in range(6)]
            w0c = vec[:, V_W0:V_W0 + KC]
            a0c = vec[:, V_A0:V_A0 + KC]
            kkc = vec[:, V_KK:V_KK + KC]
            kac = vec[:, V_KA:V_KA + KC]
            rkc = vec[:, V_RK:V_RK + KC]
            m_sl = cst[:, C_MSL:C_MSL + 64]
            m_su = cst[:, C_MSU:C_MSU + 64]
            m_u = cst[:, C_MU:C_MU + 64]
            ist = cst[:, C_IST:C_IST + 64]
            rmask = cst[:, C_RM:C_RM + T]

            def bc16(m):
                return m.unsqueeze(1).to_broadcast([128, 16, 64])

            rep = A.alloc([128, 2 * D], F32)
            lnw_rep = rep[:, 0:D]
            lnb_rep = rep[:, D:2 * D]
            omka = A.alloc([128, KC], F32)
            gneps = A.alloc([128, 1], F32)
            bones = A.alloc([128, 128], BF16)
            hsel = A.alloc([128, 2], BF16)
            w1, a1, g1, w2, a2, g2a, g2b = (small_w[n_] for n_ in ("w1", "a1", "g1", "w2", "a2", "g2a", "g2b"))
            P.add("sp", lambda e: e.dma_start(out=rep, in_=dt["rep"]), writes=["rep"], dma_sem="rep")
            TS(omka, kac, -1.0, 1.0, ALU.mult, ALU.add, ["vec"], ["omka"])
            P.add("dve", lambda e: e.memset(gneps, 64e-5), writes=["gneps"])
            P.add("dve", lambda e: e.tensor_copy(out=bones, in_=cst[:, C_BONES:C_BONES + 128]),
                  reads=["cst"], writes=["bones"])
            P.add("dve", lambda e: e.tensor_copy(out=hsel, in_=cst[:, C_HSEL:C_HSEL + 2]),
                  reads=["cst"], writes=["hsel"])

            hx = A.alloc([128, KC, T + 1], F32)
            sq = A.alloc([128, KC, T], BF16)
            zT = sq
            rstd = A.alloc([128, T], F32)
            xx = A.alloc([128, KC, T], F32)
            xm = [A.alloc([128, KC, T], BF16) for _ in range(2)]
            wt = WStream("rw", 3, [128, KC, 512])
            twT = A.alloc([64, T], BF16)
            taT = A.alloc([64, T], BF16)
            sgA = A.alloc([128, T], BF16)
            sgB = A.alloc([32, T], BF16)
            tn = ["s", "al", "cum", "kk", "rn", "kkn", "tmp", "kmod", "bs", "cp", "Em", "Ep", "Epv", "El"]
            tb = {n: A.alloc([128, T], F32) for n in tn}
            kk2 = A.alloc([128, T], BF16)
            ATf = A.alloc([128, KC, T], BF16)
            BTf = A.alloc([128, KC, T], BF16)
            KTf = A.alloc([128, KC, T], BF16)
            RTf = A.alloc([128, KC, T], BF16)
            BhTf = A.alloc([128, KC, T], BF16)
            KhTf = A.alloc([128, KC, T], BF16)
            rkT = [A.alloc([128, T], BF16) for _ in range(2)]
            Atm = A.alloc([128, 2, D], BF16)
            Bh = A.alloc([128, 2, D], BF16)
            Kh = A.alloc([128, 2, D], BF16)
            V = A.alloc([128, 2, D], BF16)
            g32 = A.alloc([128, 2, D], F32)
            rks = A.alloc([128, 2, 16], F32)
            wc = A.alloc([128, KC, 4], F32)
            cl = {n: A.alloc([128, D], BF16) for n in
                  ["Lm", "LTm", "AkT", "RbT", "RkT", "P0", "PT0", "P1", "PT1", "TT0", "TT1", "Xv", "Uv", "Ah",
                   "MT", "RhT"]}
            G32 = A.alloc([128, D], F32)
            H32 = A.alloc([128, KC, 64], F32)
            Hbf = A.alloc([128, KC, 64], BF16)
            y32 = A.alloc([128, D], F32)
            ysq = A.alloc([128, D], F32)
            yt = A.alloc([128, D], F32)
            zb = A.alloc([128, D], BF16)
            st = {n: A.alloc([128, 16], F32) for n in ["s1", "s2", "mean", "msq", "var"]}
            ps_bf6 = psum[6].bitcast(BF16)
            pp = [psum2[0], psum2[1], psum2[2]]

            P.add("dve", lambda e: e.memset(H32, 0.0), writes=["H32"])
            P.add("dve", lambda e: e.memset(Hbf, 0.0), writes=["Hbf"])
            P.add("dve", lambda e: e.memset(hx[:, :, 0:1], 0.0), writes=["hxp"])
            hxk = [("hx", kc) for kc in range(KC)]
            xxk = [("xx", kc) for kc in range(KC)]

            def wtile(wi, q):
                return wt.load(wrkv_s[wi][:, q * 512:(q + 1) * 512].rearrange("(k p) f -> p k f", p=128),
                               wkeys["wrkv%d" % wi])

            for b in range(NBR):
              try:
                if b > 0:
                    P.add("dve", lambda e: e.tensor_copy(out=hx[:, :, 0:1], in_=hx[:, :, T:T + 1]),
                          reads=hxk, writes=["hxp"])
                P.add("sp", lambda e, b=b: e.dma_start(
                    out=hx[:, :, 1:T + 1], in_=hT_s[:, :, b * T:(b + 1) * T].rearrange("k p s -> p k s")),
                    reads=[("hT", b)] + (["hxp"] if b > 0 else []), writes=hxk, dma_sem="rhx")
                hb_ = hx[:, :, 1:T + 1]
                for kc in range(KC):
                    ACT(sq[:, kc, :], hb_[:, kc, :], AF.Square, [("hx", kc)], [("sq", kc)])
                for kc in range(KC):
                    MM(psum[7][:, 0:T], ones_bf, sq[:, kc, :], [("sq", kc), "ones_bf"], [("ps", 7)],
                       start=(kc == 0), stop=(kc == KC - 1))
                ACT(rstd, psum[7][:, 0:T], AF.Sqrt, [("ps", 7), "epsc"], ["rstd"], scale=1.0 / D, bias=eps_col)
                P.add("dve", lambda e: e.reciprocal(out=rstd, in_=rstd), reads=["rstd"], writes=["rstd"])
                for kc in range(KC):
                    STT(hb_[:, kc, :], hb_[:, kc, :], g_mix[:, kc:kc + 1], rstd, ALU.mult, ALU.mult,
                        [("hx", kc), "rstd", "vec"], [("hx", kc)])
                for kc in range(KC):
                    TT(xx[:, kc, :], hx[:, kc, 0:T], hb_[:, kc, :], ALU.subtract,
                       [("hx", kc), "hxp"], [("xx", kc)])

                def mix(i, slot):
                    for kc in range(KC):
                        STT(xm[slot][:, kc, :], xx[:, kc, :], mu[i][:, kc:kc + 1], hb_[:, kc, :], ALU.mult, ALU.add,
                            [("xx", kc), ("hx", kc), "vec"], [("xm", slot, kc)])

                def proj_fm(out_ps, M, wfn, slot, wkey):
                    for kc in range(KC):
                        MM(out_ps, wfn(kc), xm[slot][:, kc, :], [wkey, ("xm", slot, kc)], [("ps", out_ps_id[0])],
                           start=(kc == 0), stop=(kc == KC - 1))

                out_ps_id = [0]
                chk('A0')
                mix(3, 0)
                out_ps_id[0] = 0
                proj_fm(psum[0][0:64, 0:T], 64, lambda kc: w1[:, kc, :], 0, "w1")
                ACT(twT, psum[0][0:64, 0:T], AF.Tanh, [("ps", 0)], ["twT"])
                mix(4, 1)
                out_ps_id[0] = 1
                proj_fm(psum[1][0:64, 0:T], 64, lambda kc: a1[:, kc, :], 1, "a1")
                ACOPY(taT, psum[1][0:64, 0:T], [("ps", 1)], ["taT"])
                mix(5, 0)
                out_ps_id[0] = 2
                proj_fm(psum[2][:, 0:T], 128, lambda kc: g1[:, kc, 0:128], 0, "g1")
                ACT(sgA, psum[2][:, 0:T], AF.Sigmoid, [("ps", 2)], ["sgA"])
                out_ps_id[0] = 3
                proj_fm(psum[3][0:32, 0:T], 32, lambda kc: g1[:, kc, 128:160], 0, "g1")
                ACT(sgB, psum[3][0:32, 0:T], AF.Sigmoid, [("ps", 3)], ["sgB"])
                for j in range(2):
                    for half in range(2):
                        pb = (j * 2 + half) % 4
                        MM(psum[pb], sgA[:, j * 128:(j + 1) * 128], g2a[:, half * 512:(half + 1) * 512],
                           ["sgA", "g2a"], [("ps", pb)], start=True, stop=False)
                        MM(psum[pb], sgB[:, j * 128:(j + 1) * 128], g2b[:, half * 512:(half + 1) * 512],
                           ["sgB", "g2b"], [("ps", pb)], start=False, stop=True)
                        ACOPY(g32[:, j, half * 512:(half + 1) * 512], psum[pb], [("ps", pb)], [("g32", j)])
                mix(2, 1)
                for half in range(2):
                    wv, wvk = wtile(2, half)
                    for j in range(2):
                        pb = (j * 2 + half) % 4
                        for kc in range(KC):
                            MM(psum[pb], xm[1][:, kc, j * 128:(j + 1) * 128], wv[:, kc, :],
                               [wvk, ("xm", 1, kc)], [("ps", pb)], start=(kc == 0), stop=(kc == KC - 1))
                        ACOPY(V[:, j, half * 512:(half + 1) * 512], psum[pb], [("ps", pb)], [("V", j)])
                chk('A1')
                mix(0, 0)
                mix(1, 1)
                for p in range(KC):
                    if p % 4 == 0:
                        wr, wrk = wtile(0, p // 4)
                        wk_, wkk = wtile(1, p // 4)
                    c4 = (p % 4) * 128
                    for kc in range(KC):
                        MM(psum[0][:, 0:T], wr[:, kc, c4:c4 + 128], xm[0][:, kc, :], [wrk, ("xm", 0, kc)],
                           [("ps", 0)], start=(kc == 0), stop=(kc == KC - 1))
                    for kc in range(KC):
                        MM(psum[1][:, 0:T], wk_[:, kc, c4:c4 + 128], xm[1][:, kc, :], [wkk, ("xm", 1, kc)],
                           [("ps", 1)], start=(kc == 0), stop=(kc == KC - 1))
                    MM(psum[2][:, 0:T], w2[:, p * 128:(p + 1) * 128], twT, ["w2", "twT"], [("ps", 2)])
                    MM(psum[3][:, 0:T], a2[:, p * 128:(p + 1) * 128], taT, ["a2", "taT"], [("ps", 3)])
                    r_ps = psum[0][:, 0:T]
                    k_ps = psum[1][:, 0:T]
                    pc = slice(p, p + 1)
                    ACT(tb["s"], psum[2][:, 0:T], AF.Sigmoid, [("ps", 2), "vec"], ["t_s"], bias=w0c[:, pc])
                    ACT(tb["al"], psum[3][:, 0:T], AF.Sigmoid, [("ps", 3), "vec"], ["t_al"], bias=a0c[:, pc])
                    ACT(tb["cp"], tb["s"], AF.Exp, ["t_s"], ["t_cp"], scale=C0)
                    P.add("dve", lambda e: e.tensor_tensor_scan(
                        out=tb["cum"], data0=rmask, data1=tb["s"], initial=0.0, op0=ALU.mult, op1=ALU.add),
                        reads=["t_s", "cst"], writes=["t_cum"])
                    TS(tb["kk"], k_ps, kkc[:, pc], None, ALU.mult, ALU.bypass, [("ps", 1), "vec"], ["t_kk"])
                    ACT(kk2, tb["kk"], AF.Square, ["t_kk"], ["kk2"])
                    MM(psum[6][:, 0:T], bones, kk2, ["bones", "kk2"], [("ps", 6)])
                    ACT(tb["rn"], psum[6][:, 0:T], AF.Sqrt, [("ps", 6)], ["t_rn"])
                    TS(tb["rn"], tb["rn"], 1e-12, None, ALU.max, ALU.bypass, ["t_rn"], ["t_rn"])
                    P.add("dve", lambda e: e.reciprocal(out=tb["rn"], in_=tb["rn"]), reads=["t_rn"], writes=["t_rn"])
                    TT(tb["kkn"], tb["kk"], tb["rn"], ALU.mult, ["t_kk", "t_rn"], ["t_kkn"])
                    TS(tb["tmp"], tb["al"], kac[:, pc], omka[:, pc], ALU.mult, ALU.add,
                       ["t_al", "vec", "omka"], ["t_tmp"])
                    TT(tb["kmod"], k_ps, tb["tmp"], ALU.mult, [("ps", 1), "t_tmp"], ["t_kmod"])
                    TT(tb["bs"], tb["kkn"], tb["al"], ALU.mult, ["t_kkn", "t_al"], ["t_bs"])
                    ACT(tb["Em"], tb["cum"], AF.Exp, ["t_cum"], ["t_Em"], scale=C0)
                    ACT(tb["Ep"], tb["cum"], AF.Exp, ["t_cum"], ["t_Ep"], scale=-C0)
                    TT(tb["Epv"], tb["Ep"], tb["cp"], ALU.mult, ["t_Ep", "t_cp"], ["t_Epv"])
                    emv = tb["Em"].rearrange("p (c t) -> p c t", t=64)
                    epv = tb["Ep"].rearrange("p (c t) -> p c t", t=64)
                    TT(tb["El"].rearrange("p (c t) -> p c t", t=64), emv,
                       epv[:, :, 63:64].to_broadcast([128, T // 64, 64]), ALU.mult, ["t_Em", "t_Ep"], ["t_El"])
                    STT(ATf[:, p, :], tb["kkn"], -1.0, tb["Epv"], ALU.mult, ALU.mult, ["t_kkn", "t_Epv"], [("ATf", p)])
                    TT(BTf[:, p, :], tb["bs"], tb["Em"], ALU.mult, ["t_bs", "t_Em"], [("BTf", p)])
                    TT(KTf[:, p, :], tb["kmod"], tb["Em"], ALU.mult, ["t_kmod", "t_Em"], [("KTf", p)])
                    TT(RTf[:, p, :], r_ps, tb["Ep"], ALU.mult, [("ps", 0), "t_Ep"], [("RTf", p)])
                    TT(BhTf[:, p, :], tb["bs"], tb["El"], ALU.mult, ["t_bs", "t_El"], [("BhTf", p)])
                    TT(KhTf[:, p, :], tb["kmod"], tb["El"], ALU.mult, ["t_kmod", "t_El"], [("KhTf", p)])
                    rk_ = rkT[p % 2]
                    STT(rk_, r_ps, rkc[:, pc], tb["kmod"], ALU.mult, ALU.mult, [("ps", 0), "t_kmod", "vec"],
                        [("rkT", p % 2)])
                    ACOPY(wc[:, p, :], tb["Ep"].rearrange("p (c t) -> p c t", t=64)[:, :, 63], ["t_Ep"], [("wc", p)])
                    for j in range(2):
                        MM(psum[7][:, j * 16 + p * 2: j * 16 + p * 2 + 2], rk_[:, j * 128:(j + 1) * 128], hsel,
                           [("rkT", p % 2), "hsel"], [("ps", 7)])
                ACOPY(rks, psum[7][:, 0:32].rearrange("p (j h) -> p j h", j=2), [("ps", 7)], ["rks"])
                chk('A2')
                for (srcf, skey, dstm, dkey) in ((ATf, "ATf", Atm, "Atm"), (BhTf, "BhTf", Bh, "Bh"), (KhTf, "KhTf", Kh, "Kh")):
                    for j in range(2):
                        for p in range(KC):
                            P.add("pe", lambda e, srcf=srcf, j=j, p=p: e.transpose(
                                out=ps_bf6[:, p * 128:(p + 1) * 128], in_=srcf[:, p, j * 128:(j + 1) * 128],
                                identity=ident_b), reads=[(skey, p), "ident_b"], writes=[("ps", 6)])
                        ACOPY(dstm[:, j, :], ps_bf6, [("ps", 6)], [(dkey, j)])

                chk('A3')
                for j in range(2):
                    rot = [0]

                    def nextpp():
                        k = rot[0] % 2
                        rot[0] += 1
                        return k

                    def layer(k, opfn, rd, feat_out=False, rtk="p0", ccs=(0, 1), kp=None):
                        jobs = [(cc, hh) for cc in ccs for hh in range(16)]
                        rtof = (lambda cc, hh: (hh % 2) * 64) if rtk == "hb" else (lambda cc, hh: cc * 64)
                        first = P.pe_cls if P.pe_cls in (0, 64) else 0
                        jobs.sort(key=lambda ch: (rtof(*ch) != first, ch[0], ch[1]))
                        wkeys_ = kp if kp is not None else [("ps", 2 * k), ("ps", 2 * k + 1)]
                        for cc, hh in jobs:
                            p_, hb2, p0 = hh // 2, (hh % 2) * 64, cc * 64
                            if feat_out:
                                o = pp[k][hb2:hb2 + 64, (cc * 8 + p_) * 64:(cc * 8 + p_ + 1) * 64]
                            else:
                                o = pp[k][p0:p0 + 64, hh * 64:(hh + 1) * 64]
                            ops = opfn(cc, hh)
                            for i, (l_, r_) in enumerate(ops):
                                MM(o, l_, r_, rd, wkeys_, start=(i == 0), stop=(i == len(ops) - 1),
                                   rt=rtof(cc, hh))

                    def tsl(buf, cc, hh):
                        return buf[cc * 64:cc * 64 + 64, hh * 64:(hh + 1) * 64]

                    def fsl(buf, cc, hh):
                        p_, hb2 = hh // 2, (hh % 2) * 64
                        return buf[hb2:hb2 + 64, (cc * 8 + p_) * 64:(cc * 8 + p_ + 1) * 64]

                    def fT(bufF, cc, hh):
                        p_, hb2 = hh // 2, (hh % 2) * 64
                        t0 = j * 128 + cc * 64
                        return bufF[hb2:hb2 + 64, p_, t0:t0 + 64]

                    def tM(bufM, cc, hh):
                        return bufM[cc * 64:cc * 64 + 64, j, hh * 64:(hh + 1) * 64]

                    def pk(k):
                        return [("ps", 2 * k), ("ps", 2 * k + 1)]

                    def ACOPY2(dst, k, wkey):
                        for hf in range(2):
                            ACOPY(dst[:, hf * 512:(hf + 1) * 512], pp[k][:, hf * 512:(hf + 1) * 512],
                                  [("ps", 2 * k + hf)], [wkey])

                    fkeys = lambda nm: [(nm, p_) for p_ in range(KC)]

                    def evac_mask(k, dst, mask):
                        for hf in range(2):
                            sl = slice(hf * 512, (hf + 1) * 512)
                            TT(cl[dst][:, sl].rearrange("p (h t) -> p h t", h=8),
                               pp[k][:, sl].rearrange("p (h t) -> p h t", h=8),
                               mask.unsqueeze(1).to_broadcast([128, 8, 64]), ALU.mult,
                               [("ps", 2 * k + hf), "cst"], [dst])

                    k = nextpp()
                    layer(k, lambda cc, hh: [(fT(ATf, cc, hh), fT(BTf, cc, hh))], fkeys("ATf") + fkeys("BTf"), rtk="hb")
                    evac_mask(k, "Lm", m_sl)
                    k = nextpp()
                    layer(k, lambda cc, hh: [(fT(BTf, cc, hh), fT(ATf, cc, hh))], fkeys("ATf") + fkeys("BTf"), rtk="hb")
                    evac_mask(k, "LTm", m_su)
                    TT(cl["TT0"].rearrange("p (h t) -> p h t", h=16), cl["LTm"].rearrange("p (h t) -> p h t", h=16),
                       bc16(ist), ALU.add, ["LTm", "cst"], ["TT0"])
                    k = nextpp()
                    layer(k, lambda cc, hh: [(fT(KTf, cc, hh), fT(ATf, cc, hh))], fkeys("ATf") + fkeys("KTf"), rtk="hb")
                    evac_mask(k, "AkT", m_su)
                    k = nextpp()
                    layer(k, lambda cc, hh: [(fT(BTf, cc, hh), fT(RTf, cc, hh))], fkeys("RTf") + fkeys("BTf"), rtk="hb")
                    evac_mask(k, "RbT", m_u)
                    k = nextpp()
                    layer(k, lambda cc, hh: [(fT(KTf, cc, hh), fT(RTf, cc, hh))], fkeys("RTf") + fkeys("KTf"), rtk="hb")
                    evac_mask(k, "RkT", m_u)
                    chk('B0')
                    k = nextpp()
                    layer(k, lambda cc, hh: [(tsl(cl["AkT"], cc, hh), tM(V, cc, hh))], ["AkT", ("V", j)])
                    ACOPY2(cl["Xv"], k, "Xv")
                    chk('B1')
                    Pn, PTn, TTn = "Lm", "LTm", "TT0"
                    for lev in range(5):
                        Pnew = "P%d" % (lev % 2)
                        PTnew = "PT%d" % (lev % 2)
                        TTnew = "TT%d" % ((lev + 1) % 2)
                        k = nextpp()
                        layer(k, lambda cc, hh, PTn=PTn, Pn=Pn: [(tsl(cl[PTn], cc, hh), tsl(cl[Pn], cc, hh))], [Pn, PTn])
                        ACOPY2(cl[Pnew], k, Pnew)
                        if lev < 4:
                            k = nextpp()
                            layer(k, lambda cc, hh, PTn=PTn, Pn=Pn: [(tsl(cl[Pn], cc, hh), tsl(cl[PTn], cc, hh))], [Pn, PTn])
                            ACOPY2(cl[PTnew], k, PTnew)
                        k = nextpp()
                        layer(k, lambda cc, hh, Pnew=Pnew, TTn=TTn: [(tsl(cl[Pnew], cc, hh), tsl(cl[TTn], cc, hh))],
                              [Pnew, TTn])
                        for hf in range(2):
                            sl = slice(hf * 512, (hf + 1) * 512)
                            TT(cl[TTnew][:, sl], pp[k][:, sl], cl[TTn][:, sl], ALU.add,
                               [("ps", 2 * k + hf), TTn], [TTnew])
                        Pn, PTn, TTn = Pnew, PTnew, TTnew
                    k = nextpp()
                    layer(k, lambda cc, hh: [(tsl(cl[TTn], cc, hh), tsl(cl["Xv"], cc, hh))], [TTn, "Xv"])
                    ACOPY2(cl["Uv"], k, "Uv")
                    k = nextpp()
                    layer(k, lambda cc, hh: [(tsl(cl[TTn], cc, hh), tM(Atm, cc, hh))], [TTn, ("Atm", j)])
                    ACOPY2(cl["Ah"], k, "Ah")
                    chk('B2')
                    k = nextpp()
                    layer(k, lambda cc, hh: [(tsl(cl["Ah"], cc, hh), tM(Bh, cc, hh))], ["Ah", ("Bh", j)], feat_out=True)
                    for cc in range(2):
                        for p_ in range(KC):
                            blk = slice((cc * 8 + p_) * 64, (cc * 8 + p_ + 1) * 64)
                            STT(cl["MT"][:, blk], ist, wc[:, p_, j * 2 + cc: j * 2 + cc + 1], pp[k][:, blk],
                                ALU.mult, ALU.add, pk(k) + ["cst", ("wc", p_)], ["MT"])
                    k = nextpp()
                    layer(k, lambda cc, hh: [(tM(Bh, cc, hh), tsl(cl["Uv"], cc, hh)), (tM(Kh, cc, hh), tM(V, cc, hh))],
                          ["Uv", ("Bh", j), ("Kh", j), ("V", j)], feat_out=True)
                    ACOPY2(G32, k, "G32")
                    k = nextpp()
                    layer(k, lambda cc, hh: [(tsl(cl["Ah"], cc, hh), tsl(cl["RbT"], cc, hh))], ["Ah", "RbT"], feat_out=True)
                    for cc in range(2):
                        t0 = j * 128 + cc * 64
                        TT(cl["RhT"][:, cc * 512:(cc + 1) * 512].rearrange("p (k t) -> p k t", k=KC),
                           pp[k][:, cc * 512:(cc + 1) * 512].rearrange("p (k t) -> p k t", k=KC),
                           RTf[:, :, t0:t0 + 64], ALU.add, pk(k) + fkeys("RTf"), ["RhT"])
                    chk('B3')
                    layer(2, lambda cc, hh: [(tsl(cl["RbT"], cc, hh), tsl(cl["Uv"], cc, hh)),
                                             (tsl(cl["RkT"], cc, hh), tM(V, cc, hh))],
                          ["RbT", "Uv", "RkT", ("V", j)])
                    for cc in range(2):
                        layer(0, lambda cc_, hh: [(fsl(cl["RhT"], cc_, hh), Hbf[(hh % 2) * 64:(hh % 2) * 64 + 64, hh // 2, :])],
                              ["RhT", "Hbf"], rtk="hb", ccs=(cc,))
                        hjobs = list(range(16))
                        first = P.pe_cls if P.pe_cls in (0, 64) else 0
                        hjobs.sort(key=lambda hh: ((hh % 2) * 64 != first, hh))
                        for hh in hjobs:
                            p_, hb2 = hh // 2, (hh % 2) * 64
                            hbank = 7 if hb2 == 0 else 6
                            MM(psum[hbank][hb2:hb2 + 64, p_ * 64:(p_ + 1) * 64], fsl(cl["MT"], cc, hh),
                               Hbf[hb2:hb2 + 64, p_, :], ["MT", "Hbf"], [("ps", hbank)], rt=hb2)
                        H32f = H32.rearrange("p k v -> p (k v)")
                        TT(H32f[0:64, :], psum[7][0:64, :], G32[0:64, cc * 512:(cc + 1) * 512], ALU.add,
                           [("ps", 7), "G32"], ["H32"])
                        TT(H32f[64:128, :], psum[6][64:128, :], G32[64:128, cc * 512:(cc + 1) * 512], ALU.add,
                           [("ps", 6), "G32"], ["H32"])
                        ACOPY(Hbf, H32, ["H32"], ["Hbf"])
                    chk('B4')
                    ACOPY2(y32, 2, "y32")
                    for hf in range(2):
                        sl = slice(hf * 512, (hf + 1) * 512)
                        TT(y32[:, sl], y32[:, sl], pp[0][:, sl], ALU.add, [("ps", hf), "y32"], ["y32"])
                    ACT(ysq, y32, AF.Square, ["y32"], ["ysq"])
                    v3 = lambda a: a.rearrange("p (h t) -> p h t", h=16)
                    P.add("dve", lambda e: e.tensor_reduce(out=st["s1"], in_=v3(y32), axis=AX.X, op=ALU.add),
                          reads=["y32"], writes=["s1"])
                    P.add("dve", lambda e: e.tensor_reduce(out=st["s2"], in_=v3(ysq), axis=AX.X, op=ALU.add),
                          reads=["ysq"], writes=["s2"])
                    TS(st["mean"], st["s1"], 1.0 / 64.0, None, ALU.mult, ALU.bypass, ["s1"], ["mean"])
                    TT(st["msq"], st["mean"], st["mean"], ALU.mult, ["mean"], ["msq"])
                    STT(st["var"], st["s2"], 1.0 / 64.0, st["msq"], ALU.mult, ALU.subtract, ["s2", "msq"], ["var"])
                    ACT(st["var"], st["var"], AF.Sqrt, ["var", "gneps"], ["var"], bias=gneps)
                    P.add("dve", lambda e: e.reciprocal(out=st["var"], in_=st["var"]), reads=["var"], writes=["var"])
                    bcs = lambda a: a.unsqueeze(2).to_broadcast([128, 16, 64])
                    TT(v3(y32), v3(y32), bcs(st["mean"]), ALU.subtract, ["y32", "mean"], ["y32"])
                    TT(v3(y32), v3(y32), bcs(st["var"]), ALU.mult, ["y32", "var"], ["y32"])
                    TT(y32, y32, lnw_rep, ALU.mult, ["y32", "rep"], ["y32"])
                    TT(y32, y32, lnb_rep, ALU.add, ["y32", "rep"], ["y32"])
                    TT(v3(yt), v3(V[:, j, :]), bcs(rks[:, j, :]), ALU.mult, [("V", j), "rks"], ["yt"])
                    TT(y32, y32, yt, ALU.add, ["y32", "yt"], ["y32"])
                    TT(zb, y32, g32[:, j, :], ALU.mult, ["y32", ("g32", j)], ["zb"])
                    for kc in range(KC):
                        P.add("pe", lambda e, kc=kc: e.transpose(
                            out=ps_bf6[:, kc * 128:(kc + 1) * 128], in_=zb[:, kc * 128:(kc + 1) * 128],
                            identity=ident_b), reads=["zb", "ident_b"], writes=[("ps", 6)])
                    ACOPY(zT[:, :, j * 128:(j + 1) * 128], ps_bf6.rearrange("p (k t) -> p k t", k=KC),
                          [("ps", 6)], [("sq", kc) for kc in range(KC)])
                chk('B5')
                P.add("sp", lambda e, b=b: e.dma_start(
                    out=xx, in_=hT_s[:, :, b * T:(b + 1) * T].rearrange("k p s -> p k s")),
                    reads=[("hT", b)], writes=xxk, dma_sem="rxx")
                for q in range(2):
                    wo_, wok = wt.load(rwo_s[:, q * 512:(q + 1) * 512].rearrange("(k p) f -> p k f", p=128),
                                       wkeys["rwo"])
                    for f4 in range(4):
                        f = q * 4 + f4
                        pb = f % 2
                        for kc in range(KC):
                            MM(psum[pb][:, 0:T], wo_[:, kc, f4 * 128:(f4 + 1) * 128], zT[:, kc, :],
                               [wok, ("sq", kc)], [("ps", pb)], start=(kc == 0), stop=(kc == KC - 1))
                        TT(xx[:, f, :], xx[:, f, :], psum[pb][:, 0:T], ALU.add, [("ps", pb), ("xx", f)], [("xx", f)])
                P.add("sp", lambda e, b=b: e.dma_start(
                    out=hT_s[:, :, b * T:(b + 1) * T].rearrange("k p s -> p k s"), in_=xx),
                    reads=xxk, writes=[("hT", b)], dma_sem="rxw")
              except _Stop:
                break

        phase_load_x()
        P.barrier()
        if mode == "mlp":
            phase_mlp(0, True)
        if mode == "gla":
            phase_gla()
            P.barrier()
            phase_final()
        if mode == "rwkv":
            phase_rwkv()
            P.barrier()
            phase_final()
        if mode == "full":
            phase_rwkv()
            P.barrier()
            phase_mlp(0, False)
            P.barrier()
            phase_gla()
            P.barrier()
            phase_mlp(1, True)
        P.add("sp", None, extra=P.last_tokens())
        P.emit(stack)
    return nc


C_IDENT = 0
C_ONES = 128
C_MU = 256
C_RM = 320
C_GN = 832
C_MSL = 1088
C_MSU = 1152
C_IST = 1216
C_BONES = 1280
C_HSEL = 1408
CST_W = 1410
V_GMIX = 0
V_GFFN = 16
V_GFIN = 32
V_BGK = 40
V_MU = 44
V_W0 = 92
V_A0 = 100
V_KK = 108
V_KA = 116
V_RK = 124
VEC_W = 132


def fm(v):
    return np.ascontiguousarray(np.asarray(v, np.float32).reshape(KC, 128).T)


def make_tables(inp):
    cst = np.zeros((128, CST_W), np.float32)
    cst[:, C_IDENT:C_IDENT + 128] = np.eye(128, dtype=np.float32)
    cst[:, C_ONES:C_ONES + 128] = 1.0
    pp = np.arange(128)[:, None] % 64
    tt = np.arange(64)[None, :]
    cst[:, C_MU:C_MU + 64] = (tt >= pp)
    cst[:, C_RM:C_RM + 512] = (np.arange(512)[None, :] % 64 != 0)
    cst[:, C_GN:C_GN + 256] = np.asarray(inp["gla_gnorm_g"], np.float32).reshape(1, 256)
    cst[:, C_MSL:C_MSL + 64] = (tt < pp)
    cst[:, C_MSU:C_MSU + 64] = (tt > pp)
    cst[:, C_IST:C_IST + 64] = (tt == pp)
    blk = np.arange(128) // 64
    cst[:, C_BONES:C_BONES + 128] = (blk[:, None] == blk[None, :])
    cst[:, C_HSEL:C_HSEL + 2] = (blk[:, None] == np.arange(2)[None, :])
    vec = np.zeros((128, VEC_W), np.float32)
    for l in range(2):
        vec[:, V_GMIX + l * KC:V_GMIX + (l + 1) * KC] = fm(inp["norm_mix_g"][l])
        vec[:, V_GFFN + l * KC:V_GFFN + (l + 1) * KC] = fm(inp["norm_ffn_g"][l])
    vec[:, V_GFIN:V_GFIN + KC] = fm(inp["final_g"])
    vec[:, V_BGK:V_BGK + 4] = np.asarray(inp["gla_b_gk2"], np.float32).reshape(4, 128).T
    for i in range(6):
        vec[:, V_MU + i * KC:V_MU + (i + 1) * KC] = fm(inp["rwkv_mu"][0][i])
    vec[:, V_W0:V_W0 + KC] = fm(inp["rwkv_w0"][0])
    vec[:, V_A0:V_A0 + KC] = fm(inp["rwkv_a0"][0])
    vec[:, V_KK:V_KK + KC] = fm(inp["rwkv_k_k"][0])
    vec[:, V_KA:V_KA + KC] = fm(inp["rwkv_k_a"][0])
    vec[:, V_RK:V_RK + KC] = fm(np.asarray(inp["rwkv_r_k"][0]).reshape(-1))
    return cst, vec


def make_in_map(inp, c, S=None):
    cst, vec = make_tables(inp)
    x = inp["x"][c]
    if S is not None:
        x = x[:S]
    m = dict(x=np.ascontiguousarray(x), cst=cst, vec=vec,
             mlp_up=np.ascontiguousarray(inp["mlp_up"]),
             mlp_down=np.ascontiguousarray(inp["mlp_down"]),
             gla_w_in=np.ascontiguousarray(inp["gla_w_in"][0]),
             gla_w_o=np.ascontiguousarray(inp["gla_w_o"][0]),
             gla_w_gk2=np.ascontiguousarray(inp["gla_w_gk2"][0]),
             rwkv_w_rkv=np.ascontiguousarray(inp["rwkv_w_rkv"][0]),
             rwkv_w_o=np.ascontiguousarray(inp["rwkv_w_o"][0]),
             rwkv_w1=np.ascontiguousarray(inp["rwkv_w1"][0]), rwkv_w2=np.ascontiguousarray(inp["rwkv_w2"][0]),
             rwkv_a1=np.ascontiguousarray(inp["rwkv_a1"][0]), rwkv_a2=np.ascontiguousarray(inp["rwkv_a2"][0]),
             rwkv_g1=np.ascontiguousarray(inp["rwkv_g1"][0]), rwkv_g2=np.ascontiguousarray(inp["rwkv_g2"][0]),
             rep=np.ascontiguousarray(np.concatenate(
                 [np.broadcast_to(np.asarray(inp["rwkv_lnx_w"][0], np.float32)[None, :], (128, D)),
                  np.broadcast_to(np.asarray(inp["rwkv_lnx_b"][0], np.float32)[None, :], (128, D))], axis=1)))
    return m


def kernel(**inp):
    S = inp["x"].shape[1]
    B = inp["x"].shape[0]
    nc = build_nc(S)
    in_maps = [make_in_map(inp, c) for c in range(B)]
    res = run_bass_kernel_spmd(nc, in_maps, core_ids=list(range(B)))
    return np.stack([r["out"] for r in res.results], axis=0)
```

```python
import contextlib
import numpy as np
import concourse.bass as bass
import concourse.mybir as mybir
from concourse.bass_utils import run_bass_kernel_spmd

F32 = mybir.dt.float32
BF16 = mybir.dt.bfloat16
AF = mybir.ActivationFunctionType
ALU = mybir.AluOpType
AX = mybir.AxisListType

D = 1024
KC = 8
DFF = 4096
NEPS = 1e-5
import os
SAME_ENGINE_SYNC = os.environ.get('SES', '1') == '1'
NOCAST = os.environ.get('NOCAST', '0') == '1'
RW_STOP = os.environ.get('RW_STOP', '')


class _Stop(Exception):
    pass


def chk(tag):
    if RW_STOP == tag:
        raise _Stop()


class Prog:
    ENGS = ("pe", "dve", "act", "pool", "sp")

    def __init__(self, nc):
        self.nc = nc
        self.ops = {e: [] for e in self.ENGS}
        self.lastw = {}
        self.readers = {}
        self.dma_cnt = {}
        self.waited = {e: {} for e in self.ENGS}
        self.marked = {e: set() for e in self.ENGS}
        self.pe_cls = None
        self.pe_tok = None

    def _need(self, eng, tok, waits):
        if tok is None:
            return
        if tok[0] == "eng":
            _, e2, idx2 = tok
            if e2 == eng and (eng == "pe" or not SAME_ENGINE_SYNC):
                return
            if e2 == eng and eng == "sp":
                return
            k = ("eng", e2)
            if self.waited[eng].get(k, -1) >= idx2:
                return
            self.waited[eng][k] = idx2
            self.marked[e2].add(idx2)
            waits.append(tok)
        else:
            _, sem, cnt = tok
            k = ("dma", sem)
            if self.waited[eng].get(k, -1) >= cnt:
                return
            self.waited[eng][k] = cnt
            waits.append(tok)

    def add(self, eng, fn, reads=(), writes=(), dma_sem=None, extra=(), force=(), rt=None):
        waits = []
        if eng == "pe" and fn is not None:
            cls = "full" if rt is None else rt
            if self.pe_cls is not None and cls != self.pe_cls:
                force = list(force) + [self.pe_tok]
        for t in force:
            if t is not None and t[0] == "eng":
                self.marked[t[1]].add(t[2])
                waits.append(t)
        for r in reads:
            self._need(eng, self.lastw.get(r), waits)
        for w in writes:
            self._need(eng, self.lastw.get(w), waits)
            for t in self.readers.get(w, ()):
                self._need(eng, t, waits)
        for t in extra:
            self._need(eng, t, waits)
        idx = len(self.ops[eng])
        if dma_sem is not None:
            self.dma_cnt[dma_sem] = self.dma_cnt.get(dma_sem, 0) + 16
            tok = ("dma", dma_sem, self.dma_cnt[dma_sem])
        else:
            tok = ("eng", eng, idx)
        self.ops[eng].append(dict(fn=fn, waits=waits, dma_sem=dma_sem))
        if eng == "pe" and fn is not None:
            self.pe_cls = cls
            self.pe_tok = tok
        for r in reads:
            self.readers.setdefault(r, []).append(tok)
        for w in writes:
            self.lastw[w] = tok
            self.readers[w] = []
        return tok

    def last_tokens(self):
        toks = []
        for e in self.ENGS:
            for i in range(len(self.ops[e]) - 1, -1, -1):
                if self.ops[e][i]["dma_sem"] is None:
                    toks.append(("eng", e, i))
                    break
        for s, c in self.dma_cnt.items():
            toks.append(("dma", s, c))
        return toks

    def barrier(self):
        toks = self.last_tokens()
        for e in self.ENGS:
            self.add(e, None, extra=toks)
        self.lastw = {}
        self.readers = {}

    def emit(self, stack):
        nc = self.nc
        esem = {e: stack.enter_context(nc.semaphore("es_" + e)) for e in self.ENGS}
        dsem = {s: stack.enter_context(nc.semaphore("ds_%d" % i))
                for i, s in enumerate(sorted(self.dma_cnt))}
        tick = {}
        for e in self.ENGS:
            c = 0
            tick[e] = {}
            for i in range(len(self.ops[e])):
                if i in self.marked[e]:
                    c += 1
                    tick[e][i] = c
        block = stack.enter_context(nc.Block())
        sect = dict(pe=block.tensor, dve=block.vector, act=block.scalar,
                    pool=block.gpsimd, sp=block.sync)

        def make(e):
            def body(eng):
                for i, op in enumerate(self.ops[e]):
                    for t in op["waits"]:
                        if t[0] == "eng":
                            eng.wait_ge(esem[t[1]], tick[t[1]][t[2]])
                        else:
                            eng.wait_ge(dsem[t[1]], t[2])
                    if op["fn"] is None:
                        if i in self.marked[e]:
                            eng.drain().then_inc(esem[e], 1)
                        continue
                    ins = op["fn"](eng)
                    if op["dma_sem"] is not None:
                        ins.then_inc(dsem[op["dma_sem"]], 16)
                    elif i in self.marked[e]:
                        ins.then_inc(esem[e], 1)
            return body

        for e in self.ENGS:
            sect[e](make(e))


class Arena:
    def __init__(self, ap, words):
        self.ap = ap
        self.words = words
        self.off = 0

    def reset(self, off=0):
        self.off = off

    def alloc(self, shape, dtype):
        assert shape[0] <= 128
        n = 1
        for s in shape[1:]:
            n *= s
        w = n if dtype == F32 else (n + 1) // 2
        w = (w + 1) // 2 * 2
        assert self.off + w <= self.words, ("SBUF arena overflow", self.off, w, self.words)
        v = self.ap[0:shape[0], self.off:self.off + w]
        self.off += w
        if dtype != F32:
            v = v.bitcast(dtype)
        v = v[:, 0:n]
        if len(shape) == 3:
            v = v.rearrange("p (a b) -> p a b", a=shape[1])
        elif len(shape) == 4:
            v = v.rearrange("p (a b c) -> p a b c", a=shape[1], b=shape[2])
        return v


def build_nc(S, mode="full", dbg=False):
    nc = bass.Bass("TRN2", target_bir_lowering=False)
    NB = S // 512
    stack = contextlib.ExitStack()
    with stack:
        P = Prog(nc)
        dt = {}

        def din(name, shape, dtype=F32):
            dt[name] = nc.dram_tensor(name, list(shape), dtype, kind="ExternalInput").ap()
            return dt[name]

        def dscr(name, shape, dtype):
            return nc.dram_tensor(name, list(shape), dtype, kind="Internal").ap()

        x_d = din("x", [S, D])
        out_d = nc.dram_tensor("out", [S, D], F32, kind="ExternalOutput").ap()
        cst_d = din("cst", [128, CST_W])
        vec_d = din("vec", [128, VEC_W])
        mlp_up_d = din("mlp_up", [2, D, DFF])
        mlp_down_d = din("mlp_down", [2, DFF, D])
        gla_w_in_d = din("gla_w_in", [D, 3088])
        gla_w_o_d = din("gla_w_o", [D, D])
        din("gla_w_gk2", [16, 512])
        rwkv_w_rkv_d = din("rwkv_w_rkv", [3, D, D])
        rwkv_w_o_d = din("rwkv_w_o", [D, D])
        din("rwkv_w1", [D, 64])
        din("rwkv_w2", [64, D])
        din("rwkv_a1", [D, 64])
        din("rwkv_a2", [64, D])
        din("rwkv_g1", [D, 160])
        din("rwkv_g2", [160, D])
        din("rep", [128, 2 * D])
        wrkv_s = dscr("wrkv_s", [3, D, D], BF16)
        rwo_s = dscr("rwo_s", [D, D], BF16)
        win_s = dscr("win_s", [D, 3088], BF16)
        gwo_s = dscr("gwo_s", [D, D], BF16)

        up_s = dscr("up_s", [2, D, DFF], BF16)
        down_s = dscr("down_s", [2, DFF, D], BF16)
        hT_s = dscr("hT_s", [KC, 128, S], F32)

        ARENA_WORDS = 51 * 1024
        arena_t = stack.enter_context(nc.sbuf_tensor("arena", [128, ARENA_WORDS], F32))
        A = Arena(arena_t[:], ARENA_WORDS)
        psum2 = [stack.enter_context(nc.psum_tensor("pp%d" % i, [128, 1024], F32))[:]
                 for i in range(4)]
        psum = [psum2[i // 2][:, (i % 2) * 512:(i % 2 + 1) * 512] for i in range(8)]

        cst = A.alloc([128, CST_W], F32)
        vec = A.alloc([128, VEC_W], F32)
        ident_f = cst[:, C_IDENT:C_IDENT + 128]
        ones_bf = A.alloc([128, 128], BF16)
        P.add("sp", lambda e: e.dma_start(out=cst, in_=cst_d), writes=["cst"], dma_sem="cst")
        P.add("sp", lambda e: e.dma_start(out=vec, in_=vec_d), writes=["vec"], dma_sem="vec")
        P.add("dve", lambda e: e.tensor_copy(out=ones_bf, in_=cst[:, C_ONES:C_ONES + 128]),
              reads=["cst"], writes=["ones_bf"])
        ident_b = A.alloc([128, 128], BF16)
        P.add("dve", lambda e: e.tensor_copy(out=ident_b, in_=ident_f), reads=["cst"], writes=["ident_b"])
        base_off = A.off

        small_w = {}
        if mode in ("rwkv", "full", "gla"):
            specs = []
            if mode in ("rwkv", "full"):
                specs += [("w1", [128, KC, 64], dt["rwkv_w1"].rearrange("(k p) f -> p k f", p=128)),
                          ("a1", [128, KC, 64], dt["rwkv_a1"].rearrange("(k p) f -> p k f", p=128)),
                          ("g1", [128, KC, 160], dt["rwkv_g1"].rearrange("(k p) f -> p k f", p=128)),
                          ("w2", [64, D], dt["rwkv_w2"]), ("a2", [64, D], dt["rwkv_a2"]),
                          ("g2a", [128, D], dt["rwkv_g2"][0:128, :]), ("g2b", [32, D], dt["rwkv_g2"][128:160, :])]
            if mode in ("gla", "full"):
                specs += [("wgk2", [16, 512], dt["gla_w_gk2"])]
            for nm, shp, src in specs:
                small_w[nm] = A.alloc(shp, BF16)
                P.add("pool", lambda e, buf=small_w[nm], src=src: e.dma_start(out=buf, in_=src),
                      writes=[nm], dma_sem=nm)
        base_off = A.off

        cast_state = dict(n=0, toks=[])
        wkeys = {}

        def cast_w(src, dst, rows, cols, name):
            step = max(1, (1 << 20) // (cols * 4))
            wkeys[name] = []
            for r0 in range(0, rows, step):
                r1 = min(rows, r0 + step)
                n = cast_state["n"]
                extra = [cast_state["toks"][n - 2]] if n >= 2 else []
                key = (name, r0)
                wkeys[name].append(key)
                tok = P.add("pool", lambda e, r0=r0, r1=r1: e.dma_start(out=dst[r0:r1, :], in_=src[r0:r1, :]),
                            writes=[key], dma_sem="cast%d" % (n % 2), extra=extra)
                cast_state["toks"].append(tok)
                cast_state["n"] = n + 1

        def cast_mlp(l):
            cast_w(mlp_up_d[l], up_s[l], D, DFF, "up%d" % l)
            cast_w(mlp_down_d[l], down_s[l], DFF, D, "down%d" % l)

        if mode in ("rwkv", "full"):
            for i in range(3):
                cast_w(rwkv_w_rkv_d[i], wrkv_s[i], D, D, "wrkv%d" % i)
            cast_w(rwkv_w_o_d, rwo_s, D, D, "rwo")
        if mode in ("mlp", "full"):
            cast_mlp(0)
        if mode in ("gla", "full"):
            cast_w(gla_w_in_d, win_s, D, 3088, "win")
            cast_w(gla_w_o_d, gwo_s, D, D, "gwo")
        if mode in ("full",):
            cast_mlp(1)

        def phase_load_x():
            A.reset(base_off)
            xin = [A.alloc([128, 4, D], F32) for _ in range(2)]
            hb = [A.alloc([128, KC, 512], F32) for _ in range(2)]
            for b in range(NB):
                xi = xin[b % 2]
                h = hb[b % 2]
                P.add("sp", lambda e, xi=xi, b=b: e.dma_start(
                    out=xi, in_=x_d[b * 512:(b + 1) * 512, :].rearrange("(j p) d -> p j d", p=128)),
                    writes=[("xin", b % 2)], dma_sem="xin%d" % (b % 2))
                for kc in range(KC):
                    ps = psum[kc % 4]
                    for j in range(4):
                        P.add("pe", lambda e, ps=ps, xi=xi, j=j, kc=kc: e.transpose(
                            out=ps[:, j * 128:(j + 1) * 128], in_=xi[:, j, kc * 128:(kc + 1) * 128],
                            identity=ident_f),
                            reads=[("xin", b % 2), "cst"], writes=[("ps", kc % 4)])
                    eng = "dve" if kc % 2 == 0 else "act"
                    if eng == "dve":
                        P.add("dve", lambda e, ps=ps, h=h, kc=kc: e.tensor_copy(out=h[:, kc, :], in_=ps),
                              reads=[("ps", kc % 4)], writes=[("hb", b % 2, kc)])
                    else:
                        P.add("act", lambda e, ps=ps, h=h, kc=kc: e.copy(out=h[:, kc, :], in_=ps),
                              reads=[("ps", kc % 4)], writes=[("hb", b % 2, kc)])
                P.add("sp", lambda e, h=h, b=b: e.dma_start(
                    out=hT_s[:, :, b * 512:(b + 1) * 512].rearrange("k p s -> p k s"), in_=h),
                    reads=[("hb", b % 2, kc) for kc in range(KC)], writes=[("hT", b)],
                    dma_sem="hTw%d" % (b % 2))

        def rms_rstd(h, hkey, sq, sqkey, rstd, rkey, psb):
            for kc in range(KC):
                P.add("act", lambda e, kc=kc: e.activation(out=sq[:, kc, :], in_=h[:, kc, :], func=AF.Square),
                      reads=[hkey + (kc,)], writes=[sqkey + (kc,)])
            for kc in range(KC):
                P.add("pe", lambda e, kc=kc: e.matmul(psum[psb], lhsT=ones_bf, rhs=sq[:, kc, :],
                                                      start=(kc == 0), stop=(kc == KC - 1)),
                      reads=[sqkey + (kc,), "ones_bf"], writes=[("ps", psb)])
            P.add("act", lambda e: e.activation(out=rstd, in_=psum[psb], func=AF.Sqrt,
                                                scale=1.0 / D, bias=eps_col),
                  reads=[("ps", psb), "epsc"], writes=[rkey])
            P.add("dve", lambda e: e.reciprocal(out=rstd, in_=rstd), reads=[rkey], writes=[rkey])

        eps_col = A.alloc([128, 1], F32)
        P.add("dve", lambda e: e.memset(eps_col, NEPS), writes=["epsc"])
        base_off = A.off

        def phase_mlp(l, final):
            A.reset(base_off)
            hb = [A.alloc([128, KC, 512], F32) for _ in range(2)]
            xn = A.alloc([128, KC, 512], BF16)
            h1 = A.alloc([128, 32, 512], BF16)
            rstd = A.alloc([128, 512], F32)
            rl = [A.alloc([128, 512], F32) for _ in range(2)]
            NWU = 4
            wu = [A.alloc([128, KC, 512], BF16) for _ in range(NWU)]
            NWD = 4
            wd = [A.alloc([128, 8, 512], BF16) for _ in range(NWD)]
            if final:
                yo = [A.alloc([128, 4, D], F32) for _ in range(1)]
                yT = A.alloc([128, KC, 512], F32)
            g_ffn = vec[:, V_GFFN + l * KC: V_GFFN + (l + 1) * KC]
            g_fin = vec[:, V_GFIN: V_GFIN + KC]
            nu = 0
            nd = 0
            for b in range(NB):
                h = hb[b % 2]
                hkey = ("mh", b % 2)
                P.add("sp", lambda e, h=h, b=b: e.dma_start(
                    out=h, in_=hT_s[:, :, b * 512:(b + 1) * 512].rearrange("k p s -> p k s")),
                    reads=[("hT", b)], writes=[hkey + (kc,) for kc in range(KC)],
                    dma_sem="mh%d" % (b % 2))
                sq = h1[:, 0:KC, :]
                rms_rstd(h, hkey, sq, ("h1",), rstd, "rstd", 7)
                for kc in range(KC):
                    P.add("dve", lambda e, h=h, kc=kc: e.scalar_tensor_tensor(
                        out=xn[:, kc, :], in0=h[:, kc, :], scalar=g_ffn[:, kc:kc + 1], in1=rstd,
                        op0=ALU.mult, op1=ALU.mult),
                        reads=[hkey + (kc,), "rstd", "vec"], writes=[("xn", kc)])
                for eg in range(8):
                    w = wu[nu % NWU]
                    wkey = ("wu", nu % NWU)
                    P.add("sp", lambda e, w=w, eg=eg: e.dma_start(
                        out=w, in_=up_s[l][:, eg * 512:(eg + 1) * 512].rearrange("(k p) e -> p k e", p=128)),
                        reads=wkeys["up%d" % l], writes=[wkey], dma_sem="wu%d" % (nu % NWU))
                    nu += 1
                    for j in range(4):
                        et = eg * 4 + j
                        pb = et % 4
                        for kc in range(KC):
                            P.add("pe", lambda e, w=w, j=j, kc=kc, pb=pb: e.matmul(
                                psum[pb], lhsT=w[:, kc, j * 128:(j + 1) * 128], rhs=xn[:, kc, :],
                                start=(kc == 0), stop=(kc == KC - 1)),
                                reads=[wkey, ("xn", kc)], writes=[("ps", pb)])
                        r = rl[et % 2]
                        P.add("act", lambda e, r=r, pb=pb: e.activation(out=r, in_=psum[pb], func=AF.Relu),
                              reads=[("ps", pb)], writes=[("rl", et % 2)])
                        P.add("dve", lambda e, r=r, pb=pb, et=et: e.tensor_tensor(
                            out=h1[:, et, :], in0=r, in1=psum[pb], op=ALU.mult),
                            reads=[("ps", pb), ("rl", et % 2)], writes=[("h1", et)])
                for fg in range(2):
                    for e4 in range(4):
                        w = wd[nd % NWD]
                        wkey = ("wd", nd % NWD)
                        P.add("sp", lambda e, w=w, fg=fg, e4=e4: e.dma_start(
                            out=w, in_=down_s[l][e4 * 1024:(e4 + 1) * 1024, fg * 512:(fg + 1) * 512]
                            .rearrange("(k p) f -> p k f", p=128)),
                            reads=wkeys["down%d" % l], writes=[wkey], dma_sem="wd%d" % (nd % NWD))
                        nd += 1
                        for fj in range(4):
                            pb = 4 + fj
                            for ek in range(8):
                                et = e4 * 8 + ek
                                P.add("pe", lambda e, w=w, fj=fj, ek=ek, et=et, pb=pb, e4=e4: e.matmul(
                                    psum[pb], lhsT=w[:, ek, fj * 128:(fj + 1) * 128], rhs=h1[:, et, :],
                                    start=(e4 == 0 and ek == 0), stop=(e4 == 3 and ek == 7)),
                                    reads=[wkey, ("h1", et)], writes=[("ps", pb)])
                    for fj in range(4):
                        f = fg * 4 + fj
                        pb = 4 + fj
                        P.add("dve", lambda e, h=h, f=f, pb=pb: e.tensor_tensor(
                            out=h[:, f, :], in0=h[:, f, :], in1=psum[pb], op=ALU.add),
                            reads=[("ps", pb), hkey + (f,)], writes=[hkey + (f,)])
                if not final:
                    P.add("sp", lambda e, h=h, b=b: e.dma_start(
                        out=hT_s[:, :, b * 512:(b + 1) * 512].rearrange("k p s -> p k s"), in_=h),
                        reads=[hkey + (kc,) for kc in range(KC)], writes=[("hT", b)],
                        dma_sem="mhw%d" % (b % 2))
                else:
                    final_out(h, hkey, b, h1[:, 0:KC, :], ("h1",), rstd, yT, yo[0])

        def final_out(h, hkey, b, sq2, sqkey, rstd, yT, y):
            g_fin = vec[:, V_GFIN: V_GFIN + KC]
            rms_rstd(h, hkey, sq2, sqkey, rstd, "rstd", 7)
            for kc in range(KC):
                P.add("dve", lambda e, h=h, kc=kc: e.scalar_tensor_tensor(
                    out=yT[:, kc, :], in0=h[:, kc, :], scalar=g_fin[:, kc:kc + 1], in1=rstd,
                    op0=ALU.mult, op1=ALU.mult),
                    reads=[hkey + (kc,), "rstd", "vec"], writes=[("yT", kc)])
            for j in range(4):
                for half in range(2):
                    pb = (j * 2 + half) % 4
                    for q in range(4):
                        kc = half * 4 + q
                        P.add("pe", lambda e, pb=pb, q=q, kc=kc, j=j: e.transpose(
                            out=psum[pb][:, q * 128:(q + 1) * 128],
                            in_=yT[:, kc, j * 128:(j + 1) * 128], identity=ident_f),
                            reads=[("yT", kc), "cst"], writes=[("ps", pb)])
                    if half == 0:
                        P.add("act", lambda e, y=y, j=j, pb=pb: e.copy(
                            out=y[:, j, 0:512], in_=psum[pb]),
                            reads=[("ps", pb)], writes=[("yo", j, 0)])
                    else:
                        P.add("dve", lambda e, y=y, j=j, pb=pb: e.tensor_copy(
                            out=y[:, j, 512:1024], in_=psum[pb]),
                            reads=[("ps", pb)], writes=[("yo", j, 1)])
            P.add("sp", lambda e, y=y, b=b: e.dma_start(
                out=out_d[b * 512:(b + 1) * 512, :].rearrange("(j p) d -> p j d", p=128), in_=y),
                reads=[("yo", j, hf) for j in range(4) for hf in range(2)],
                writes=[("out", b)], dma_sem="out")

        def phase_final():
            A.reset(base_off)
            hb = [A.alloc([128, KC, 512], F32) for _ in range(2)]
            sq = A.alloc([128, KC, 512], BF16)
            rstd = A.alloc([128, 512], F32)
            yT = A.alloc([128, KC, 512], F32)
            y = A.alloc([128, 4, D], F32)
            for b in range(NB):
                h = hb[b % 2]
                hkey = ("fh", b % 2)
                P.add("sp", lambda e, h=h, b=b: e.dma_start(
                    out=h, in_=hT_s[:, :, b * 512:(b + 1) * 512].rearrange("k p s -> p k s")),
                    reads=[("hT", b)], writes=[hkey + (kc,) for kc in range(KC)],
                    dma_sem="fh%d" % (b % 2))
                final_out(h, hkey, b, sq, ("fsq",), rstd, yT, y)


        class WStream:
            def __init__(self, name, nslots, shape):
                self.name = name
                self.slots = [A.alloc(shape, BF16) for _ in range(nslots)]
                self.n = 0

            def load(self, src, srckeys, view=None):
                i = self.n % len(self.slots)
                self.n += 1
                w = self.slots[i]
                dst = w if view is None else view(w)
                key = (self.name, i)
                P.add("sp", lambda e: e.dma_start(out=dst, in_=src), reads=srckeys, writes=[key],
                      dma_sem="%s%d" % (self.name, i))
                return w, key

        def load_norm(b, h, hkey, sq, sqkey, rstd, hn, gcols, sem):
            P.add("sp", lambda e: e.dma_start(
                out=h, in_=hT_s[:, :, b * 512:(b + 1) * 512].rearrange("k p s -> p k s")),
                reads=[("hT", b)], writes=[hkey + (kc,) for kc in range(KC)], dma_sem=sem)
            rms_rstd(h, hkey, sq, sqkey, rstd, "rstd", 7)
            for kc in range(KC):
                P.add("dve", lambda e, kc=kc: e.scalar_tensor_tensor(
                    out=hn[:, kc, :], in0=h[:, kc, :], scalar=gcols[:, kc:kc + 1], in1=rstd,
                    op0=ALU.mult, op1=ALU.mult),
                    reads=[hkey + (kc,), "rstd", "vec"], writes=[("hn", kc)])

        def store_h(b, h, hkey, sem):
            P.add("sp", lambda e: e.dma_start(
                out=hT_s[:, :, b * 512:(b + 1) * 512].rearrange("k p s -> p k s"), in_=h),
                reads=[hkey + (kc,) for kc in range(KC)], writes=[("hT", b)], dma_sem=sem)

        def out_proj(wo, wokey, zT, zkey, h, hkey):
            for f in range(KC):
                pb = 4 + (f % 2)
                for kc in range(KC):
                    P.add("pe", lambda e, f=f, kc=kc, pb=pb: e.matmul(
                        psum[pb], lhsT=wo[:, kc, f * 128:(f + 1) * 128], rhs=zT[:, kc, :],
                        start=(kc == 0), stop=(kc == KC - 1)),
                        reads=[wokey, (zkey, kc)], writes=[("ps", pb)])
                P.add("dve", lambda e, f=f, pb=pb: e.tensor_tensor(
                    out=h[:, f, :], in0=h[:, f, :], in1=psum[pb], op=ALU.add),
                    reads=[("ps", pb), hkey + (f,)], writes=[hkey + (f,)])

        def phase_gla():
            A.reset(base_off)
            l = 1
            g_mix = vec[:, V_GMIX + l * KC: V_GMIX + (l + 1) * KC]
            bgk = vec[:, V_BGK:V_BGK + 4]
            mask_u = cst[:, C_MU:C_MU + 64]
            rmask = cst[:, C_RM:C_RM + 512]
            gn_rep = cst[:, C_GN:C_GN + 256]
            hb = [A.alloc([128, KC, 512], F32) for _ in range(2)]
            hn = A.alloc([128, KC, 512], BF16)
            sq = A.alloc([128, KC, 512], BF16)
            zT = sq
            rstd = A.alloc([128, 512], F32)
            wt = WStream("gw", 3, [128, KC, 512])
            wo = A.alloc([128, KC, D], BF16)
            wgl = A.alloc([128, KC, 16], BF16)
            wgk2 = small_w["wgk2"]
            gl = A.alloc([16, 512], BF16)
            gkp = A.alloc([128, 4, 512], F32)
            cum = A.alloc([128, 4, 512], F32)
            et = [A.alloc([128, 512], F32) for _ in range(2)]
            qtT = A.alloc([128, 4, 512], BF16)
            ktT = A.alloc([128, 4, 512], BF16)
            khT = A.alloc([128, 4, 512], BF16)
            kh = A.alloc([128, 4, 512], BF16)
            V = A.alloc([128, 4, D], BF16)
            sog = A.alloc([128, 4, D], F32)
            scT = A.alloc([128, 256], BF16)
            S32 = A.alloc([128, 4, 256], F32)
            Sbf = A.alloc([128, 4, 256], BF16)
            elast = A.alloc([128, 4, 8], F32)
            t1 = [A.alloc([128, 256], F32) for _ in range(2)]
            zb = [A.alloc([128, D], BF16) for _ in range(2)]
            ssq = A.alloc([128, 4], F32)
            rinv = A.alloc([128, 4], F32)
            junk = A.alloc([128, 256], BF16)
            ps_bf6 = psum[6].bitcast(BF16)

            P.add("sp", lambda e: e.dma_start(out=wo, in_=gwo_s.rearrange("(k p) f -> p k f", p=128)),
                  reads=wkeys["gwo"], writes=["gwo_sb"], dma_sem="gwo_sb")
            P.add("sp", lambda e: e.dma_start(
                out=wgl, in_=win_s[:, 3072:3088].rearrange("(k p) f -> p k f", p=128)),
                reads=wkeys["win"], writes=["wgl"], dma_sem="wgl")
            P.add("dve", lambda e: e.memset(S32, 0.0), writes=[("S32", hh) for hh in range(4)])
            P.add("dve", lambda e: e.memset(Sbf, 0.0), writes=[("Sbf", hh) for hh in range(4)])

            def wcols(c0):
                return win_s[:, c0:c0 + 512].rearrange("(k p) f -> p k f", p=128)

            for b in range(NB):
                h = hb[b % 2]
                hkey = ("gh", b % 2)
                load_norm(b, h, hkey, sq, ("sq",), rstd, hn, g_mix, "gh%d" % (b % 2))
                for kc in range(KC):
                    P.add("pe", lambda e, kc=kc: e.matmul(psum[4][0:16, :], lhsT=wgl[:, kc, :], rhs=hn[:, kc, :],
                                                          start=(kc == 0), stop=(kc == KC - 1)),
                          reads=["wgl", ("hn", kc)], writes=[("ps", 4)])
                P.add("act", lambda e: e.copy(out=gl, in_=psum[4][0:16, :]), reads=[("ps", 4)], writes=["gl"])
                for hh in range(4):
                    pb = 4 + (hh + 1) % 2
                    P.add("pe", lambda e, hh=hh, pb=pb: e.matmul(
                        psum[pb], lhsT=wgk2[:, hh * 128:(hh + 1) * 128], rhs=gl, start=True, stop=True),
                        reads=["wgk2", "gl"], writes=[("ps", pb)], rt=0)
                    P.add("act", lambda e, hh=hh, pb=pb: e.activation(
                        out=gkp[:, hh, :], in_=psum[pb], func=AF.Sigmoid, bias=bgk[:, hh:hh + 1]),
                        reads=[("ps", pb), "vec"], writes=[("gkp", hh)])
                for hh in range(4):
                    P.add("act", lambda e, hh=hh: e.activation(out=gkp[:, hh, :], in_=gkp[:, hh, :], func=AF.Ln),
                          reads=[("gkp", hh)], writes=[("gkp", hh)])
                    P.add("dve", lambda e, hh=hh: e.tensor_tensor_scan(
                        out=cum[:, hh, :], data0=rmask, data1=gkp[:, hh, :], initial=0.0,
                        op0=ALU.mult, op1=ALU.add),
                        reads=[("gkp", hh), "cst"], writes=[("cum", hh)])
                P.add("act", lambda e: e.activation(
                    out=elast, in_=cum.rearrange("p h (c t) -> p h c t", t=64)[:, :, :, 63],
                    func=AF.Exp, scale=1.0 / 16.0),
                    reads=[("cum", hh) for hh in range(4)], writes=["elast"])
                wq, wqk = wt.load(wcols(0), wkeys["win"])
                for hh in range(4):
                    pb = 4 + hh % 2
                    e_ = et[hh % 2]
                    for kc in range(KC):
                        P.add("pe", lambda e, hh=hh, kc=kc, pb=pb: e.matmul(
                            psum[pb], lhsT=wq[:, kc, hh * 128:(hh + 1) * 128], rhs=hn[:, kc, :],
                            start=(kc == 0), stop=(kc == KC - 1)),
                            reads=[wqk, ("hn", kc)], writes=[("ps", pb)])
                    P.add("act", lambda e, hh=hh, e_=e_: e.activation(
                        out=e_, in_=cum[:, hh, :], func=AF.Exp, scale=1.0 / 16.0),
                        reads=[("cum", hh)], writes=[("et", hh % 2)])
                    P.add("dve", lambda e, hh=hh, e_=e_, pb=pb: e.scalar_tensor_tensor(
                        out=qtT[:, hh, :], in0=psum[pb], scalar=128.0 ** -0.5, in1=e_,
                        op0=ALU.mult, op1=ALU.mult),
                        reads=[("ps", pb), ("et", hh % 2)], writes=[("qtT", hh)])
                wk, wkk = wt.load(wcols(512), wkeys["win"])
                for hh in range(4):
                    pb = 4 + hh % 2
                    for kc in range(KC):
                        P.add("pe", lambda e, hh=hh, kc=kc, pb=pb: e.matmul(
                            psum[pb], lhsT=wk[:, kc, hh * 128:(hh + 1) * 128], rhs=hn[:, kc, :],
                            start=(kc == 0), stop=(kc == KC - 1)),
                            reads=[wkk, ("hn", kc)], writes=[("ps", pb)])
                    P.add("act", lambda e, hh=hh: e.activation(
                        out=et[0], in_=cum[:, hh, :], func=AF.Exp, scale=-1.0 / 16.0),
                        reads=[("cum", hh)], writes=[("et", 0)])
                    P.add("dve", lambda e, hh=hh, pb=pb: e.tensor_tensor(
                        out=ktT[:, hh, :], in0=psum[pb], in1=et[0], op=ALU.mult),
                        reads=[("ps", pb), ("et", 0)], writes=[("ktT", hh)])
                    cv = cum[:, hh, :].rearrange("p (c t) -> p c t", t=64)
                    P.add("dve", lambda e, hh=hh, cv=cv: e.tensor_tensor(
                        out=et[1].rearrange("p (c t) -> p c t", t=64),
                        in0=cv[:, :, 63:64].to_broadcast([128, 8, 64]), in1=cv, op=ALU.subtract),
                        reads=[("cum", hh)], writes=[("et", 1)])
                    P.add("act", lambda e: e.activation(out=et[1], in_=et[1], func=AF.Exp, scale=1.0 / 16.0),
                          reads=[("et", 1)], writes=[("et", 1)])
                    P.add("dve", lambda e, hh=hh, pb=pb: e.tensor_tensor(
                        out=khT[:, hh, :], in0=psum[pb], in1=et[1], op=ALU.mult),
                        reads=[("ps", pb), ("et", 1)], writes=[("khT", hh)])
                for j in range(4):
                    for hh in range(4):
                        P.add("pe", lambda e, j=j, hh=hh: e.transpose(
                            out=ps_bf6[:, hh * 128:(hh + 1) * 128], in_=khT[:, hh, j * 128:(j + 1) * 128],
                            identity=ident_b),
                            reads=[("khT", hh), "ident_b"], writes=[("ps", 6)])
                    P.add("act", lambda e, j=j: e.copy(out=kh[:, j, :], in_=ps_bf6[:, 0:512]),
                          reads=[("ps", 6)], writes=[("kh", j)])
                for half in range(2):
                    wv, wvk = wt.load(wcols(1024 + half * 512), wkeys["win"])
                    for j in range(4):
                        pb = 4 + j % 2
                        for kc in range(KC):
                            P.add("pe", lambda e, j=j, kc=kc, pb=pb, wv=wv: e.matmul(
                                psum[pb], lhsT=hn[:, kc, j * 128:(j + 1) * 128], rhs=wv[:, kc, :],
                                start=(kc == 0), stop=(kc == KC - 1)),
                                reads=[wvk, ("hn", kc)], writes=[("ps", pb)])
                        P.add("act", lambda e, j=j, pb=pb, half=half: e.copy(
                            out=V[:, j, half * 512:(half + 1) * 512], in_=psum[pb]),
                            reads=[("ps", pb)], writes=[("V", j, half)])
                for half in range(2):
                    wg, wgk = wt.load(wcols(2048 + half * 512), wkeys["win"])
                    for j in range(4):
                        pb = 4 + j % 2
                        for kc in range(KC):
                            P.add("pe", lambda e, j=j, kc=kc, pb=pb, wg=wg: e.matmul(
                                psum[pb], lhsT=hn[:, kc, j * 128:(j + 1) * 128], rhs=wg[:, kc, :],
                                start=(kc == 0), stop=(kc == KC - 1)),
                                reads=[wgk, ("hn", kc)], writes=[("ps", pb)])
                        P.add("act", lambda e, j=j, pb=pb, half=half: e.activation(
                            out=sog[:, j, half * 512:(half + 1) * 512], in_=psum[pb], func=AF.Silu),
                            reads=[("ps", pb)], writes=[("sog", j, half)])
                for j in range(4):
                    for cc in range(2):
                        c = j * 2 + cc
                        p0 = cc * 64
                        t0 = j * 128 + cc * 64
                        for hh in range(4):
                            P.add("pe", lambda e, hh=hh, p0=p0, t0=t0: e.matmul(
                                psum[2][p0:p0 + 64, hh * 64:(hh + 1) * 64],
                                lhsT=ktT[:, hh, t0:t0 + 64], rhs=qtT[:, hh, t0:t0 + 64], start=True, stop=True),
                                reads=[("ktT", hh), ("qtT", hh)], writes=[("ps", 2)])
                        P.add("dve", lambda e, p0=p0: e.tensor_tensor(
                            out=scT[p0:p0 + 64, :].rearrange("p (h t) -> p h t", h=4),
                            in0=psum[2][p0:p0 + 64, 0:256].rearrange("p (h t) -> p h t", h=4),
                            in1=mask_u[p0:p0 + 64, :].unsqueeze(1).to_broadcast([64, 4, 64]), op=ALU.mult),
                            reads=[("ps", 2), "cst"], writes=[("scT", cc)])
                        for hh in range(4):
                            ob = hh // 2
                            oc = (hh % 2) * 256
                            P.add("pe", lambda e, hh=hh, p0=p0, ob=ob, oc=oc, j=j: e.matmul(
                                psum[ob][p0:p0 + 64, oc:oc + 256], lhsT=scT[p0:p0 + 64, hh * 64:(hh + 1) * 64],
                                rhs=V[p0:p0 + 64, j, hh * 256:(hh + 1) * 256], start=True, stop=False),
                                reads=[("scT", cc), ("V", j, hh // 2)], writes=[("ps", ob)], rt=p0)
                            P.add("pe", lambda e, hh=hh, p0=p0, ob=ob, oc=oc, t0=t0: e.matmul(
                                psum[ob][p0:p0 + 64, oc:oc + 256], lhsT=qtT[:, hh, t0:t0 + 64],
                                rhs=Sbf[:, hh, :], start=False, stop=True),
                                reads=[("qtT", hh), ("Sbf", hh)], writes=[("ps", ob)])
                        for hh in range(4):
                            gb = 3 if hh % 2 == 0 else 7
                            P.add("pe", lambda e, hh=hh, p0=p0, gb=gb, j=j: e.matmul(
                                psum[gb][:, 0:256], lhsT=kh[p0:p0 + 64, j, hh * 128:(hh + 1) * 128],
                                rhs=V[p0:p0 + 64, j, hh * 256:(hh + 1) * 256], start=True, stop=True),
                                reads=[("kh", j), ("V", j, hh // 2)], writes=[("ps", gb)], rt=p0)
                            P.add("dve", lambda e, hh=hh, gb=gb, c=c: e.scalar_tensor_tensor(
                                out=S32[:, hh, :], in0=S32[:, hh, :], scalar=elast[:, hh, c:c + 1],
                                in1=psum[gb][:, 0:256], op0=ALU.mult, op1=ALU.add),
                                reads=[("ps", gb), ("S32", hh), "elast"], writes=[("S32", hh)])
                            P.add("act", lambda e, hh=hh: e.copy(out=Sbf[:, hh, :], in_=S32[:, hh, :]),
                                  reads=[("S32", hh)], writes=[("Sbf", hh)])
                    z = zb[j % 2]
                    for hh in range(4):
                        ob = hh // 2
                        oc = (hh % 2) * 256
                        P.add("act", lambda e, hh=hh, ob=ob, oc=oc: e.activation(
                            out=junk, in_=psum[ob][:, oc:oc + 256], func=AF.Square, accum_out=ssq[:, hh:hh + 1]),
                            reads=[("ps", ob)], writes=["junk", ("ssq", hh)])
                    P.add("dve", lambda e: e.tensor_scalar(out=rinv, in0=ssq, scalar1=1.0 / 256.0, scalar2=NEPS,
                                                           op0=ALU.mult, op1=ALU.add),
                          reads=[("ssq", hh) for hh in range(4)], writes=["rinv"])
                    P.add("act", lambda e: e.activation(out=rinv, in_=rinv, func=AF.Sqrt),
                          reads=["rinv"], writes=["rinv"])
                    P.add("dve", lambda e: e.reciprocal(out=rinv, in_=rinv), reads=["rinv"], writes=["rinv"])
                    for hh in range(4):
                        ob = hh // 2
                        oc = (hh % 2) * 256
                        tt = t1[hh % 2]
                        P.add("dve", lambda e, hh=hh, ob=ob, oc=oc, tt=tt: e.scalar_tensor_tensor(
                            out=tt, in0=psum[ob][:, oc:oc + 256], scalar=rinv[:, hh:hh + 1], in1=gn_rep,
                            op0=ALU.mult, op1=ALU.mult),
                            reads=[("ps", ob), "rinv", "cst"], writes=[("t1", hh % 2)])
                        P.add("dve", lambda e, hh=hh, tt=tt, z=z, j=j: e.tensor_tensor(
                            out=z[:, hh * 256:(hh + 1) * 256], in0=tt, in1=sog[:, j, hh * 256:(hh + 1) * 256],
                            op=ALU.mult),
                            reads=[("t1", hh % 2), ("sog", j, hh // 2)], writes=[("zb", j % 2, hh)])
                    for kc in range(KC):
                        P.add("pe", lambda e, kc=kc, z=z: e.transpose(
                            out=ps_bf6[:, kc * 128:(kc + 1) * 128], in_=z[:, kc * 128:(kc + 1) * 128],
                            identity=ident_b),
                            reads=[("zb", j % 2, kc // 2), "ident_b"], writes=[("ps", 6)])
                    P.add("act", lambda e, j=j: e.copy(
                        out=zT[:, :, j * 128:(j + 1) * 128],
                        in_=ps_bf6.rearrange("p (k t) -> p k t", k=KC)),
                        reads=[("ps", 6)], writes=[("sq", kc) for kc in range(KC)])
                out_proj(wo, "gwo_sb", zT, "sq", h, hkey)
                store_h(b, h, hkey, "ghw%d" % (b % 2))


        def MM(out, lhsT, rhs, reads, writes, start=True, stop=True, rt=None):
            cls = None if lhsT.partition_size == 128 else lhsT.start_partition
            return P.add("pe", lambda e: e.matmul(out, lhsT=lhsT, rhs=rhs, start=start, stop=stop),
                         reads=reads, writes=writes, rt=cls)

        def TT(out, in0, in1, op, reads, writes):
            P.add("dve", lambda e: e.tensor_tensor(out=out, in0=in0, in1=in1, op=op), reads=reads, writes=writes)

        def STT(out, in0, scalar, in1, op0, op1, reads, writes):
            P.add("dve", lambda e: e.scalar_tensor_tensor(out=out, in0=in0, scalar=scalar, in1=in1,
                                                          op0=op0, op1=op1), reads=reads, writes=writes)

        def TS(out, in0, s1, s2, op0, op1, reads, writes):
            P.add("dve", lambda e: e.tensor_scalar(out=out, in0=in0, scalar1=s1, scalar2=s2, op0=op0, op1=op1),
                  reads=reads, writes=writes)

        def ACT(out, in_, func, reads, writes, **kw):
            P.add("act", lambda e: e.activation(out=out, in_=in_, func=func, **kw), reads=reads, writes=writes)

        def ACOPY(out, in_, reads, writes):
            P.add("act", lambda e: e.copy(out=out, in_=in_), reads=reads, writes=writes)

        def phase_rwkv():
            A.reset(base_off)
            HORDER = [h for i in range(8) for h in (i, i + 8)]
            T = 256
            NBR = S // T
            C0 = float(np.exp(-0.5))
            g_mix = vec[:, V_GMIX: V_GMIX + KC]
            mu = [vec[:, V_MU + i * KC: V_MU + (i + 1) * KC] for i in range(6)]
            w0c = vec[:, V_W0:V_W0 + KC]
            a0c = vec[:, V_A0:V_A0 + KC]
            kkc = vec[:, V_KK:V_KK + KC]
            kac = vec[:, V_KA:V_KA + KC]
            rkc = vec[:, V_RK:V_RK + KC]
            m_sl = cst[:, C_MSL:C_MSL + 64]
            m_su = cst[:, C_MSU:C_MSU + 64]
            m_u = cst[:, C_MU:C_MU + 64]
            ist = cst[:, C_IST:C_IST + 64]
            rmask = cst[:, C_RM:C_RM + T]

            def bc16(m):
                return m.unsqueeze(1).to_broadcast([128, 16, 64])

            rep = A.alloc([128, 2 * D], F32)
            lnw_rep = rep[:, 0:D]
            lnb_rep = rep[:, D:2 * D]
            omka = A.alloc([128, KC], F32)
            gneps = A.alloc([128, 1], F32)
            bones = A.alloc([128, 128], BF16)
            hsel = A.alloc([128, 2], BF16)
            w1, a1, g1, w2, a2, g2a, g2b = (small_w[n_] for n_ in ("w1", "a1", "g1", "w2", "a2", "g2a", "g2b"))
            P.add("sp", lambda e: e.dma_start(out=rep, in_=dt["rep"]), writes=["rep"], dma_sem="rep")
            TS(omka, kac, -1.0, 1.0, ALU.mult, ALU.add, ["vec"], ["omka"])
            P.add("dve", lambda e: e.memset(gneps, 64e-5), writes=["gneps"])
            P.add("dve", lambda e: e.tensor_copy(out=bones, in_=cst[:, C_BONES:C_BONES + 128]),
                  reads=["cst"], writes=["bones"])
            P.add("dve", lambda e: e.tensor_copy(out=hsel, in_=cst[:, C_HSEL:C_HSEL + 2]),
                  reads=["cst"], writes=["hsel"])

            hx = A.alloc([128, KC, T + 1], F32)
            sq = A.alloc([128, KC, T], BF16)
            zT = sq
            rstd = A.alloc([128, T], F32)
            xx = A.alloc([128, KC, T], F32)
            xm = [A.alloc([128, KC, T], BF16) for _ in range(2)]
            wt = WStream("rw", 3, [128, KC, 512])
            twT = A.alloc([64, T], BF16)
            taT = A.alloc([64, T], BF16)
            sgA = A.alloc([128, T], BF16)
            sgB = A.alloc([32, T], BF16)
            tn = ["s", "al", "cum", "kk", "rn", "kkn", "tmp", "kmod", "bs", "cp", "Em", "Ep", "Epv", "El"]
            tb = {n: A.alloc([128, T], F32) for n in tn}
            kk2 = A.alloc([128, T], BF16)
            ATf = A.alloc([128, KC, T], BF16)
            BTf = A.alloc([128, KC, T], BF16)
            KTf = A.alloc([128, KC, T], BF16)
            RTf = A.alloc([128, KC, T], BF16)
            BhTf = A.alloc([128, KC, T], BF16)
            KhTf = A.alloc([128, KC, T], BF16)
            rkT = [A.alloc([128, T], BF16) for _ in range(2)]
            Atm = A.alloc([128, 2, D], BF16)
            Bh = A.alloc([128, 2, D], BF16)
            Kh = A.alloc([128, 2, D], BF16)
            V = A.alloc([128, 2, D], BF16)
            g32 = A.alloc([128, 2, D], F32)
            rks = A.alloc([128, 2, 16], F32)
            wc = A.alloc([128, KC, 4], F32)
            cl = {n: A.alloc([128, D], BF16) for n in
                  ["Lm", "LTm", "AkT", "RbT", "RkT", "P0", "PT0", "P1", "PT1", "TT0", "TT1", "Xv", "Uv", "Ah",
                   "MT", "RhT"]}
            G32 = A.alloc([128, D], F32)
            H32 = A.alloc([128, KC, 64], F32)
            Hbf = A.alloc([128, KC, 64], BF16)
            y32 = A.alloc([128, D], F32)
            ysq = A.alloc([128, D], F32)
            yt = A.alloc([128, D], F32)
            zb = A.alloc([128, D], BF16)
            st = {n: A.alloc([128, 16], F32) for n in ["s1", "s2", "mean", "msq", "var"]}
            ps_bf6 = psum[6].bitcast(BF16)
            pp = [psum2[0], psum2[1], psum2[2]]

            P.add("dve", lambda e: e.memset(H32, 0.0), writes=["H32"])
            P.add("dve", lambda e: e.memset(Hbf, 0.0), writes=["Hbf"])
            P.add("dve", lambda e: e.memset(hx[:, :, 0:1], 0.0), writes=["hxp"])
            hxk = [("hx", kc) for kc in range(KC)]
            xxk = [("xx", kc) for kc in range(KC)]

            def wtile(wi, q):
                return wt.load(wrkv_s[wi][:, q * 512:(q + 1) * 512].rearrange("(k p) f -> p k f", p=128),
                               wkeys["wrkv%d" % wi])

            for b in range(NBR):
              try:
                if b > 0:
                    P.add("dve", lambda e: e.tensor_copy(out=hx[:, :, 0:1], in_=hx[:, :, T:T + 1]),
                          reads=hxk, writes=["hxp"])
                P.add("sp", lambda e, b=b: e.dma_start(
                    out=hx[:, :, 1:T + 1], in_=hT_s[:, :, b * T:(b + 1) * T].rearrange("k p s -> p k s")),
                    reads=[("hT", b)] + (["hxp"] if b > 0 else []), writes=hxk, dma_sem="rhx")
                hb_ = hx[:, :, 1:T + 1]
                for kc in range(KC):
                    ACT(sq[:, kc, :], hb_[:, kc, :], AF.Square, [("hx", kc)], [("sq", kc)])
                for kc in range(KC):
                    MM(psum[7][:, 0:T], ones_bf, sq[:, kc, :], [("sq", kc), "ones_bf"], [("ps", 7)],
                       start=(kc == 0), stop=(kc == KC - 1))
                ACT(rstd, psum[7][:, 0:T], AF.Sqrt, [("ps", 7), "epsc"], ["rstd"], scale=1.0 / D, bias=eps_col)
                P.add("dve", lambda e: e.reciprocal(out=rstd, in_=rstd), reads=["rstd"], writes=["rstd"])
                for kc in range(KC):
                    STT(hb_[:, kc, :], hb_[:, kc, :], g_mix[:, kc:kc + 1], rstd, ALU.mult, ALU.mult,
                        [("hx", kc), "rstd", "vec"], [("hx", kc)])
                for kc in range(KC):
                    TT(xx[:, kc, :], hx[:, kc, 0:T], hb_[:, kc, :], ALU.subtract,
                       [("hx", kc), "hxp"], [("xx", kc)])

                def mix(i, slot):
                    for kc in range(KC):
                        STT(xm[slot][:, kc, :], xx[:, kc, :], mu[i][:, kc:kc + 1], hb_[:, kc, :], ALU.mult, ALU.add,
                            [("xx", kc), ("hx", kc), "vec"], [("xm", slot, kc)])

                def proj_fm(out_ps, M, wfn, slot, wkey):
                    for kc in range(KC):
                        MM(out_ps, wfn(kc), xm[slot][:, kc, :], [wkey, ("xm", slot, kc)], [("ps", out_ps_id[0])],
                           start=(kc == 0), stop=(kc == KC - 1))

                out_ps_id = [0]
                chk('A0')
                mix(3, 0)
                out_ps_id[0] = 0
                proj_fm(psum[0][0:64, 0:T], 64, lambda kc: w1[:, kc, :], 0, "w1")
                ACT(twT, psum[0][0:64, 0:T], AF.Tanh, [("ps", 0)], ["twT"])
                mix(4, 1)
                out_ps_id[0] = 1
                proj_fm(psum[1][0:64, 0:T], 64, lambda kc: a1[:, kc, :], 1, "a1")
                ACOPY(taT, psum[1][0:64, 0:T], [("ps", 1)], ["taT"])
                mix(5, 0)
                out_ps_id[0] = 2
                proj_fm(psum[2][:, 0:T], 128, lambda kc: g1[:, kc, 0:128], 0, "g1")
                ACT(sgA, psum[2][:, 0:T], AF.Sigmoid, [("ps", 2)], ["sgA"])
                out_ps_id[0] = 3
                proj_fm(psum[3][0:32, 0:T], 32, lambda kc: g1[:, kc, 128:160], 0, "g1")
                ACT(sgB, psum[3][0:32, 0:T], AF.Sigmoid, [("ps", 3)], ["sgB"])
                for j in range(2):
                    for half in range(2):
                        pb = (j * 2 + half) % 4
                        MM(psum[pb], sgA[:, j * 128:(j + 1) * 128], g2a[:, half * 512:(half + 1) * 512],
                           ["sgA", "g2a"], [("ps", pb)], start=True, stop=False)
                        MM(psum[pb], sgB[:, j * 128:(j + 1) * 128], g2b[:, half * 512:(half + 1) * 512],
                           ["sgB", "g2b"], [("ps", pb)], start=False, stop=True)
                        ACOPY(g32[:, j, half * 512:(half + 1) * 512], psum[pb], [("ps", pb)], [("g32", j)])
                mix(2, 1)
                for half in range(2):
                    wv, wvk = wtile(2, half)
                    for j in range(2):
                        pb = (j * 2 + half) % 4
                        for kc in range(KC):
                            MM(psum[pb], xm[1][:, kc, j * 128:(j + 1) * 128], wv[:, kc, :],
                               [wvk, ("xm", 1, kc)], [("ps", pb)], start=(kc == 0), stop=(kc == KC - 1))
                        ACOPY(V[:, j, half * 512:(half + 1) * 512], psum[pb], [("ps", pb)], [("V", j)])
                chk('A1')
                mix(0, 0)
                mix(1, 1)
                for p in range(KC):
                    if p % 4 == 0:
                        wr, wrk = wtile(0, p // 4)
                        wk_, wkk = wtile(1, p // 4)
                    c4 = (p % 4) * 128
                    for kc in range(KC):
                        MM(psum[0][:, 0:T], wr[:, kc, c4:c4 + 128], xm[0][:, kc, :], [wrk, ("xm", 0, kc)],
                           [("ps", 0)], start=(kc == 0), stop=(kc == KC - 1))
                    for kc in range(KC):
                        MM(psum[1][:, 0:T], wk_[:, kc, c4:c4 + 128], xm[1][:, kc, :], [wkk, ("xm", 1, kc)],
                           [("ps", 1)], start=(kc == 0), stop=(kc == KC - 1))
                    MM(psum[2][:, 0:T], w2[:, p * 128:(p + 1) * 128], twT, ["w2", "twT"], [("ps", 2)])
                    MM(psum[3][:, 0:T], a2[:, p * 128:(p + 1) * 128], taT, ["a2", "taT"], [("ps", 3)])
                    r_ps = psum[0][:, 0:T]
                    k_ps = psum[1][:, 0:T]
                    pc = slice(p, p + 1)
                    ACT(tb["s"], psum[2][:, 0:T], AF.Sigmoid, [("ps", 2), "vec"], ["t_s"], bias=w0c[:, pc])
                    ACT(tb["al"], psum[3][:, 0:T], AF.Sigmoid, [("ps", 3), "vec"], ["t_al"], bias=a0c[:, pc])
                    P.add("dve", lambda e: e.tensor_tensor_scan(
                        out=tb["cum"], data0=rmask, data1=tb["s"], initial=0.0, op0=ALU.mult, op1=ALU.add),
                        reads=["t_s", "cst"], writes=["t_cum"])
                    TS(tb["kk"], k_ps, kkc[:, pc], None, ALU.mult, ALU.bypass, [("ps", 1), "vec"], ["t_kk"])
                    ACT(kk2, tb["kk"], AF.Square, ["t_kk"], ["kk2"])
                    MM(psum[6][:, 0:T], bones, kk2, ["bones", "kk2"], [("ps", 6)])
                    ACT(tb["rn"], psum[6][:, 0:T], AF.Sqrt, [("ps", 6)], ["t_rn"])
                    TS(tb["rn"], tb["rn"], 1e-12, None, ALU.max, ALU.bypass, ["t_rn"], ["t_rn"])
                    P.add("dve", lambda e: e.reciprocal(out=tb["rn"], in_=tb["rn"]), reads=["t_rn"], writes=["t_rn"])
                    TT(tb["kkn"], tb["kk"], tb["rn"], ALU.mult, ["t_kk", "t_rn"], ["t_kkn"])
                    TS(tb["tmp"], tb["al"], kac[:, pc], omka[:, pc], ALU.mult, ALU.add,
                       ["t_al", "vec", "omka"], ["t_tmp"])
                    TT(tb["kmod"], k_ps, tb["tmp"], ALU.mult, [("ps", 1), "t_tmp"], ["t_kmod"])
                    TT(tb["bs"], tb["kkn"], tb["al"], ALU.mult, ["t_kkn", "t_al"], ["t_bs"])
                    ACT(tb["Em"], tb["cum"], AF.Exp, ["t_cum"], ["t_Em"], scale=C0)
                    ACT(tb["Ep"], tb["cum"], AF.Exp, ["t_cum"], ["t_Ep"], scale=-C0)
                    TT(tb["cp"], tb["cum"], tb["s"], ALU.subtract, ["t_cum", "t_s"], ["t_cp"])
                    ACT(tb["Epv"], tb["cp"], AF.Exp, ["t_cp"], ["t_Epv"], scale=-C0)
                    cv = tb["cum"].rearrange("p (c t) -> p c t", t=64)
                    TT(tb["cp"].rearrange("p (c t) -> p c t", t=64), cv,
                       cv[:, :, 63:64].to_broadcast([128, T // 64, 64]), ALU.subtract, ["t_cum", "t_Epv"], ["t_cp"])
                    ACT(tb["El"], tb["cp"], AF.Exp, ["t_cp"], ["t_El"], scale=C0)
                    STT(ATf[:, p, :], tb["kkn"], -1.0, tb["Epv"], ALU.mult, ALU.mult, ["t_kkn", "t_Epv"], [("ATf", p)])
                    TT(BTf[:, p, :], tb["bs"], tb["Em"], ALU.mult, ["t_bs", "t_Em"], [("BTf", p)])
                    TT(KTf[:, p, :], tb["kmod"], tb["Em"], ALU.mult, ["t_kmod", "t_Em"], [("KTf", p)])
                    TT(RTf[:, p, :], r_ps, tb["Ep"], ALU.mult, [("ps", 0), "t_Ep"], [("RTf", p)])
                    TT(BhTf[:, p, :], tb["bs"], tb["El"], ALU.mult, ["t_bs", "t_El"], [("BhTf", p)])
                    TT(KhTf[:, p, :], tb["kmod"], tb["El"], ALU.mult, ["t_kmod", "t_El"], [("KhTf", p)])
                    rk_ = rkT[p % 2]
                    STT(rk_, r_ps, rkc[:, pc], tb["kmod"], ALU.mult, ALU.mult, [("ps", 0), "t_kmod", "vec"],
                        [("rkT", p % 2)])
                    ACOPY(wc[:, p, :], tb["Ep"].rearrange("p (c t) -> p c t", t=64)[:, :, 63], ["t_Ep"], [("wc", p)])
                    for j in range(2):
                        MM(psum[7][:, j * 16 + p * 2: j * 16 + p * 2 + 2], rk_[:, j * 128:(j + 1) * 128], hsel,
                           [("rkT", p % 2), "hsel"], [("ps", 7)])
                ACOPY(rks, psum[7][:, 0:32].rearrange("p (j h) -> p j h", j=2), [("ps", 7)], ["rks"])
                chk('A2')
                for (srcf, skey, dstm, dkey) in ((ATf, "ATf", Atm, "Atm"), (BhTf, "BhTf", Bh, "Bh"), (KhTf, "KhTf", Kh, "Kh")):
                    for j in range(2):
                        for p in range(KC):
                            P.add("pe", lambda e, srcf=srcf, j=j, p=p: e.transpose(
                                out=ps_bf6[:, p * 128:(p + 1) * 128], in_=srcf[:, p, j * 128:(j + 1) * 128],
                                identity=ident_b), reads=[(skey, p), "ident_b"], writes=[("ps", 6)])
                        ACOPY(dstm[:, j, :], ps_bf6, [("ps", 6)], [(dkey, j)])

                chk('A3')
                for j in range(2):
                    rot = [0]

                    def nextpp():
                        k = rot[0] % 3
                        rot[0] += 1
                        return k

                    def layer(k, opfn, rd, feat_out=False, rtk="p0", ccs=(0, 1), kp=None):
                        jobs = [(cc, hh) for cc in ccs for hh in range(16)]
                        rtof = (lambda cc, hh: (hh % 2) * 64) if rtk == "hb" else (lambda cc, hh: cc * 64)
                        first = P.pe_cls if P.pe_cls in (0, 64) else 0
                        jobs.sort(key=lambda ch: (rtof(*ch) != first, ch[0], ch[1]))
                        wkeys_ = kp if kp is not None else [("ps", 2 * k), ("ps", 2 * k + 1)]
                        for cc, hh in jobs:
                            p_, hb2, p0 = hh // 2, (hh % 2) * 64, cc * 64
                            if feat_out:
                                o = pp[k][hb2:hb2 + 64, (cc * 8 + p_) * 64:(cc * 8 + p_ + 1) * 64]
                            else:
                                o = pp[k][p0:p0 + 64, hh * 64:(hh + 1) * 64]
                            ops = opfn(cc, hh)
                            for i, (l_, r_) in enumerate(ops):
                                MM(o, l_, r_, rd, wkeys_, start=(i == 0), stop=(i == len(ops) - 1),
                                   rt=rtof(cc, hh))

                    def tsl(buf, cc, hh):
                        return buf[cc * 64:cc * 64 + 64, hh * 64:(hh + 1) * 64]

                    def fsl(buf, cc, hh):
                        p_, hb2 = hh // 2, (hh % 2) * 64
                        return buf[hb2:hb2 + 64, (cc * 8 + p_) * 64:(cc * 8 + p_ + 1) * 64]

                    def fT(bufF, cc, hh):
                        p_, hb2 = hh // 2, (hh % 2) * 64
                        t0 = j * 128 + cc * 64
                        return bufF[hb2:hb2 + 64, p_, t0:t0 + 64]

                    def tM(bufM, cc, hh):
                        return bufM[cc * 64:cc * 64 + 64, j, hh * 64:(hh + 1) * 64]

                    def pk(k):
                        return [("ps", 2 * k), ("ps", 2 * k + 1)]

                    def ACOPY2(dst, k, wkey):
                        for hf in range(2):
                            ACOPY(dst[:, hf * 512:(hf + 1) * 512], pp[k][:, hf * 512:(hf + 1) * 512],
                                  [("ps", 2 * k + hf)], [wkey])

                    fkeys = lambda nm: [(nm, p_) for p_ in range(KC)]

                    def evac_mask(k, dst, mask):
                        for hf in range(2):
                            sl = slice(hf * 512, (hf + 1) * 512)
                            TT(cl[dst][:, sl].rearrange("p (h t) -> p h t", h=8),
                               pp[k][:, sl].rearrange("p (h t) -> p h t", h=8),
                               mask.unsqueeze(1).to_broadcast([128, 8, 64]), ALU.mult,
                               [("ps", 2 * k + hf), "cst"], [dst])

                    k = nextpp()
                    layer(k, lambda cc, hh: [(fT(ATf, cc, hh), fT(BTf, cc, hh))], fkeys("ATf") + fkeys("BTf"), rtk="hb")
                    evac_mask(k, "Lm", m_sl)
                    k = nextpp()
                    layer(k, lambda cc, hh: [(fT(BTf, cc, hh), fT(ATf, cc, hh))], fkeys("ATf") + fkeys("BTf"), rtk="hb")
                    evac_mask(k, "LTm", m_su)
                    TT(cl["TT0"].rearrange("p (h t) -> p h t", h=16), cl["LTm"].rearrange("p (h t) -> p h t", h=16),
                       bc16(ist), ALU.add, ["LTm", "cst"], ["TT0"])
                    k = nextpp()
                    layer(k, lambda cc, hh: [(fT(KTf, cc, hh), fT(ATf, cc, hh))], fkeys("ATf") + fkeys("KTf"), rtk="hb")
                    evac_mask(k, "AkT", m_su)
                    k = nextpp()
                    layer(k, lambda cc, hh: [(fT(BTf, cc, hh), fT(RTf, cc, hh))], fkeys("RTf") + fkeys("BTf"), rtk="hb")
                    evac_mask(k, "RbT", m_u)
                    k = nextpp()
                    layer(k, lambda cc, hh: [(fT(KTf, cc, hh), fT(RTf, cc, hh))], fkeys("RTf") + fkeys("KTf"), rtk="hb")
                    evac_mask(k, "RkT", m_u)
                    chk('B0')
                    k = nextpp()
                    layer(k, lambda cc, hh: [(tsl(cl["AkT"], cc, hh), tM(V, cc, hh))], ["AkT", ("V", j)])
                    ACOPY2(cl["Xv"], k, "Xv")
                    chk('B1')
                    Pn, PTn, TTn = "Lm", "LTm", "TT0"
                    for lev in range(5):
                        Pnew = "P%d" % (lev % 2)
                        PTnew = "PT%d" % (lev % 2)
                        TTnew = "TT%d" % ((lev + 1) % 2)
                        k = nextpp()
                        layer(k, lambda cc, hh, PTn=PTn, Pn=Pn: [(tsl(cl[PTn], cc, hh), tsl(cl[Pn], cc, hh))], [Pn, PTn])
                        ACOPY2(cl[Pnew], k, Pnew)
                        if lev < 4:
                            k = nextpp()
                            layer(k, lambda cc, hh, PTn=PTn, Pn=Pn: [(tsl(cl[Pn], cc, hh), tsl(cl[PTn], cc, hh))], [Pn, PTn])
                            ACOPY2(cl[PTnew], k, PTnew)
                        k = nextpp()
                        layer(k, lambda cc, hh, Pnew=Pnew, TTn=TTn: [(tsl(cl[Pnew], cc, hh), tsl(cl[TTn], cc, hh))],
                              [Pnew, TTn])
                        for hf in range(2):
                            sl = slice(hf * 512, (hf + 1) * 512)
                            TT(cl[TTnew][:, sl], pp[k][:, sl], cl[TTn][:, sl], ALU.add,
                               [("ps", 2 * k + hf), TTn], [TTnew])
                        Pn, PTn, TTn = Pnew, PTnew, TTnew
                    k = nextpp()
                    layer(k, lambda cc, hh: [(tsl(cl[TTn], cc, hh), tsl(cl["Xv"], cc, hh))], [TTn, "Xv"])
                    ACOPY2(cl["Uv"], k, "Uv")
                    k = nextpp()
                    layer(k, lambda cc, hh: [(tsl(cl[TTn], cc, hh), tM(Atm, cc, hh))], [TTn, ("Atm", j)])
                    ACOPY2(cl["Ah"], k, "Ah")
                    chk('B2')
                    k = nextpp()
                    layer(k, lambda cc, hh: [(tsl(cl["Ah"], cc, hh), tM(Bh, cc, hh))], ["Ah", ("Bh", j)], feat_out=True)
                    for cc in range(2):
                        for p_ in range(KC):
                            blk = slice((cc * 8 + p_) * 64, (cc * 8 + p_ + 1) * 64)
                            STT(cl["MT"][:, blk], ist, wc[:, p_, j * 2 + cc: j * 2 + cc + 1], pp[k][:, blk],
                                ALU.mult, ALU.add, pk(k) + ["cst", ("wc", p_)], ["MT"])
                    k = nextpp()
                    layer(k, lambda cc, hh: [(tM(Bh, cc, hh), tsl(cl["Uv"], cc, hh)), (tM(Kh, cc, hh), tM(V, cc, hh))],
                          ["Uv", ("Bh", j), ("Kh", j), ("V", j)], feat_out=True)
                    ACOPY2(G32, k, "G32")
                    k = nextpp()
                    layer(k, lambda cc, hh: [(tsl(cl["Ah"], cc, hh), tsl(cl["RbT"], cc, hh))], ["Ah", "RbT"], feat_out=True)
                    for cc in range(2):
                        t0 = j * 128 + cc * 64
                        TT(cl["RhT"][:, cc * 512:(cc + 1) * 512].rearrange("p (k t) -> p k t", k=KC),
                           pp[k][:, cc * 512:(cc + 1) * 512].rearrange("p (k t) -> p k t", k=KC),
                           RTf[:, :, t0:t0 + 64], ALU.add, pk(k) + fkeys("RTf"), ["RhT"])
                    chk('B3')
                    layer(2, lambda cc, hh: [(tsl(cl["RbT"], cc, hh), tsl(cl["Uv"], cc, hh)),
                                             (tsl(cl["RkT"], cc, hh), tM(V, cc, hh))],
                          ["RbT", "Uv", "RkT", ("V", j)])
                    for cc in range(2):
                        layer(0, lambda cc_, hh: [(fsl(cl["RhT"], cc_, hh), Hbf[(hh % 2) * 64:(hh % 2) * 64 + 64, hh // 2, :])],
                              ["RhT", "Hbf"], rtk="hb", ccs=(cc,))
                        hjobs = list(range(16))
                        first = P.pe_cls if P.pe_cls in (0, 64) else 0
                        hjobs.sort(key=lambda hh: ((hh % 2) * 64 != first, hh))
                        for hh in hjobs:
                            p_, hb2 = hh // 2, (hh % 2) * 64
                            hbank = 7 if hb2 == 0 else 6
                            MM(psum[hbank][hb2:hb2 + 64, p_ * 64:(p_ + 1) * 64], fsl(cl["MT"], cc, hh),
                               Hbf[hb2:hb2 + 64, p_, :], ["MT", "Hbf"], [("ps", hbank)], rt=hb2)
                        H32f = H32.rearrange("p k v -> p (k v)")
                        TT(H32f[0:64, :], psum[7][0:64, :], G32[0:64, cc * 512:(cc + 1) * 512], ALU.add,
                           [("ps", 7), "G32"], ["H32"])
                        TT(H32f[64:128, :], psum[6][64:128, :], G32[64:128, cc * 512:(cc + 1) * 512], ALU.add,
                           [("ps", 6), "G32"], ["H32"])
                        ACOPY(Hbf, H32, ["H32"], ["Hbf"])
                    chk('B4')
                    ACOPY2(y32, 2, "y32")
                    for hf in range(2):
                        sl = slice(hf * 512, (hf + 1) * 512)
                        TT(y32[:, sl], y32[:, sl], pp[0][:, sl], ALU.add, [("ps", hf), "y32"], ["y32"])
                    ACT(ysq, y32, AF.Square, ["y32"], ["ysq"])
                    v3 = lambda a: a.rearrange("p (h t) -> p h t", h=16)
                    P.add("dve", lambda e: e.tensor_reduce(out=st["s1"], in_=v3(y32), axis=AX.X, op=ALU.add),
                          reads=["y32"], writes=["s1"])
                    P.add("dve", lambda e: e.tensor_reduce(out=st["s2"], in_=v3(ysq), axis=AX.X, op=ALU.add),
                          reads=["ysq"], writes=["s2"])
                    TS(st["mean"], st["s1"], 1.0 / 64.0, None, ALU.mult, ALU.bypass, ["s1"], ["mean"])
                    TT(st["msq"], st["mean"], st["mean"], ALU.mult, ["mean"], ["msq"])
                    STT(st["var"], st["s2"], 1.0 / 64.0, st["msq"], ALU.mult, ALU.subtract, ["s2", "msq"], ["var"])
                    ACT(st["var"], st["var"], AF.Sqrt, ["var", "gneps"], ["var"], bias=gneps)
                    P.add("dve", lambda e: e.reciprocal(out=st["var"], in_=st["var"]), reads=["var"], writes=["var"])
                    bcs = lambda a: a.unsqueeze(2).to_broadcast([128, 16, 64])
                    TT(v3(y32), v3(y32), bcs(st["mean"]), ALU.subtract, ["y32", "mean"], ["y32"])
                    TT(v3(y32), v3(y32), bcs(st["var"]), ALU.mult, ["y32", "var"], ["y32"])
                    TT(y32, y32, lnw_rep, ALU.mult, ["y32", "rep"], ["y32"])
                    TT(y32, y32, lnb_rep, ALU.add, ["y32", "rep"], ["y32"])
                    TT(v3(yt), v3(V[:, j, :]), bcs(rks[:, j, :]), ALU.mult, [("V", j), "rks"], ["yt"])
                    TT(y32, y32, yt, ALU.add, ["y32", "yt"], ["y32"])
                    TT(zb, y32, g32[:, j, :], ALU.mult, ["y32", ("g32", j)], ["zb"])
                    for kc in range(KC):
                        P.add("pe", lambda e, kc=kc: e.transpose(
                            out=ps_bf6[:, kc * 128:(kc + 1) * 128], in_=zb[:, kc * 128:(kc + 1) * 128],
                            identity=ident_b), reads=["zb", "ident_b"], writes=[("ps", 6)])
                    ACOPY(zT[:, :, j * 128:(j + 1) * 128], ps_bf6.rearrange("p (k t) -> p k t", k=KC),
                          [("ps", 6)], [("sq", kc) for kc in range(KC)])
                chk('B5')
                P.add("sp", lambda e, b=b: e.dma_start(
                    out=xx, in_=hT_s[:, :, b * T:(b + 1) * T].rearrange("k p s -> p k s")),
                    reads=[("hT", b)], writes=xxk, dma_sem="rxx")
                for q in range(2):
                    wo_, wok = wt.load(rwo_s[:, q * 512:(q + 1) * 512].rearrange("(k p) f -> p k f", p=128),
                                       wkeys["rwo"])
                    for f4 in range(4):
                        f = q * 4 + f4
                        pb = f % 2
                        for kc in range(KC):
                            MM(psum[pb][:, 0:T], wo_[:, kc, f4 * 128:(f4 + 1) * 128], zT[:, kc, :],
                               [wok, ("sq", kc)], [("ps", pb)], start=(kc == 0), stop=(kc == KC - 1))
                        TT(xx[:, f, :], xx[:, f, :], psum[pb][:, 0:T], ALU.add, [("ps", pb), ("xx", f)], [("xx", f)])
                P.add("sp", lambda e, b=b: e.dma_start(
                    out=hT_s[:, :, b * T:(b + 1) * T].rearrange("k p s -> p k s"), in_=xx),
                    reads=xxk, writes=[("hT", b)], dma_sem="rxw")
              except _Stop:
                break

        phase_load_x()
        P.barrier()
        if mode == "mlp":
            phase_mlp(0, True)
        if mode == "gla":
            phase_gla()
            P.barrier()
            phase_final()
        if mode == "rwkv":
            phase_rwkv()
            P.barrier()
            phase_final()
        if mode == "full":
            phase_rwkv()
            P.barrier()
            phase_mlp(0, False)
            P.barrier()
            phase_gla()
            P.barrier()
            phase_mlp(1, True)
        P.add("sp", None, extra=P.last_tokens())
        P.emit(stack)
    return nc


C_IDENT = 0
C_ONES = 128
C_MU = 256
C_RM = 320
C_GN = 832
C_MSL = 1088
C_MSU = 1152
C_IST = 1216
C_BONES = 1280
C_HSEL = 1408
CST_W = 1410
V_GMIX = 0
V_GFFN = 16
V_GFIN = 32
V_BGK = 40
V_MU = 44
V_W0 = 92
V_A0 = 100
V_KK = 108
V_KA = 116
V_RK = 124
VEC_W = 132


def fm(v):
    return np.ascontiguousarray(np.asarray(v, np.float32).reshape(KC, 128).T)


def make_tables(inp):
    cst = np.zeros((128, CST_W), np.float32)
    cst[:, C_IDENT:C_IDENT + 128] = np.eye(128, dtype=np.float32)
    cst[:, C_ONES:C_ONES + 128] = 1.0
    pp = np.arange(128)[:, None] % 64
    tt = np.arange(64)[None, :]
    cst[:, C_MU:C_MU + 64] = (tt >= pp)
    cst[:, C_RM:C_RM + 512] = (np.arange(512)[None, :] % 64 != 0)
    cst[:, C_GN:C_GN + 256] = np.asarray(inp["gla_gnorm_g"], np.float32).reshape(1, 256)
    cst[:, C_MSL:C_MSL + 64] = (tt < pp)
    cst[:, C_MSU:C_MSU + 64] = (tt > pp)
    cst[:, C_IST:C_IST + 64] = (tt == pp)
    blk = np.arange(128) // 64
    cst[:, C_BONES:C_BONES + 128] = (blk[:, None] == blk[None, :])
    cst[:, C_HSEL:C_HSEL + 2] = (blk[:, None] == np.arange(2)[None, :])
    vec = np.zeros((128, VEC_W), np.float32)
    for l in range(2):
        vec[:, V_GMIX + l * KC:V_GMIX + (l + 1) * KC] = fm(inp["norm_mix_g"][l])
        vec[:, V_GFFN + l * KC:V_GFFN + (l + 1) * KC] = fm(inp["norm_ffn_g"][l])
    vec[:, V_GFIN:V_GFIN + KC] = fm(inp["final_g"])
    vec[:, V_BGK:V_BGK + 4] = np.asarray(inp["gla_b_gk2"], np.float32).reshape(4, 128).T
    for i in range(6):
        vec[:, V_MU + i * KC:V_MU + (i + 1) * KC] = fm(inp["rwkv_mu"][0][i])
    vec[:, V_W0:V_W0 + KC] = fm(inp["rwkv_w0"][0])
    vec[:, V_A0:V_A0 + KC] = fm(inp["rwkv_a0"][0])
    vec[:, V_KK:V_KK + KC] = fm(inp["rwkv_k_k"][0])
    vec[:, V_KA:V_KA + KC] = fm(inp["rwkv_k_a"][0])
    vec[:, V_RK:V_RK + KC] = fm(np.asarray(inp["rwkv_r_k"][0]).reshape(-1))
    return cst, vec


def make_in_map(inp, c, S=None):
    cst, vec = make_tables(inp)
    x = inp["x"][c]
    if S is not None:
        x = x[:S]
    m = dict(x=np.ascontiguousarray(x), cst=cst, vec=vec,
             mlp_up=np.ascontiguousarray(inp["mlp_up"]),
             mlp_down=np.ascontiguousarray(inp["mlp_down"]),
             gla_w_in=np.ascontiguousarray(inp["gla_w_in"][0]),
             gla_w_o=np.ascontiguousarray(inp["gla_w_o"][0]),
             gla_w_gk2=np.ascontiguousarray(inp["gla_w_gk2"][0]),
             rwkv_w_rkv=np.ascontiguousarray(inp["rwkv_w_rkv"][0]),
             rwkv_w_o=np.ascontiguousarray(inp["rwkv_w_o"][0]),
             rwkv_w1=np.ascontiguousarray(inp["rwkv_w1"][0]), rwkv_w2=np.ascontiguousarray(inp["rwkv_w2"][0]),
             rwkv_a1=np.ascontiguousarray(inp["rwkv_a1"][0]), rwkv_a2=np.ascontiguousarray(inp["rwkv_a2"][0]),
             rwkv_g1=np.ascontiguousarray(inp["rwkv_g1"][0]), rwkv_g2=np.ascontiguousarray(inp["rwkv_g2"][0]),
             rep=np.ascontiguousarray(np.concatenate(
                 [np.broadcast_to(np.asarray(inp["rwkv_lnx_w"][0], np.float32)[None, :], (128, D)),
                  np.broadcast_to(np.asarray(inp["rwkv_lnx_b"][0], np.float32)[None, :], (128, D))], axis=1)))
    return m


def kernel(**inp):
    S = inp["x"].shape[1]
    B = inp["x"].shape[0]
    nc = build_nc(S)
    in_maps = [make_in_map(inp, c) for c in range(B)]
    res = run_bass_kernel_spmd(nc, in_maps, core_ids=list(range(B)))
    return np.stack([r["out"] for r in res.results], axis=0)
```
